# Optimizing a Trainium2 kernel written in Bass

```python
import math
import jax, jax.numpy as jnp
from jax import lax
import numpy as np

D_MODEL = 1024
BATCH = 8
SEQ = 2048
DEPTH = 1
DEC_BATCH = 128
DEC_SEQ = 8
PAST_LEN = 2048
PAGE_SIZE = 128

ATT_W = D_MODEL // 2
HEAD_DIM = 64
N_HEADS = ATT_W // HEAD_DIM
MOBA_BLOCK = 256
MOBA_TOPK = 3
Q_CHUNK = 64
SSM_W = D_MODEL // 2
SSM_GROUP = 16
SSM_GROUPS = SSM_W // SSM_GROUP
SSM_STATE = 64
PEER_HEADS = 8
PEER_NKEYS = 128
PEER_EXPERTS = PEER_NKEYS * PEER_NKEYS
PEER_DKEY = 256
PEER_TOPK = 16
TOK_CHUNK = 128
PROJ_W = 3 * ATT_W + SSM_W + 2 * D_MODEL
DN_ALPHA = (2.0 * DEPTH) ** 0.25
DN_BETA = (8.0 * DEPTH) ** -0.25
LN_EPS = 1e-5

kernel_name = 'moba_s5_peer_hybrid_step'


def layer_norm(x, g, b):
    xf = x.astype(jnp.float32)
    mu = jnp.mean(xf, axis=-1, keepdims=True)
    var = jnp.mean(jnp.square(xf - mu), axis=-1, keepdims=True)
    y = (xf - mu) * lax.rsqrt(var + LN_EPS) * g.astype(jnp.float32) + b.astype(jnp.float32)
    return y.astype(x.dtype)


def alibi_slopes():
    return jnp.exp2(-8.0 * (jnp.arange(N_HEADS, dtype=jnp.float32) + 1.0) / N_HEADS)


def moba_sequence(q, k, v, q_pos, chunk, slopes):
    n_blocks = k.shape[1] // MOBA_BLOCK
    kb = k.reshape(N_HEADS, n_blocks, MOBA_BLOCK, HEAD_DIM)
    vb = v.reshape(N_HEADS, n_blocks, MOBA_BLOCK, HEAD_DIM)
    kmean = jnp.mean(kb.astype(jnp.float32), axis=2)
    h_ix = jnp.arange(N_HEADS)[:, None, None]
    blk_ids = jnp.arange(n_blocks, dtype=jnp.int32)
    offs = jnp.arange(MOBA_BLOCK, dtype=jnp.int32)
    scale = HEAD_DIM ** -0.5

    def attend(args):
        qc, pc = args
        c = pc.shape[0]
        cur = pc // MOBA_BLOCK
        gate = jnp.einsum('hcd,hnd->hcn', qc.astype(jnp.float32), kmean)
        fully_past = blk_ids[None, None, :] < cur[None, :, None]
        gate = jnp.where(fully_past, gate, -jnp.inf)
        _, top = lax.top_k(gate, MOBA_TOPK)
        own = jnp.broadcast_to(cur[None, :, None], (N_HEADS, c, 1)).astype(top.dtype)
        blocks = jnp.concatenate([top, own], axis=-1)
        valid = jnp.concatenate([top < cur[None, :, None], jnp.ones((N_HEADS, c, 1), bool)], axis=-1)
        kg = kb[h_ix, blocks]
        vg = vb[h_ix, blocks]
        kpos = blocks[..., None] * MOBA_BLOCK + offs
        dist = (pc[None, :, None, None] - kpos).astype(jnp.float32)
        s = jnp.einsum('hcd,hcsbd->hcsb', qc, kg).astype(jnp.float32) * scale
        s = s - slopes[:, None, None, None] * dist
        mask = valid[..., None] & (dist >= 0)
        s = jnp.where(mask, s, -jnp.inf)
        p = jax.nn.softmax(s.reshape(N_HEADS, c, -1), axis=-1).reshape(s.shape)
        return jnp.einsum('hcsb,hcsbd->hcd', p.astype(vg.dtype), vg)

    t = q.shape[1]
    nq = t // chunk
    qs = q.reshape(N_HEADS, nq, chunk, HEAD_DIM).transpose(1, 0, 2, 3)
    ps = q_pos.reshape(nq, chunk)
    outs = lax.map(attend, (qs, ps))
    return outs.transpose(1, 0, 2, 3).reshape(N_HEADS, t, HEAD_DIM)


def moba_attention(q, k_all, v_all, start):
    bn, t, _ = q.shape
    l = k_all.shape[1]
    n_blocks = max(-(-l // MOBA_BLOCK), MOBA_TOPK)
    pad = n_blocks * MOBA_BLOCK - l
    qh = q.reshape(bn, t, N_HEADS, HEAD_DIM).transpose(0, 2, 1, 3)
    kh = jnp.pad(k_all, ((0, 0), (0, pad), (0, 0), (0, 0))).transpose(0, 2, 1, 3)
    vh = jnp.pad(v_all, ((0, 0), (0, pad), (0, 0), (0, 0))).transpose(0, 2, 1, 3)
    q_pos = start + jnp.arange(t, dtype=jnp.int32)
    chunk = Q_CHUNK if t % Q_CHUNK == 0 else t
    slopes = alibi_slopes()
    out = lax.map(lambda a: moba_sequence(a[0], a[1], a[2], q_pos, chunk, slopes), (qh, kh, vh))
    return out.transpose(0, 2, 1, 3).reshape(bn, t, ATT_W)


def _ssm_combine(e1, e2):
    a1r, a1i, b1r, b1i = e1
    a2r, a2i, b2r, b2i = e2
    return (a2r * a1r - a2i * a1i,
            a2r * a1i + a2i * a1r,
            a2r * b1r - a2i * b1i + b2r,
            a2r * b1i + a2i * b1r + b2i)


def s5_branch(u, h0_re, h0_im, a_re, a_im, log_dt, b_re, b_im, c_re, c_im, d_skip, w_glu, b_glu):
    bn, t, _ = u.shape
    f32 = jnp.float32
    uf = u.astype(f32).reshape(bn, t, SSM_GROUPS, SSM_GROUP)
    dt = jnp.exp(log_dt.astype(f32))[:, None]
    ar, ai = a_re.astype(f32), a_im.astype(f32)
    mag = jnp.exp(dt * ar)
    ang = dt * ai
    abar_re, abar_im = mag * jnp.cos(ang), mag * jnp.sin(ang)
    den = ar * ar + ai * ai
    nr, ni = abar_re - 1.0, abar_im
    f_re = (nr * ar + ni * ai) / den
    f_im = (ni * ar - nr * ai) / den
    br, bi = b_re.astype(f32), b_im.astype(f32)
    bbar_re = f_re[:, :, None] * br - f_im[:, :, None] * bi
    bbar_im = f_re[:, :, None] * bi + f_im[:, :, None] * br
    bu_re = jnp.einsum('btgc,gpc->btgp', uf, bbar_re)
    bu_im = jnp.einsum('btgc,gpc->btgp', uf, bbar_im)
    h0r, h0i = h0_re.astype(f32), h0_im.astype(f32)
    bu_re = bu_re.at[:, 0].add(abar_re * h0r - abar_im * h0i)
    bu_im = bu_im.at[:, 0].add(abar_re * h0i + abar_im * h0r)
    a_r_t = jnp.broadcast_to(abar_re, bu_re.shape)
    a_i_t = jnp.broadcast_to(abar_im, bu_re.shape)
    _, _, h_re, h_im = lax.associative_scan(_ssm_combine, (a_r_t, a_i_t, bu_re, bu_im), axis=1)
    y = (jnp.einsum('btgp,gcp->btgc', h_re, c_re.astype(f32))
         - jnp.einsum('btgp,gcp->btgc', h_im, c_im.astype(f32))
         + d_skip.astype(f32).reshape(SSM_GROUPS, SSM_GROUP) * uf)
    z = jax.nn.gelu(y.reshape(bn, t, SSM_W))
    out = z * jax.nn.sigmoid(z @ w_glu.astype(f32) + b_glu.astype(f32))
    return out.astype(u.dtype), h_re[:, -1].astype(h0_re.dtype), h_im[:, -1].astype(h0_re.dtype)


def peer_ffn(x, w_pq, sub_k1, sub_k2, peer_u, peer_v):
    shape = x.shape
    xf = x.reshape(-1, D_MODEL)
    n = xf.shape[0]
    n_pad = -(-n // TOK_CHUNK) * TOK_CHUNK
    xp = jnp.pad(xf, ((0, n_pad - n), (0, 0))).reshape(n_pad // TOK_CHUNK, TOK_CHUNK, D_MODEL)
    half = PEER_DKEY // 2

    def chunk_fn(xc):
        q = (xc @ w_pq).astype(jnp.float32).reshape(TOK_CHUNK, PEER_HEADS, PEER_DKEY)
        s1 = jnp.einsum('nhk,hmk->nhm', q[..., :half], sub_k1.astype(jnp.float32))
        s2 = jnp.einsum('nhk,hmk->nhm', q[..., half:], sub_k2.astype(jnp.float32))
        v1, i1 = lax.top_k(s1, PEER_TOPK)
        v2, i2 = lax.top_k(s2, PEER_TOPK)
        cand = (v1[..., :, None] + v2[..., None, :]).reshape(TOK_CHUNK, PEER_HEADS, PEER_TOPK * PEER_TOPK)
        sc, ci = lax.top_k(cand, PEER_TOPK)
        e = (jnp.take_along_axis(i1, ci // PEER_TOPK, axis=-1) * PEER_NKEYS
             + jnp.take_along_axis(i2, ci % PEER_TOPK, axis=-1))
        g = jax.nn.softmax(sc, axis=-1)
        ue = peer_u[e]
        a = jax.nn.gelu(jnp.einsum('nd,nhkd->nhk', xc, ue).astype(jnp.float32))
        return jnp.einsum('nhk,nhkd->nd', (g * a).astype(x.dtype), peer_v[e])

    y = lax.map(chunk_fn, xp).reshape(n_pad, D_MODEL)[:n]
    return y.reshape(shape)


def hybrid_layer(x, past_k, past_v, h0_re, h0_im, start,
                 w_in, b_in, w_a, w_b, w_o, ln1_g, ln1_b,
                 a_re, a_im, log_dt, b_re, b_im, c_re, c_im, d_skip, w_glu, b_glu,
                 ln2_g, ln2_b, w_pq, sub_k1, sub_k2, peer_u, peer_v):
    bn, t, _ = x.shape
    proj = x @ w_in + b_in
    q, k, v, u, ga, gb = jnp.split(
        proj, [ATT_W, 2 * ATT_W, 3 * ATT_W, 3 * ATT_W + SSM_W, 3 * ATT_W + SSM_W + D_MODEL], axis=-1)
    k_rows = k.reshape(bn, t, N_HEADS, HEAD_DIM)
    v_rows = v.reshape(bn, t, N_HEADS, HEAD_DIM)
    if past_k is None:
        k_all, v_all = k_rows, v_rows
    else:
        k_all = jnp.concatenate([past_k.astype(k_rows.dtype), k_rows], axis=1)
        v_all = jnp.concatenate([past_v.astype(v_rows.dtype), v_rows], axis=1)
    y_att = moba_attention(q, k_all, v_all, start)
    y_ssm, h_re, h_im = s5_branch(u, h0_re, h0_im, a_re, a_im, log_dt, b_re, b_im,
                                  c_re, c_im, d_skip, w_glu, b_glu)
    merged = jax.nn.sigmoid(ga) * (y_att @ w_a) + jax.nn.sigmoid(gb) * (y_ssm @ w_b)
    x1 = layer_norm(DN_ALPHA * x + merged @ w_o, ln1_g, ln1_b)
    x2 = layer_norm(DN_ALPHA * x1 + peer_ffn(x1, w_pq, sub_k1, sub_k2, peer_u, peer_v), ln2_g, ln2_b)
    return x2, k_rows, v_rows, h_re, h_im


def setup_inputs(seed: int = 0) -> dict:
    key = jax.random.key(seed)
    ks = jax.random.split(key, 32)
    nrm = lambda i, shape, s: jax.random.normal(ks[i], shape, jnp.float32) * s
    n_pages = PAST_LEN // PAGE_SIZE
    n_pool = (DEC_BATCH * n_pages * 5 + 3) // 4
    page_table = jax.random.permutation(ks[0], n_pool)[:DEC_BATCH * n_pages].reshape(
        DEC_BATCH, n_pages).astype(jnp.int32)
    a_im0 = math.pi * jnp.arange(SSM_STATE, dtype=jnp.float32)[None, :]
    return {
        'x_prompt': nrm(1, (BATCH, SEQ, D_MODEL), 1.0),
        'x_sample': nrm(2, (DEC_BATCH, DEC_SEQ, D_MODEL), 1.0),
        'cache_k': nrm(3, (n_pool, PAGE_SIZE, N_HEADS, HEAD_DIM), 1.0),
        'cache_v': nrm(4, (n_pool, PAGE_SIZE, N_HEADS, HEAD_DIM), 1.0),
        'state_ssm_re': nrm(5, (DEC_BATCH, SSM_GROUPS, SSM_STATE), 0.5),
        'state_ssm_im': nrm(6, (DEC_BATCH, SSM_GROUPS, SSM_STATE), 0.5),
        'page_table': page_table,
        'w_in': nrm(7, (D_MODEL, PROJ_W), D_MODEL ** -0.5),
        'b_in': nrm(8, (PROJ_W,), 0.01),
        'w_a': nrm(9, (ATT_W, D_MODEL), DN_BETA * ATT_W ** -0.5),
        'w_b': nrm(10, (SSM_W, D_MODEL), DN_BETA * SSM_W ** -0.5),
        'w_o': nrm(11, (D_MODEL, D_MODEL), DN_BETA * D_MODEL ** -0.5),
        'ln1_g': 1.0 + nrm(12, (D_MODEL,), 0.01),
        'ln1_b': nrm(13, (D_MODEL,), 0.01),
        'a_re': -0.5 + nrm(14, (SSM_GROUPS, SSM_STATE), 0.01),
        'a_im': a_im0 + nrm(15, (SSM_GROUPS, SSM_STATE), 0.01),
        'log_dt': jax.random.uniform(ks[16], (SSM_GROUPS,), jnp.float32, math.log(1e-3), math.log(1e-1)),
        'b_re': nrm(17, (SSM_GROUPS, SSM_STATE, SSM_GROUP), (2.0 * SSM_GROUP) ** -0.5),
        'b_im': nrm(18, (SSM_GROUPS, SSM_STATE, SSM_GROUP), (2.0 * SSM_GROUP) ** -0.5),
        'c_re': nrm(19, (SSM_GROUPS, SSM_GROUP, SSM_STATE), (2.0 * SSM_STATE) ** -0.5),
        'c_im': nrm(20, (SSM_GROUPS, SSM_GROUP, SSM_STATE), (2.0 * SSM_STATE) ** -0.5),
        'd_skip': nrm(21, (SSM_W,), 1.0),
        'w_glu': nrm(22, (SSM_W, SSM_W), SSM_W ** -0.5),
        'b_glu': nrm(23, (SSM_W,), 0.01),
        'ln2_g': 1.0 + nrm(24, (D_MODEL,), 0.01),
        'ln2_b': nrm(25, (D_MODEL,), 0.01),
        'w_pq': nrm(26, (D_MODEL, PEER_HEADS * PEER_DKEY), D_MODEL ** -0.5),
        'sub_k1': nrm(27, (PEER_HEADS, PEER_NKEYS, PEER_DKEY // 2), (PEER_DKEY // 2) ** -0.5),
        'sub_k2': nrm(28, (PEER_HEADS, PEER_NKEYS, PEER_DKEY // 2), (PEER_DKEY // 2) ** -0.5),
        'peer_u': nrm(29, (PEER_EXPERTS, D_MODEL), D_MODEL ** -0.5),
        'peer_v': nrm(30, (PEER_EXPERTS, D_MODEL), DN_BETA * PEER_HEADS ** -0.5),
    }


def reference(x_prompt, x_sample, cache_k, cache_v, state_ssm_re, state_ssm_im, page_table,
              w_in, b_in, w_a, w_b, w_o, ln1_g, ln1_b,
              a_re, a_im, log_dt, b_re, b_im, c_re, c_im, d_skip, w_glu, b_glu,
              ln2_g, ln2_b, w_pq, sub_k1, sub_k2, peer_u, peer_v):
    weights = (w_in, b_in, w_a, w_b, w_o, ln1_g, ln1_b,
               a_re, a_im, log_dt, b_re, b_im, c_re, c_im, d_skip, w_glu, b_glu,
               ln2_g, ln2_b, w_pq, sub_k1, sub_k2, peer_u, peer_v)
    h0 = jnp.zeros((x_prompt.shape[0], SSM_GROUPS, SSM_STATE), x_prompt.dtype)
    y_prompt = x_prompt
    for _ in range(DEPTH):
        y_prompt, k_prompt, v_prompt, ssm_re_prompt, ssm_im_prompt = hybrid_layer(
            y_prompt, None, None, h0, h0, 0, *weights)
    db, n_pages = page_table.shape
    past_k = cache_k[page_table].reshape(db, n_pages * PAGE_SIZE, N_HEADS, HEAD_DIM)
    past_v = cache_v[page_table].reshape(db, n_pages * PAGE_SIZE, N_HEADS, HEAD_DIM)
    y_sample = x_sample
    for _ in range(DEPTH):
        y_sample, k_sample, v_sample, ssm_re_sample, ssm_im_sample = hybrid_layer(
            y_sample, past_k, past_v, state_ssm_re, state_ssm_im, n_pages * PAGE_SIZE, *weights)
    return (y_prompt, y_sample, k_prompt, v_prompt, k_sample, v_sample,
            ssm_re_prompt, ssm_im_prompt, ssm_re_sample, ssm_im_sample)
```

```python
from contextlib import ExitStack
import numpy as np
import concourse.bass as bass
import concourse.mybir as mybir
from concourse.bass_utils import run_bass_kernel_spmd

F32 = mybir.dt.float32
BF16 = mybir.dt.bfloat16
I32 = mybir.dt.int32
U32 = mybir.dt.uint32
AF = mybir.ActivationFunctionType
ALU = mybir.AluOpType
AX = mybir.AxisListType

NCORES = 8
D = 1024
SEQ = 2048
NT_P = 16
NT = 17
PROJ = 4096
SAME_ENGINE_SYNC = True


class Sched:
    EPOCH = 12000
    DEPOCH = 700

    def __init__(self, nc, stack):
        self.nc = nc
        self.stack = stack
        self.names = ['sp', 'act', 'dve', 'pool', 'pe']
        self.prog = {e: [] for e in self.names}
        self.cnt = {e: 0 for e in self.names}
        self.esems = {e: [] for e in self.names}
        self.dsems = {}
        self.dcnt = {}
        self.seen = {e: {} for e in self.names}
        self.last_w = {}
        self.readers = {}
        self.nsem = 0

    def _newsem(self, name):
        self.nsem += 1
        return self.stack.enter_context(self.nc.semaphore(name))

    def _esem(self, eng, n):
        ep = (n - 1) // self.EPOCH
        while len(self.esems[eng]) <= ep:
            self.esems[eng].append(self._newsem("e_%s_%d" % (eng, len(self.esems[eng]))))
        return self.esems[eng][ep], (n - 1) % self.EPOCH + 1

    def _dsem(self, key, n):
        ep = (n - 1) // self.DEPOCH
        lst = self.dsems.setdefault(key, [])
        while len(lst) <= ep:
            lst.append(self._newsem("d%d" % self.nsem))
        return lst[ep], ((n - 1) % self.DEPOCH + 1) * 16

    def _wait_for(self, consumer, tok, waits):
        kind, key, n = tok
        if kind == 'e':
            if key == consumer and not SAME_ENGINE_SYNC:
                return
            if key == consumer and consumer in ('pe', 'sp'):
                return
            if self.seen[consumer].get(('e', key), 0) >= n:
                return
            self.seen[consumer][('e', key)] = n
            waits.append(self._esem(key, n))
        else:
            n = self.dcnt[key]
            if self.seen[consumer].get(('d', key), 0) >= n:
                return
            self.seen[consumer][('d', key)] = n
            waits.append(self._dsem(key, n))

    def _deps(self, eng, reads, writes):
        waits = []
        for r in reads:
            t = self.last_w.get(r)
            if t is not None:
                self._wait_for(eng, t, waits)
        for w in writes:
            t = self.last_w.get(w)
            if t is not None:
                self._wait_for(eng, t, waits)
            for t in self.readers.get(w, ()):
                self._wait_for(eng, t, waits)
        return waits

    def _commit(self, tok, reads, writes):
        for r in reads:
            self.readers.setdefault(r, []).append(tok)
        for w in writes:
            self.last_w[w] = tok
            self.readers[w] = []

    def op(self, eng, fn, reads=(), writes=(), dma_key=None):
        waits = self._deps(eng, reads, writes)
        if dma_key is not None:
            self.dcnt[dma_key] = self.dcnt.get(dma_key, 0) + 1
            n = self.dcnt[dma_key]
            sem, _ = self._dsem(dma_key, n)
            self.prog[eng].append((waits, fn, (sem, None), 16))
            self._commit(('d', dma_key, n), reads, writes)
            return
        self.cnt[eng] += 1
        n = self.cnt[eng]
        tok = ('e', eng, n)
        self.prog[eng].append((waits, fn, self._esem(eng, n), 1))
        self._commit(tok, reads, writes)

    def dma(self, out, in_, reads=(), writes=(), key=None, eng='sp', **kw):
        if key is None:
            key = writes[0] if (writes and not str(writes[0]).startswith('dram')) else reads[0]
        waits = self._deps(eng, reads, writes)
        self.dcnt[key] = self.dcnt.get(key, 0) + 1
        n = self.dcnt[key]
        tok = ('d', key, n)
        sem, _ = self._dsem(key, n)
        self.prog[eng].append((waits, (lambda e, o=out, i=in_, k=kw: e.dma_start(out=o, in_=i, **k)), (sem, None), 16))
        self._commit(tok, reads, writes)

    def finish(self, eng='sp'):
        waits = []
        for key in list(self.dcnt):
            self._wait_for(eng, ('d', key, self.dcnt[key]), waits)
        self.prog[eng].append((waits, None, None, 0))

    def barrier(self):
        for e in self.names:
            waits = []
            for o in self.names:
                if o != e and self.cnt[o] > 0:
                    self._wait_for(e, ('e', o, self.cnt[o]), waits)
            for key in list(self.dcnt):
                self._wait_for(e, ('d', key, self.dcnt[key]), waits)
            self.prog[e].append((waits, None, None, 0))

    def replay(self, block):
        def mk(name):
            def run(e):
                for waits, fn, semv, inc in self.prog[name]:
                    for s, v in waits:
                        e.wait_ge(s, v)
                    if fn is None:
                        continue
                    ins = fn(e)
                    ins.then_inc(semv[0], inc)
            return run
        block.sync(mk('sp'))
        block.scalar(mk('act'))
        block.vector(mk('dve'))
        block.gpsimd(mk('pool'))
        block.tensor(mk('pe'))


import math
TWO_PI = 2.0 * math.pi


DN_ALPHA = 2.0 ** 0.25
LN_EPS = 1e-5


def layer_norm(S, src, dst, stats, mv, g_bc, b_bc, src_res, dst_res, gb_res):
    for c in range(2):
        S.op('dve', lambda e, c=c: e.bn_stats(out=stats[:, c, :], in_=src[:, c * 512:(c + 1) * 512]), reads=src_res, writes=[("lnstats", c)])
    S.op('dve', lambda e: e.bn_aggr(out=mv[:, 0:2], in_=stats[:]), reads=[("lnstats", 0), ("lnstats", 1)], writes=["lnmv"])
    S.op('dve', lambda e: e.tensor_scalar(out=mv[:, 2:3], in0=mv[:, 1:2], scalar1=LN_EPS, scalar2=None, op0=ALU.add), reads=["lnmv"], writes=["lnmv2"])
    S.op('act', lambda e: e.activation(out=mv[:, 2:3], in_=mv[:, 2:3], func=AF.Sqrt), reads=["lnmv2"], writes=["lnmv2"])
    S.op('dve', lambda e: e.reciprocal(out=mv[:, 3:4], in_=mv[:, 2:3]), reads=["lnmv2"], writes=["lnmv3"])
    S.op('dve', lambda e: e.tensor_scalar(out=dst[:], in0=src[:], scalar1=mv[:, 0:1], scalar2=mv[:, 3:4], op0=ALU.subtract, op1=ALU.mult),
         reads=src_res + ["lnmv", "lnmv3"], writes=[dst_res])
    S.op('dve', lambda e: e.tensor_tensor(out=dst[:], in0=dst[:], in1=g_bc[:], op=ALU.mult), reads=[dst_res] + gb_res, writes=[dst_res])
    S.op('dve', lambda e: e.tensor_tensor(out=dst[:], in0=dst[:], in1=b_bc[:], op=ALU.add), reads=[dst_res] + gb_res, writes=[dst_res])


def build(nc, debug=False, glimit=None, stop_after=3, p2tiles=NT):
    st = ExitStack()
    S = Sched(nc, st)

    def din(name, shape, dt=F32):
        return nc.dram_tensor(name, list(shape), dt, kind="ExternalInput").ap()

    def dout(name, shape, dt=F32):
        return nc.dram_tensor(name, list(shape), dt, kind="ExternalOutput").ap()

    def mk_sb(stack):
        return lambda name, shape, dt=F32: stack.enter_context(nc.sbuf_tensor(name, list(shape), dt))

    def mk_ps(stack):
        return lambda name, shape, dt=F32: stack.enter_context(nc.psum_tensor(name, list(shape), dt))

    sb = mk_sb(st)

    x_tm = din("x_tm", [NT, 128, D])
    x_fm = din("x_fm", [NT, 128, 8, 128])
    w_in = din("w_in", [D, PROJ])
    b_in_bc = din("b_in_bc", [128, PROJ])
    b_in_fm = din("b_in_fm", [128, 32])
    are_tm = din("are_tm", [128, 32, 64]); aim_tm = din("aim_tm", [128, 32, 64]); ldt_tm = din("ldt_tm", [128, 32])
    are_fm = din("are_fm", [128, 32]); aim_fm = din("aim_fm", [128, 32])
    are_bd = din("are_bd", [128, 4, 64]); aim_bd = din("aim_bd", [128, 4, 64]); ldt_bd = din("ldt_bd", [128, 4])
    bre_bd = din("bre_bd", [128, 4, 64]); bim_bd = din("bim_bd", [128, 4, 64]); maskbd = din("maskbd", [128, 4])
    c_fm = din("c_fm", [128, 32, 16]); d_fm = din("d_fm", [128, 4]); bglu_fm = din("bglu_fm", [128, 4])
    w_glu = din("w_glu", [512, 512])
    h0_fm = din("h0_fm", [128, 32, 16]); h0sw_fm = din("h0sw_fm", [128, 32, 16])
    cst = din("cst", [128, 5, 128])
    k_out = dout("k_out", [NT, 128, 512])
    v_out = dout("v_out", [NT, 128, 512])
    ssm_p = dout("ssm_p", [32, 128])
    ssm_s = dout("ssm_s", [16, 32, 128])
    if debug:
        dbg_y = dout("dbg_y", [128, 4, NT * 128], BF16)

    bfm = sb("bfm", [128, 32])
    cst_sb = sb("cst_sb", [128, 5, 128])
    ident = cst_sb[:, 0, :]
    psw = cst_sb[:, 1, :]
    sgn = cst_sb[:, 4, 0:1]
    s12 = ExitStack()
    yssmT = s12.enter_context(nc.sbuf_tensor("yssmT", [128, 4, NT * 128], BF16))
    qT32s = s12.enter_context(nc.sbuf_tensor("qT32s", [128, 4, 128], F32))
    qTbs = s12.enter_context(nc.sbuf_tensor("qTbs", [128, 4, 128], BF16))
    kTs_new = s12.enter_context(nc.sbuf_tensor("kTs_new", [128, 4, 128], BF16))
    Vs_new = s12.enter_context(nc.sbuf_tensor("Vs_new", [128, 8, 65], BF16))
    sgas = s12.enter_context(nc.sbuf_tensor("sgas", [128, 8, 128], BF16))
    sgbs = s12.enter_context(nc.sbuf_tensor("sgbs", [128, 8, 128], BF16))

    S.dma(bfm[:], b_in_fm, writes=["bfm"])
    S.dma(cst_sb[:], cst, writes=["cst"])

    w_in_r = w_in.rearrange("(k p) n -> p k n", p=128)

    with ExitStack() as s1:
        sb1 = mk_sb(s1)
        ps1 = mk_ps(s1)
        EmR = sb1("EmR", [128, 32, 64]); EmI = sb1("EmI", [128, 32, 64])
        EpR = sb1("EpR", [128, 32, 128]); EpI = sb1("EpI", [128, 32, 128])
        BD = sb1("BD", [128, 4, 4, 128], BF16)
        Cmat = sb1("Cmat", [128, 32, 16], BF16)
        Dd = sb1("Dd", [128, 4, 128], BF16)
        wglu_bf = sb1("wglu_bf", [128, 4, 512], BF16)
        bglu = sb1("bglu", [128, 4])
        tri_bf = sb1("tri_bf", [128, 2, 128], BF16)
        h0f = sb1("h0f", [128, 32, 16]); h0sw = sb1("h0sw", [128, 32, 16])
        S.dma(h0f[:], h0_fm, writes=["h0f"]); S.dma(h0sw[:], h0sw_fm, writes=["h0sw"])
        w_u = sb1("w_u", [128, 8, 512], BF16)
        S.dma(bglu[:], bglu_fm, writes=["bglu"])
        S.op('dve', lambda e: e.tensor_copy(out=tri_bf[:], in_=cst_sb[:, 2:4, :]), reads=["cst"], writes=["tri_bf"])

        with ExitStack() as sp:
            sbp = mk_sb(sp)
            T = [sbp("ptmp%d" % i, [128, 1024]) for i in range(9)]
            Ti = sbp("ptmpi", [128, 1024], I32)
            small = sbp("psmall", [128, 8, 32])
            jp1 = sbp("jp1", [128, 2])
            ip1 = sbp("ip1", [128, 128])

            def rs(name):
                return ("prep", name)
            PR = [rs("x")]

            def P(eng, fn):
                S.op(eng, fn, reads=PR + ["cst"], writes=PR)

            def PD(out, in_):
                S.dma(out, in_, reads=PR, writes=PR, key="prep_dma")

            def sincos(theta, n, out_sin, out_cos):
                for (off, dst) in ((math.pi + TWO_PI, out_sin), (1.5 * math.pi + TWO_PI, out_cos)):
                    a = T[7][:, 0:n]; kf = T[8][:, 0:n]; ki = Ti[:, 0:n]
                    P('dve', lambda e, a=a, off=off: e.tensor_scalar(out=a, in0=theta, scalar1=off, scalar2=None, op0=ALU.add))
                    P('dve', lambda e, a=a, ki=ki: e.tensor_scalar(out=ki, in0=a, scalar1=1.0 / TWO_PI, scalar2=None, op0=ALU.mult))
                    P('dve', lambda e, kf=kf, ki=ki: e.tensor_copy(out=kf, in_=ki))
                    P('dve', lambda e, a=a, kf=kf: e.scalar_tensor_tensor(out=a, in0=kf, scalar=-TWO_PI, in1=a, op0=ALU.mult, op1=ALU.add))
                    P('dve', lambda e, a=a, kf=kf: e.tensor_scalar(out=kf, in0=a, scalar1=TWO_PI, scalar2=-TWO_PI, op0=ALU.is_ge, op1=ALU.mult))
                    P('dve', lambda e, a=a, kf=kf: e.tensor_tensor(out=a, in0=a, in1=kf, op=ALU.add))
                    P('dve', lambda e, a=a, kf=kf: e.tensor_scalar(out=kf, in0=a, scalar1=0.0, scalar2=TWO_PI, op0=ALU.is_lt, op1=ALU.mult))
                    P('dve', lambda e, a=a, kf=kf: e.tensor_tensor(out=a, in0=a, in1=kf, op=ALU.add))
                    P('dve', lambda e, a=a: e.tensor_scalar(out=a, in0=a, scalar1=-math.pi, scalar2=None, op0=ALU.add))
                    P('act', lambda e, a=a, dst=dst: e.activation(out=dst, in_=a, func=AF.Sin))

            S.op('pool', lambda e: e.iota(jp1[:, 0:1], pattern=[[0, 1]], base=1, channel_multiplier=1, allow_small_or_imprecise_dtypes=True), writes=PR)
            S.op('pool', lambda e: e.iota(ip1[:], pattern=[[1, 128]], base=1, channel_multiplier=0, allow_small_or_imprecise_dtypes=True), reads=PR, writes=PR)
            P('dve', lambda e: e.tensor_scalar(out=jp1[:, 1:2], in0=jp1[:, 0:1], scalar1=-1.0, scalar2=None, op0=ALU.mult))
            ldt = small[:, 0, :]; dtt = small[:, 1, :]; arf = small[:, 2, :]; aif = small[:, 3, :]
            PD(ldt, ldt_tm)
            PD(arf, are_fm)
            PD(aif, aim_fm)
            tq = small[:, 4, :]
            P('dve', lambda e: e.tensor_scalar(out=tq, in0=ldt, scalar1=0.125, scalar2=None, op0=ALU.mult))
            P('dve', lambda e: e.tensor_scalar(out=dtt, in0=tq, scalar1=1.0 / 12.0, scalar2=1.0, op0=ALU.mult, op1=ALU.add))
            for n in range(11, 0, -1):
                P('dve', lambda e: e.tensor_tensor(out=dtt, in0=dtt, in1=tq, op=ALU.mult))
                P('dve', lambda e, n=n: e.tensor_scalar(out=dtt, in0=dtt, scalar1=1.0 / n, scalar2=1.0, op0=ALU.mult, op1=ALU.add))
            for _ in range(3):
                P('dve', lambda e: e.tensor_tensor(out=dtt, in0=dtt, in1=dtt, op=ALU.mult))
            P('dve', lambda e: e.tensor_tensor(out=arf, in0=arf, in1=dtt, op=ALU.mult))
            P('dve', lambda e: e.tensor_tensor(out=aif, in0=aif, in1=dtt, op=ALU.mult))

            for gc in range(4):
                gs = slice(8 * gc, 8 * gc + 8)
                v3 = lambda ap: ap.rearrange("p (g q) -> p g q", g=8)
                ar = T[0][:, 0:512]; ai = T[1][:, 0:512]; mg = T[2][:, 0:512]; sn = T[3][:, 0:512]; cs = T[4][:, 0:512]
                t4 = T[5][:, 0:512]; th = T[5][:, 512:1024]
                PD(v3(ar), are_tm[:, gs, :])
                PD(v3(ai), aim_tm[:, gs, :])
                dtb = dtt[:, gs].unsqueeze(2).to_broadcast([128, 8, 64])
                P('dve', lambda e, ar=ar, dtb=dtb: e.tensor_tensor(out=v3(ar), in0=v3(ar), in1=dtb, op=ALU.mult))
                P('dve', lambda e, ai=ai, dtb=dtb: e.tensor_tensor(out=v3(ai), in0=v3(ai), in1=dtb, op=ALU.mult))
                P('act', lambda e, mg=mg, ar=ar: e.activation(out=mg, in_=ar, func=AF.Exp, scale=jp1[:, 1:2]))
                P('dve', lambda e, th=th, ai=ai: e.tensor_scalar(out=th, in0=ai, scalar1=jp1[:, 0:1], scalar2=None, op0=ALU.mult))
                sincos(th, 512, sn, cs)
                P('dve', lambda e, gs=gs, cs=cs, mg=mg: e.tensor_tensor(out=EmR[:, gs, :], in0=v3(cs), in1=v3(mg), op=ALU.mult))
                P('dve', lambda e, gs=gs, sn=sn, mg=mg: e.scalar_tensor_tensor(out=EmI[:, gs, :], in0=v3(sn), scalar=-1.0, in1=v3(mg), op0=ALU.mult, op1=ALU.mult))
                v3f = lambda ap: ap.rearrange("p (g i) -> p g i", g=8)
                ipb = ip1[:].unsqueeze(1).to_broadcast([128, 8, 128])
                P('dve', lambda e, gs=gs, ipb=ipb: e.tensor_tensor(out=v3f(T[0][:]), in0=arf[:, gs].unsqueeze(2).to_broadcast([128, 8, 128]), in1=ipb, op=ALU.mult))
                P('dve', lambda e, gs=gs, ipb=ipb: e.tensor_tensor(out=v3f(T[1][:]), in0=aif[:, gs].unsqueeze(2).to_broadcast([128, 8, 128]), in1=ipb, op=ALU.mult))
                P('act', lambda e: e.activation(out=T[0][:], in_=T[0][:], func=AF.Exp))
                sincos(T[1][:], 1024, T[2][:], T[3][:])
                P('dve', lambda e, gs=gs: e.tensor_tensor(out=EpR[:, gs, :], in0=v3f(T[3][:]), in1=v3f(T[0][:]), op=ALU.mult))
                P('dve', lambda e, gs=gs: e.scalar_tensor_tensor(out=EpI[:, gs, :], in0=v3f(T[2][:]), scalar=sgn, in1=v3f(T[0][:]), op0=ALU.mult, op1=ALU.mult))

            q3 = lambda t, o=0: t[:, o:o + 256].rearrange("p (q r) -> p q r", q=4)
            ab = q3(T[0]); ai_ = q3(T[0], 256); br = q3(T[0], 512); bi = q3(T[0], 768)
            ldb = small[:, 4, 0:4]; dtbd = small[:, 5, 0:4]; mk = small[:, 6, 0:4]; dsk = small[:, 7, 0:4]
            PD(ab, are_bd)
            PD(ai_, aim_bd)
            PD(ldb, ldt_bd)
            PD(br, bre_bd)
            PD(bi, bim_bd)
            PD(mk, maskbd)
            PD(dsk, d_fm)
            P('act', lambda e: e.activation(out=dtbd, in_=ldb, func=AF.Exp))
            dtq = dtbd.unsqueeze(2).to_broadcast([128, 4, 64])
            dar = q3(T[1]); dai = q3(T[1], 256); mg = q3(T[1], 512)
            P('dve', lambda e: e.tensor_tensor(out=dar, in0=ab, in1=dtq, op=ALU.mult))
            P('dve', lambda e: e.tensor_tensor(out=dai, in0=ai_, in1=dtq, op=ALU.mult))
            P('act', lambda e: e.activation(out=mg, in_=dar, func=AF.Exp))
            sincos(T[1][:, 256:512], 256, T[2][:, 0:256], T[2][:, 256:512])
            ni = q3(T[2], 0); nr = q3(T[2], 256)
            P('dve', lambda e: e.tensor_tensor(out=nr, in0=nr, in1=mg, op=ALU.mult))
            P('dve', lambda e: e.tensor_scalar(out=nr, in0=nr, scalar1=-1.0, scalar2=None, op0=ALU.add))
            P('dve', lambda e: e.tensor_tensor(out=ni, in0=ni, in1=mg, op=ALU.mult))
            den = q3(T[2], 512); t1 = q3(T[2], 768); t2 = q3(T[3], 0); fr = q3(T[3], 256); fi = q3(T[3], 512)
            P('dve', lambda e: e.tensor_tensor(out=den, in0=ab, in1=ab, op=ALU.mult))
            P('dve', lambda e: e.tensor_tensor(out=t1, in0=ai_, in1=ai_, op=ALU.mult))
            P('dve', lambda e: e.tensor_tensor(out=den, in0=den, in1=t1, op=ALU.add))
            P('dve', lambda e: e.reciprocal(out=den, in_=den))
            P('dve', lambda e: e.tensor_tensor(out=t1, in0=nr, in1=ab, op=ALU.mult))
            P('dve', lambda e: e.tensor_tensor(out=t2, in0=ni, in1=ai_, op=ALU.mult))
            P('dve', lambda e: e.tensor_tensor(out=t1, in0=t1, in1=t2, op=ALU.add))
            P('dve', lambda e: e.tensor_tensor(out=fr, in0=t1, in1=den, op=ALU.mult))
            P('dve', lambda e: e.tensor_tensor(out=t1, in0=ni, in1=ab, op=ALU.mult))
            P('dve', lambda e: e.tensor_tensor(out=t2, in0=nr, in1=ai_, op=ALU.mult))
            P('dve', lambda e: e.tensor_tensor(out=t1, in0=t1, in1=t2, op=ALU.subtract))
            P('dve', lambda e: e.tensor_tensor(out=fi, in0=t1, in1=den, op=ALU.mult))
            bfull = T[4][:, 0:512].rearrange("p (q h r) -> p q h r", q=4, h=2)
            P('dve', lambda e: e.tensor_tensor(out=t1, in0=fr, in1=br, op=ALU.mult))
            P('dve', lambda e: e.tensor_tensor(out=t2, in0=fi, in1=bi, op=ALU.mult))
            P('dve', lambda e: e.tensor_tensor(out=bfull[:, :, 0, :], in0=t1, in1=t2, op=ALU.subtract))
            P('dve', lambda e: e.tensor_tensor(out=t1, in0=fr, in1=bi, op=ALU.mult))
            P('dve', lambda e: e.tensor_tensor(out=t2, in0=fi, in1=br, op=ALU.mult))
            P('dve', lambda e: e.tensor_tensor(out=bfull[:, :, 1, :], in0=t1, in1=t2, op=ALU.add))
            bfl = T[4][:, 0:512].rearrange("p (q r) -> p q r", q=4)
            for gl4 in range(4):
                P('dve', lambda e, gl4=gl4: e.tensor_scalar(out=BD[:, :, gl4, :], in0=bfl, scalar1=mk[:, gl4:gl4 + 1], scalar2=None, op0=ALU.mult))
            cst32 = T[5][:, 0:512].rearrange("p (g c) -> p g c", g=32)
            PD(cst32, c_fm)
            P('dve', lambda e: e.tensor_copy(out=Cmat[0:64], in_=cst32[0:64]))
            P('dve', lambda e: e.tensor_scalar(out=Cmat[64:128], in0=cst32[64:128], scalar1=-1.0, scalar2=None, op0=ALU.mult))
            for q in range(4):
                P('dve', lambda e, q=q: e.tensor_scalar(out=Dd[:, q, :], in0=ident, scalar1=dsk[:, q:q + 1], scalar2=None, op0=ALU.mult))
            w_glu_r = w_glu.rearrange("(k p) n -> p k n", p=128)
            for half in range(2):
                wg32 = T[6][:, 0:1024].rearrange("p (k n) -> p k n", k=2)
                PD(wg32, w_glu_r[:, 2 * half:2 * half + 2, :])
                P('dve', lambda e, half=half, wg32=wg32: e.tensor_copy(out=wglu_bf[:, 2 * half:2 * half + 2, :], in_=wg32))
            for kh in range(4):
                wu32 = T[kh % 2][:, 0:1024].rearrange("p (k n) -> p k n", k=2)
                PD(wu32, w_in_r[:, 2 * kh:2 * kh + 2, 1536:2048])
                P('pool', lambda e, kh=kh, wu32=wu32: e.tensor_copy(out=w_u[:, 2 * kh:2 * kh + 2, :], in_=wu32))
            S.op('dve', lambda e: e.engine_nop(), reads=PR, writes=["s5tab"])
        S.barrier()
        if debug:
            dbg_em = dout("dbg_em", [2, 128, 32, 64]); dbg_ep = dout("dbg_ep", [2, 128, 32, 128])
            S.dma(dbg_em[0], EmR[:], reads=["s5tab"], writes=["dram_dbg1"], key="dbgk")
            S.dma(dbg_em[1], EmI[:], reads=["s5tab"], writes=["dram_dbg2"], key="dbgk")
            S.dma(dbg_ep[0], EpR[:], reads=["s5tab"], writes=["dram_dbg3"], key="dbgk")
            S.dma(dbg_ep[1], EpI[:], reads=["s5tab"], writes=["dram_dbg4"], key="dbgk")

        xT32 = [sb1("xT32_%d" % i, [128, 8, 128], F32) for i in range(2)]
        xT = [sb1("xT_%d" % i, [128, 8, 128], BF16) for i in range(2)]
        uT = [sb1("uT_%d" % i, [128, 4, 128], BF16) for i in range(2)]
        Xp = sb1("Xp", [128, 32, 192], BF16)
        zh = sb1("zh", [128, 4, 16, 8]); zh2 = sb1("zh2", [128, 4, 16, 8]); tmpz = sb1("tmpz", [128, 4, 128])
        tA = [sb1("tA%d" % i, [128, 2, 2, 64]) for i in range(2)]
        tB = [sb1("tB%d" % i, [128, 2, 2, 64]) for i in range(2)]
        T1 = [sb1("T1_%d" % i, [128, 4, 128]) for i in range(2)]
        T2 = [sb1("T2_%d" % i, [128, 4, 128]) for i in range(2)]
        ZT = sb1("ZT", [128, 32, 128], BF16)
        hc1 = [sb1("hc1_%d" % i, [128, 32]) for i in range(2)]
        hc2 = [sb1("hc2_%d" % i, [128, 32]) for i in range(2)]
        hs = sb1("hs", [128, 32, 16])
        hsT = sb1("hsT", [16, 2, 4, 128])
        hpT = sb1("hpT", [32, 128])
        z_sb = sb1("z_sb", [128, 512])
        zT_bf = sb1("zT_bf", [128, 4, 128], BF16)
        sg = sb1("sg", [128, 4, 128])
        pA = ps1("pA", [128, 4, 128])
        pXl = [ps1("pX%d" % i, [128, 256]) for i in range(2)]
        pW1 = ps1("pW1", [128, 4, 128]); pW2 = ps1("pW2", [128, 4, 128])
        py = ps1("py", [128, 512])
        pzT = ps1("pzT", [128, 4, 128])
        phc = ps1("phc", [128, 512])

        S.op('pool', lambda e: e.memset(hc1[0][:], 0.0), writes=[("hc1", 0)])
        S.op('pool', lambda e: e.memset(hc2[0][:], 0.0), writes=[("hc2", 0)])

        for t in range(NT):
            b = t % 2
            sample = (t == NT - 1)
            hb = 0 if (sample or t == 0) else (t % 2)
            if sample:
                S.op('pool', lambda e: e.memset(hc1[0][:], 0.0), writes=[("hc1", 0)])
                S.op('pool', lambda e: e.memset(hc2[0][:], 0.0), writes=[("hc2", 0)])
            hn = 1 - hb
            S.dma(xT32[b][:], x_fm[t], writes=[("xT32", b)])
            S.op('pool', lambda e, b=b: e.tensor_copy(out=xT[b][:], in_=xT32[b][:]),
                 reads=[("xT32", b)], writes=[("xT", b)])
            for q in range(4):
                for kk in range(8):
                    S.op('pe', lambda e, b=b, q=q, kk=kk: e.matmul(
                        pA[:, q, :], lhsT=w_u[:, kk, 128 * q:128 * (q + 1)], rhs=xT[b][:, kk, :],
                        start=(kk == 0), stop=(kk == 7)),
                        reads=[("xT", b), "s5tab"], writes=["pA"])
                S.op('act', lambda e, b=b, q=q: e.activation(out=uT[b][:, q, :], in_=pA[:, q, :], func=AF.Identity,
                                                             bias=bfm[:, 12 + q:13 + q]),
                     reads=["pA", "bfm"], writes=[("uT", b, q)])
            for hcx in range(16):
                g0 = 2 * hcx
                q = g0 // 8
                base = 64 * ((g0 % 8) // 4)
                gl4 = g0 % 4
                xb = hcx % 2
                S.op('pe', lambda e, b=b, q=q, base=base, gl4=gl4, xb=xb: e.matmul(
                    pXl[xb][:], lhsT=uT[b][base:base + 64, q, :],
                    rhs=BD[base:base + 64, q, gl4:gl4 + 2, :].rearrange("p a r -> p (a r)"), start=True, stop=True),
                    reads=[("uT", b, q), "s5tab"], writes=[("pX", xb)])
                pxv = pXl[xb][:].rearrange("p (g h r) -> p g h r", g=2, h=2)
                src = pxv
                srcres = ("pX", xb)
                emr = EmR[:, g0:g0 + 2, :].unsqueeze(2).to_broadcast([128, 2, 2, 64])
                emi = EmI[:, g0:g0 + 2, :].unsqueeze(2).to_broadcast([128, 2, 2, 64])
                S.op('dve', lambda e, xb=xb, src=src, emr=emr: e.tensor_tensor(out=tA[xb][:], in0=src, in1=emr, op=ALU.mult),
                     reads=[srcres, "s5tab"], writes=[("tA", xb)])
                S.op('dve', lambda e, xb=xb, src=src, emi=emi: e.tensor_tensor(out=tB[xb][:], in0=src, in1=emi, op=ALU.mult),
                     reads=[srcres, "s5tab"], writes=[("tB", xb)])
                S.op('dve', lambda e, xb=xb, g0=g0: e.tensor_tensor(out=Xp[:, g0:g0 + 2, 0:64], in0=tA[xb][:, :, 0, :], in1=tB[xb][:, :, 1, :], op=ALU.subtract),
                     reads=[("tA", xb), ("tB", xb)], writes=[("Xp", g0, 0)])
                S.op('dve', lambda e, xb=xb, g0=g0: e.tensor_tensor(out=Xp[:, g0:g0 + 2, 64:128], in0=tA[xb][:, :, 1, :], in1=tB[xb][:, :, 0, :], op=ALU.add),
                     reads=[("tA", xb), ("tB", xb)], writes=[("Xp", g0, 1)])
                S.op('pool', lambda e, g0=g0: e.tensor_copy(out=Xp[:, g0:g0 + 2, 128:192], in_=Xp[:, g0:g0 + 2, 0:64]),
                     reads=[("Xp", g0, 0)], writes=[("Xp", g0, 2)])
            tri = tri_bf[:, 1 if sample else 0, :]
            for gq in range(8):
                tb = gq % 2
                for gl in range(4):
                    g = 4 * gq + gl
                    g0 = (g // 2) * 2
                    S.op('pe', lambda e, g=g, gl=gl, tri=tri: e.matmul(pW1[:, gl, :], lhsT=Xp[:, g, 0:128], rhs=tri, start=True, stop=True),
                         reads=[("Xp", g0, 0), ("Xp", g0, 1), "tri_bf"], writes=["pW1"])
                    S.op('pe', lambda e, g=g, gl=gl, tri=tri: e.matmul(pW2[:, gl, :], lhsT=Xp[:, g, 64:192], rhs=tri, start=True, stop=True),
                         reads=[("Xp", g0, 1), ("Xp", g0, 2), "tri_bf"], writes=["pW2"])
                for gl in range(4):
                    g = 4 * gq + gl
                    S.op('dve', lambda e, g=g, gl=gl, tb=tb, hb=hb: e.scalar_tensor_tensor(
                        out=T1[tb][:, gl, :], in0=pW1[:, gl, :], scalar=hc1[hb][:, g:g + 1], in1=EpR[:, g, :], op0=ALU.add, op1=ALU.mult),
                        reads=["pW1", ("hc1", hb), "s5tab"], writes=[("T1", tb, gl)])
                    S.op('dve', lambda e, g=g, gl=gl, tb=tb, hb=hb: e.scalar_tensor_tensor(
                        out=T2[tb][:, gl, :], in0=pW2[:, gl, :], scalar=hc2[hb][:, g:g + 1], in1=EpI[:, g, :], op0=ALU.add, op1=ALU.mult),
                        reads=["pW2", ("hc2", hb), "s5tab"], writes=[("T2", tb, gl)])
                rT = [("T1", tb, gl) for gl in range(4)] + [("T2", tb, gl) for gl in range(4)]
                if sample:
                    gsl = slice(4 * gq, 4 * gq + 4)
                    S.op('dve', lambda e, gsl=gsl: e.tensor_tensor(out=zh[:], in0=EpR[:, gsl, 0:8].unsqueeze(2).to_broadcast([128, 4, 16, 8]),
                                                                   in1=h0f[:, gsl, :].unsqueeze(3).to_broadcast([128, 4, 16, 8]), op=ALU.mult),
                         reads=["s5tab", "h0f"], writes=["zh"])
                    S.op('dve', lambda e, gsl=gsl: e.tensor_tensor(out=zh2[:], in0=EpI[:, gsl, 0:8].unsqueeze(2).to_broadcast([128, 4, 16, 8]),
                                                                   in1=h0sw[:, gsl, :].unsqueeze(3).to_broadcast([128, 4, 16, 8]), op=ALU.mult),
                         reads=["s5tab", "h0sw"], writes=["zh2"])
                    S.op('pool', lambda e, tb=tb: e.tensor_tensor(out=tmpz[:], in0=T1[tb][:], in1=T2[tb][:], op=ALU.add), reads=rT, writes=["tmpz"])
                    S.op('pool', lambda e: e.tensor_tensor(out=tmpz[:], in0=tmpz[:], in1=zh[:].rearrange("p g a b -> p g (a b)"), op=ALU.add), reads=["tmpz", "zh"], writes=["tmpz"])
                    S.op('pool', lambda e: e.tensor_tensor(out=tmpz[:], in0=tmpz[:], in1=zh2[:].rearrange("p g a b -> p g (a b)"), op=ALU.add), reads=["tmpz", "zh2"], writes=["tmpz"])
                    S.op('pool', lambda e, gq=gq: e.tensor_copy(out=ZT[:, 4 * gq:4 * gq + 4, :], in_=tmpz[:]), reads=["tmpz"], writes=[("ZT", gq)])
                    S.op('pool', lambda e, gq=gq: e.tensor_copy(out=hs[:, 4 * gq:4 * gq + 4, :], in_=tmpz[:, :, 7::8]), reads=["tmpz"], writes=[("hs", gq)])
                else:
                    S.op('pool', lambda e, gq=gq, tb=tb: e.tensor_tensor(out=ZT[:, 4 * gq:4 * gq + 4, :], in0=T1[tb][:], in1=T2[tb][:], op=ALU.add),
                         reads=rT, writes=[("ZT", gq)])
                    S.op('pool', lambda e, gq=gq, tb=tb, hn=hn: e.tensor_tensor(out=hc1[hn][:, 4 * gq:4 * gq + 4], in0=T1[tb][:, :, 127], in1=T2[tb][:, :, 127], op=ALU.add),
                         reads=rT, writes=[("hc1", hn, gq)])
            if not sample:
                S.op('pe', lambda e, hn=hn: e.matmul(phc[:, 0:32], lhsT=psw, rhs=hc1[hn][:], start=True, stop=True),
                     reads=[("hc1", hn, gq) for gq in range(8)] + ["cst"], writes=["phc"])
                S.op('dve', lambda e, hn=hn: e.tensor_copy(out=hc2[hn][:], in_=phc[:, 0:32]), reads=["phc"], writes=[("hc2", hn)])
                S.op('dve', lambda e, hn=hn: e.engine_nop(), reads=[("hc1", hn, gq) for gq in range(8)], writes=[("hc1", hn)])
            if debug and t == 0:
                dbg_xp = dout("dbg_xp", [128, 32, 192], BF16); dbg_zt = dout("dbg_zt", [128, 32, 128], BF16)
                S.dma(dbg_xp, Xp[:], reads=[("Xp", g0, k) for g0 in range(0, 32, 2) for k in range(3)], writes=["dram_dbg5"], key="dbgk")
                S.dma(dbg_zt, ZT[:], reads=[("ZT", gq) for gq in range(8)], writes=["dram_dbg6"], key="dbgk")
            for q in range(4):
                S.op('pe', lambda e, b=b, q=q: e.matmul(py[:, q * 128:(q + 1) * 128], lhsT=uT[b][:, q, :], rhs=Dd[:, q, :], start=(q == 0), stop=False),
                     reads=[("uT", b, q), "s5tab"], writes=["py"])
            for g in range(32):
                S.op('pe', lambda e, g=g: e.matmul(py[:, g * 16:(g + 1) * 16], lhsT=ZT[:, g, :], rhs=Cmat[:, g, :], start=False, stop=(g == 31)),
                     reads=[("ZT", g // 4), "s5tab"], writes=["py"])
            S.op('act', lambda e: e.activation(out=z_sb[:], in_=py[:], func=AF.Gelu_apprx_tanh), reads=["py"], writes=["z_sb"])
            for q in range(4):
                S.op('pe', lambda e, q=q: e.transpose(pzT[:, q, :], z_sb[:, q * 128:(q + 1) * 128], ident), reads=["z_sb", "cst"], writes=["pzT"])
            S.op('act', lambda e: e.activation(out=zT_bf[:], in_=pzT[:], func=AF.Copy), reads=["pzT"], writes=["zT_bf"])
            for fc in range(4):
                for kc in range(4):
                    S.op('pe', lambda e, fc=fc, kc=kc: e.matmul(pA[:, fc, :], lhsT=wglu_bf[:, kc, fc * 128:(fc + 1) * 128], rhs=zT_bf[:, kc, :],
                                                               start=(kc == 0), stop=(kc == 3)),
                         reads=["zT_bf", "s5tab"], writes=["pA"])
                S.op('act', lambda e, fc=fc: e.activation(out=sg[:, fc, :], in_=pA[:, fc, :], func=AF.Sigmoid, bias=bglu[:, fc:fc + 1]),
                     reads=["pA", "bglu"], writes=[("sg", fc)])
            S.op('dve', lambda e, t=t: e.tensor_tensor(out=yssmT[:, :, t * 128:(t + 1) * 128], in0=pzT[:], in1=sg[:], op=ALU.mult),
                 reads=["pzT"] + [("sg", fc) for fc in range(4)], writes=[("yssmT", t)])
            if t == NT_P - 1:
                S.op('pe', lambda e, hn=hn: e.transpose(phc[0:32, 128:256], hc1[hn][:], ident), reads=[("hc1", hn), "cst"], writes=["phc"])
                S.op('dve', lambda e: e.tensor_copy(out=hpT[:], in_=phc[0:32, 128:256]), reads=["phc"], writes=["hpT"])
                S.dma(ssm_p, hpT[:], reads=["hpT"], writes=["dram_ssm_p"])
            if sample:
                for gq in range(8):
                    for gl in range(4):
                        g = 4 * gq + gl
                        S.op('pe', lambda e, g=g, gl=gl: e.transpose(phc[0:16, gl * 128:(gl + 1) * 128], hs[:, g, :], ident),
                             reads=[("hs", gq), "cst"], writes=["phc"])
                    S.op('dve', lambda e, gq=gq: e.tensor_copy(out=hsT[:, gq % 2, :, :], in_=phc[0:16, :].rearrange("p (g r) -> p g r", g=4)),
                         reads=["phc"], writes=[("hsT", gq % 2)])
                    S.dma(ssm_s[:, 4 * gq:4 * gq + 4, :], hsT[:, gq % 2, :, :], reads=[("hsT", gq % 2)], writes=[("dram_ssm_s", gq)])
        if debug:
            S.dma(dbg_y, yssmT[:], reads=[("yssmT", t) for t in range(NT)], writes=["dram_dbg_y"])


    S.barrier()
    if stop_after < 2:
        S.finish('sp')
        with nc.Block() as block:
            S.replay(block)
        s12.close()
        st.close()
        return nc
    x1_d = nc.dram_tensor("x1_scratch", [NT, 128, D], F32, kind="Internal").ap() if not debug else dout("x1_scratch", [NT, 128, D])
    w_a = din("w_a", [512, D]); w_b = din("w_b", [512, D]); w_o = din("w_o", [D, D])
    ln1g_bc = din("ln1g_bc", [128, D]); ln1b_bc = din("ln1b_bc", [128, D])
    acst = din("acst", [128, 8 + 128 + 128])
    ltab = din("ltab", [9, 9, 128])
    BIGM = 30000.0
    with ExitStack() as s2:
        sb2 = mk_sb(s2)
        ps2 = mk_ps(s2)
        wq = sb2("wq", [128, 8, 3584], BF16)
        kT_all = sb2("kT_all", [128, 4, SEQ], BF16)
        V_all = sb2("V_all", [128, NT_P, 8, 65], BF16)
        wa_bf = sb2("wa_bf", [128, 4, D], BF16); wb_bf = sb2("wb_bf", [128, 4, D], BF16); wo_bf = sb2("wo_bf", [128, 8, D], BF16)
        lng = sb2("lng", [128, D]); lnb = sb2("lnb", [128, D])
        bkv = sb2("bkv", [128, 1024])
        acs = sb2("acs", [128, 264])
        slq = acs[:, 0:8]; bexp = acs[:, 8:136].rearrange("p (h d) -> p h d", h=8)
        ltab_bf = sb2("ltab_bf", [9, 9, 128], BF16)
        causal_bf = sb2("causal_bf", [128, 128], BF16)
        ident_bf = sb2("ident_bf", [128, 128], BF16)
        ksum = sb2("ksum", [128, NT_P, 4])
        kmT = sb2("kmT", [128, 4, 8])
        kmTz = sb2("kmTz", [128, 8, 8])
        Mq = sb2("Mq", [128, 8, 9])
        with ExitStack() as sl:
            sbl = mk_sb(sl)
            stg = [sbl("stg%d" % i, [128, 8, 256]) for i in range(2)]
            lt32 = sbl("lt32", [9, 9, 128])
            ns = [0]

            def load_cast(dst_ap_fn, src_ap, nk, ncol, res):
                b = ns[0] % 2
                ns[0] += 1
                S.dma(stg[b][:, 0:nk, 0:ncol], src_ap, writes=[("stg", b)])
                S.op('pool', lambda e, b=b: e.tensor_copy(out=dst_ap_fn(), in_=stg[b][:, 0:nk, 0:ncol]), reads=[("stg", b)], writes=[res])
            for c in range(14):
                src_c = c * 256 if c < 6 else 2048 + (c - 6) * 256
                load_cast(lambda c=c: wq[:, :, c * 256:(c + 1) * 256], w_in_r[:, :, src_c:src_c + 256], 8, 256, "wq")
            war = w_a.rearrange("(k p) n -> p k n", p=128); wbr = w_b.rearrange("(k p) n -> p k n", p=128)
            wor = w_o.rearrange("(k p) n -> p k n", p=128)
            for c in range(4):
                load_cast(lambda c=c: wa_bf[:, :, c * 256:(c + 1) * 256], war[:, :, c * 256:(c + 1) * 256], 4, 256, "wa")
                load_cast(lambda c=c: wb_bf[:, :, c * 256:(c + 1) * 256], wbr[:, :, c * 256:(c + 1) * 256], 4, 256, "wb")
                load_cast(lambda c=c: wo_bf[:, :, c * 256:(c + 1) * 256], wor[:, :, c * 256:(c + 1) * 256], 8, 256, "wo")
            S.dma(lng[:], ln1g_bc, writes=["lng"]); S.dma(lnb[:], ln1b_bc, writes=["lnb"])
            S.dma(bkv[:], b_in_bc[:, 512:1536], writes=["bkv"])
            S.dma(acs[:], acst, writes=["acs"])
            S.dma(lt32[:], ltab, writes=["lt32"])
            S.op('dve', lambda e: e.tensor_copy(out=ltab_bf[:], in_=lt32[:]), reads=["lt32"], writes=["ltab_bf"])
            S.op('dve', lambda e: e.tensor_copy(out=causal_bf[:], in_=acs[:, 136:264]), reads=["acs"], writes=["causal_bf"])
            S.op('dve', lambda e: e.tensor_copy(out=ident_bf[:], in_=ident), reads=["cst"], writes=["ident_bf"])
            S.op('pool', lambda e: e.memset(V_all[:, :, :, 64:65], 1.0), writes=["V_ones"])
            S.op('pool', lambda e: e.memset(kmT[:], 0.0), writes=["kmT"])
            S.op('pool', lambda e: e.memset(kmTz[:], 0.0), writes=["kmTz"])
            S.op('pool', lambda e: e.memset(Mq[:, :, 0:8], 0.0), writes=["Mq"])
            S.op('dve', lambda e: e.tensor_copy(out=Mq[:, :, 8], in_=slq), reads=["acs", "Mq"], writes=["Mq"])
        S.barrier()

        xU32 = [sb2("p2xT32_0", [128, 8, 128], F32)] * 2
        xU = [sb2("p2xT_0", [128, 8, 128], BF16)] * 2
        xtm = [sb2("xtm0", [128, D])] * 2
        qT32 = sb2("qT32", [128, 4, 128]); qTb = sb2("qTb", [128, 4, 128], BF16)
        kv_sb = sb2("kv_sb", [128, 1024])
        sga = sb2("sga", [128, 8, 128], BF16); sgb = sb2("sgb", [128, 8, 128], BF16)
        gm = sb2("gm", [128, 8, 8]); mx = sb2("mx", [128, 8, 8])
        R_bf = sb2("R_bf", [9, 8, 128], BF16)
        PT = [sb2("PT%d" % i, [128, 4, 128], BF16) for i in range(2)]
        rsum = sb2("rsum", [128, 8])
        yatt = sb2("yatt", [128, 8, 64])
        yattT = sb2("yattT", [128, 4, 128], BF16)
        mtmp = sb2("mtmp", [128, 4, 128])
        mergedT = sb2("mergedT", [128, 8, 128], BF16)
        pre = sb2("pre", [128, D]); x1 = pre
        stats = sb2("stats", [128, 2, 6]); mv = sb2("mv", [128, 4])
        pFA = ps2("pFA", [128, 4, 128]); pFB = ps2("pFB", [128, 4, 128])
        pT0 = ps2("pT0", [128, 512]); pT1 = ps2("pT1", [128, 512])
        pS = [ps2("pS%d" % i, [128, 4, 128]) for i in range(2)]
        pO = [ps2("pO%d" % i, [128, 4, 65]) for i in range(2)]
        sctr = [0]

        for t in range(p2tiles):
            b = t % 2
            sample = (t == NT - 1)
            cur = t // 2
            S.dma(xU32[b][:], x_fm[t], writes=[("p2xT32", 0)])
            S.dma(xtm[b][:], x_tm[t], writes=[("xtm", 0)])
            S.op('pool', lambda e, b=b: e.tensor_copy(out=xU[b][:], in_=xU32[b][:]), reads=[("p2xT32", 0)], writes=[("p2xT", 0)])
            for (pb, pbn, col0, which) in ((pFA, "pFA", 0, 'q'), (pFB, "pFB", 512, 'k')):
                for q in range(4):
                    for kk in range(8):
                        S.op('pe', lambda e, b=b, q=q, kk=kk, pb=pb, col0=col0: e.matmul(
                            pb[:, q, :], lhsT=wq[:, kk, col0 + 128 * q:col0 + 128 * (q + 1)], rhs=xU[b][:, kk, :],
                            start=(kk == 0), stop=(kk == 7)), reads=[("p2xT", 0), "wq"], writes=[pbn])
                for q in range(4):
                    if which == 'q':
                        qdst = qT32s if sample else qT32
                        S.op('act', lambda e, q=q, qdst=qdst: e.activation(out=qdst[:, q, :], in_=pFA[:, q, :], func=AF.Identity, bias=bfm[:, q:q + 1]),
                             reads=["pFA", "bfm"], writes=["qT32"])
                    elif sample:
                        S.op('act', lambda e, q=q: e.activation(out=kTs_new[:, q, :], in_=pFB[:, q, :], func=AF.Identity, bias=bfm[:, 4 + q:5 + q]),
                             reads=["pFB", "bfm"], writes=["kTs_new"])
                    else:
                        S.op('act', lambda e, q=q, t=t: e.activation(out=kT_all[:, q, t * 128:(t + 1) * 128], in_=pFB[:, q, :], func=AF.Identity,
                                                                   bias=bfm[:, 4 + q:5 + q], accum_out=ksum[:, t, q:q + 1]),
                             reads=["pFB", "bfm"], writes=[("kT", t), ("ksum", t)])
                if which == 'q':
                    if sample:
                        S.op('pool', lambda e: e.tensor_copy(out=qTbs[:], in_=qT32s[:]), reads=["qT32"], writes=["qTb"])
                    else:
                        S.op('pool', lambda e: e.tensor_copy(out=qTb[:], in_=qT32[:]), reads=["qT32"], writes=["qTb"])
            if (not sample) and t % 2 == 1:
                n = t // 2
                S.op('dve', lambda e, t=t, n=n: e.tensor_tensor(out=kmT[:, :, n], in0=ksum[:, t - 1, :], in1=ksum[:, t, :], op=ALU.add),
                     reads=[("ksum", t - 1), ("ksum", t), "kmT"], writes=["kmT"])
                S.op('dve', lambda e, n=n: e.tensor_scalar(out=kmT[:, :, n], in0=kmT[:, :, n], scalar1=1.0 / 256.0, scalar2=None, op0=ALU.mult),
                     reads=["kmT"], writes=["kmT"])
                kz = kmTz[:].rearrange("p (c two) n -> p c two n", two=2)
                S.op('dve', lambda e, n=n, kz=kz: e.tensor_copy(out=kz[0:64, :, 0, n], in_=kmT[0:64, :, n]), reads=["kmT", "kmTz"], writes=["kmTz"])
                S.op('dve', lambda e, n=n, kz=kz: e.tensor_copy(out=kz[64:128, :, 1, n], in_=kmT[64:128, :, n]), reads=["kmT", "kmTz"], writes=["kmTz"])
            for j, (pt, ptn) in enumerate(((pT0, "pT0"), (pT1, "pT1"))):
                c0 = 512 + 512 * j
                for kk in range(8):
                    S.op('pe', lambda e, b=b, kk=kk, pt=pt, c0=c0: e.matmul(pt[:], lhsT=xU[b][:, kk, :], rhs=wq[:, kk, c0:c0 + 512],
                                                                       start=(kk == 0), stop=(kk == 7)),
                         reads=[("p2xT", 0), "wq"], writes=[ptn])
                S.op('dve', lambda e, j=j, pt=pt: e.tensor_tensor(out=kv_sb[:, j * 512:(j + 1) * 512], in0=pt[:], in1=bkv[:, j * 512:(j + 1) * 512], op=ALU.add),
                     reads=[ptn, "bkv"], writes=[("kv_sb", j)])
            S.dma(k_out[t], kv_sb[:, 0:512], reads=[("kv_sb", 0)], writes=[("dram_k", t)], key="kv_sb")
            S.dma(v_out[t], kv_sb[:, 512:1024], reads=[("kv_sb", 1)], writes=[("dram_v", t)], key="kv_sb")
            if not sample:
                S.op('pool', lambda e, t=t: e.tensor_copy(out=V_all[:, t, :, 0:64], in_=kv_sb[:, 512:1024].rearrange("p (h d) -> p h d", h=8)),
                     reads=[("kv_sb", 1)], writes=[("V", t)])
            else:
                S.op('pool', lambda e: e.memset(Vs_new[:, :, 64:65], 1.0), writes=["Vs_new1"])
                S.op('pool', lambda e: e.tensor_copy(out=Vs_new[:, :, 0:64], in_=kv_sb[:, 512:1024].rearrange("p (h d) -> p h d", h=8)),
                     reads=[("kv_sb", 1)], writes=["Vs_new"])
            for gi, (dst, dn, col0, bcol) in enumerate((((sgas if sample else sga), "sga", 1536, 16), ((sgbs if sample else sgb), "sgb", 2560, 24))):
                for half in range(2):
                    pb, pbn = ((pFA, "pFA"), (pFB, "pFB"))[half]
                    for q in range(4):
                        fc = 4 * half + q
                        for kk in range(8):
                            S.op('pe', lambda e, b=b, q=q, kk=kk, pb=pb, col0=col0, fc=fc: e.matmul(
                                pb[:, q, :], lhsT=wq[:, kk, col0 + 128 * fc:col0 + 128 * (fc + 1)], rhs=xU[b][:, kk, :],
                                start=(kk == 0), stop=(kk == 7)), reads=[("p2xT", 0), "wq"], writes=[pbn])
                    for q in range(4):
                        fc = 4 * half + q
                        S.op('act', lambda e, q=q, fc=fc, pb=pb, dst=dst, bcol=bcol: e.activation(
                            out=dst[:, fc, :], in_=pb[:, q, :], func=AF.Sigmoid, bias=bfm[:, bcol + fc:bcol + fc + 1]),
                            reads=[pbn, "bfm"], writes=[(dn, fc)])
            if sample:
                continue
            if not sample:
                if cur >= 1:
                    for h in range(8):
                        hp, hcx = h % 2, h // 2
                        S.op('pe', lambda e, h=h, hp=hp, hcx=hcx: e.matmul(
                            pT0[:, h * 8:(h + 1) * 8], lhsT=qT32[:, hcx, :], rhs=kmTz[:, h, :],
                            start=(h == 0), stop=(h == 7)), reads=["qT32", "kmTz"], writes=["pT0"])
                    S.op('pool', lambda e: e.memset(gm[:], -1.0e30), writes=["gm"])
                    S.op('dve', lambda e, cur=cur: e.tensor_copy(out=gm[:, :, 0:cur], in_=pT0[:, 0:64].rearrange("p (h n) -> p h n", h=8)[:, :, 0:cur]),
                         reads=["pT0", "gm"], writes=["gm"])
                    for h in range(8):
                        S.op('dve', lambda e, h=h: e.max(out=mx[:, h, :], in_=gm[:, h, :]), reads=["gm"], writes=[("mx", h)])
                        S.op('dve', lambda e, h=h: e.tensor_scalar(out=Mq[:, h, 0:8], in0=gm[:, h, :], scalar1=mx[:, h, 2:3], scalar2=-1.0,
                                                                   op0=ALU.is_ge, op1=ALU.add),
                             reads=["gm", ("mx", h), "Mq"], writes=["Mq"])
                for h in range(8):
                    tgt, tn = (pT1, "pT1") if h < 4 else (pFA, "pFA")
                    tv = tgt[0:9, :].rearrange("p (h r) -> p h r", h=4) if h < 4 else tgt[0:9, :, :]
                    S.op('pe', lambda e, h=h, tv=tv: e.transpose(tv[:, h % 4, :], Mq[:, h, :], ident), reads=["Mq", "cst"], writes=[tn])
                S.op('act', lambda e: e.activation(out=R_bf[:, 0:4, :], in_=pT1[0:9, :].rearrange("p (h r) -> p h r", h=4), func=AF.Copy, scale=BIGM),
                     reads=["pT1"], writes=["R_bf0"])
                S.op('act', lambda e: e.activation(out=R_bf[:, 4:8, :], in_=pFA[0:9, :, :], func=AF.Copy, scale=BIGM),
                     reads=["pFA"], writes=["R_bf1"])
                for h in range(8):
                    hp, hcx = h % 2, h // 2
                    ob, obn = pO[h // 4], "pO%d" % (h // 4)
                    kt = 0
                    while kt <= t:
                        nsl = min(4, t + 1 - kt)
                        bk = sctr[0] % 2
                        sctr[0] += 1
                        for sl_ in range(nsl):
                            k2 = kt + sl_
                            n = k2 // 2
                            var = n if n < cur else 8
                            S.op('pe', lambda e, hp=hp, hcx=hcx, k2=k2, bk=bk, sl_=sl_: e.matmul(
                                pS[bk][:, sl_, :], lhsT=kT_all[hp * 64:(hp + 1) * 64, hcx, k2 * 128:(k2 + 1) * 128],
                                rhs=qTb[hp * 64:(hp + 1) * 64, hcx, :], start=True, stop=False),
                                reads=[("kT", k2), "qTb"], writes=[("pS", bk)])
                            S.op('pe', lambda e, h=h, var=var, bk=bk, sl_=sl_, last=(k2 != t): e.matmul(
                                pS[bk][:, sl_, :], lhsT=ltab_bf[0:9, var, :], rhs=R_bf[0:9, h, :], start=False, stop=last),
                                reads=["ltab_bf", "R_bf%d" % (h // 4)], writes=[("pS", bk)])
                            if k2 == t:
                                S.op('pe', lambda e, bk=bk, sl_=sl_: e.matmul(pS[bk][:, sl_, :], lhsT=ident_bf[:], rhs=causal_bf[:], start=False, stop=True),
                                     reads=["ident_bf", "causal_bf"], writes=[("pS", bk)])
                        for sl_ in range(nsl):
                            k2 = kt + sl_
                            S.op('act', lambda e, h=h, bk=bk, sl_=sl_, dl=t - k2: e.activation(
                                out=PT[bk][:, sl_, :], in_=pS[bk][:, sl_, :], func=AF.Exp, scale=0.125, bias=bexp[:, h, dl:dl + 1]),
                                reads=[("pS", bk), "acs"], writes=[("PT", bk)])
                        for sl_ in range(nsl):
                            k2 = kt + sl_
                            S.op('pe', lambda e, h=h, bk=bk, sl_=sl_, k2=k2, ob=ob, t=t: e.matmul(
                                ob[:, h % 4, :], lhsT=PT[bk][:, sl_, :], rhs=V_all[:, k2, h, :], start=(k2 == 0), stop=(k2 == t)),
                                reads=[("PT", bk), ("V", k2), "V_ones"], writes=[obn])
                        kt += nsl
                for k in range(2):
                    S.op('dve', lambda e, k=k: e.reciprocal(out=rsum[:, 4 * k:4 * k + 4], in_=pO[k][:, :, 64]), reads=["pO%d" % k], writes=[("rsum", k)])
                    S.op('dve', lambda e, k=k: e.tensor_tensor(out=yatt[:, 4 * k:4 * k + 4, :], in0=pO[k][:, :, 0:64],
                                                               in1=rsum[:, 4 * k:4 * k + 4].unsqueeze(2).to_broadcast([128, 4, 64]), op=ALU.mult),
                         reads=["pO%d" % k, ("rsum", k)], writes=[("yatt", k)])
            else:
                S.op('pool', lambda e: e.memset(yatt[:], 0.0), writes=[("yatt", 0), ("yatt", 1)])
            yv = yatt[:].rearrange("p h d -> p (h d)")
            for q in range(4):
                S.op('pe', lambda e, q=q, yv=yv: e.transpose(pFA[:, q, :], yv[:, q * 128:(q + 1) * 128], ident),
                     reads=[("yatt", 0), ("yatt", 1), "cst"], writes=["pFA"])
            S.op('act', lambda e: e.activation(out=yattT[:], in_=pFA[:], func=AF.Copy), reads=["pFA"], writes=["yattT"])
            for half in range(2):
                for q in range(4):
                    fc = 4 * half + q
                    for kc in range(4):
                        S.op('pe', lambda e, q=q, fc=fc, kc=kc: e.matmul(pFA[:, q, :], lhsT=wa_bf[:, kc, fc * 128:(fc + 1) * 128], rhs=yattT[:, kc, :],
                                                                      start=(kc == 0), stop=(kc == 3)), reads=["wa", "yattT"], writes=["pFA"])
                for q in range(4):
                    fc = 4 * half + q
                    for kc in range(4):
                        S.op('pe', lambda e, q=q, fc=fc, kc=kc, t=t: e.matmul(pFB[:, q, :], lhsT=wb_bf[:, kc, fc * 128:(fc + 1) * 128],
                                                                           rhs=yssmT[:, kc, t * 128:(t + 1) * 128],
                                                                           start=(kc == 0), stop=(kc == 3)), reads=["wb", ("yssmT", t)], writes=["pFB"])
                hs_ = slice(4 * half, 4 * half + 4)
                S.op('dve', lambda e, hs_=hs_: e.tensor_tensor(out=mtmp[:], in0=pFA[:], in1=sga[:, hs_, :], op=ALU.mult),
                     reads=["pFA"] + [("sga", fc) for fc in range(8)], writes=["mtmp"])
                S.op('dve', lambda e, hs_=hs_: e.tensor_tensor(out=mergedT[:, hs_, :], in0=pFB[:], in1=sgb[:, hs_, :], op=ALU.mult),
                     reads=["pFB"] + [("sgb", fc) for fc in range(8)], writes=[("mergedT", half)])
                S.op('pool', lambda e, hs_=hs_: e.tensor_tensor(out=mergedT[:, hs_, :], in0=mergedT[:, hs_, :], in1=mtmp[:], op=ALU.add),
                     reads=["mtmp", ("mergedT", half)], writes=[("mergedT", half)])
            for j, (pt, ptn) in enumerate(((pT0, "pT0"), (pT1, "pT1"))):
                for kc in range(8):
                    S.op('pe', lambda e, j=j, kc=kc, pt=pt: e.matmul(pt[:], lhsT=mergedT[:, kc, :], rhs=wo_bf[:, kc, j * 512:(j + 1) * 512],
                                                                 start=(kc == 0), stop=(kc == 7)),
                         reads=[("mergedT", 0), ("mergedT", 1), "wo"], writes=[ptn])
                S.op('dve', lambda e, j=j, pt=pt, b=b: e.scalar_tensor_tensor(out=pre[:, j * 512:(j + 1) * 512], in0=xtm[b][:, j * 512:(j + 1) * 512],
                                                                          scalar=DN_ALPHA, in1=pt[:], op0=ALU.mult, op1=ALU.add),
                     reads=[ptn, ("xtm", 0)], writes=[("pre", j)])
            layer_norm(S, pre, x1, stats, mv, lng, lnb, [("pre", 0), ("pre", 1)], "x1", ["lng", "lnb"])
            S.dma(x1_d[t], x1[:], reads=["x1", ("pre", 0), ("pre", 1)], writes=[("dram_x1", t)], key="x1dma")


    S.barrier()
    if p2tiles == NT:
        cache_k = din("cache_k", [2560 * 8, 16 * 512]); cache_v = din("cache_v", [2560 * 8, 16 * 512])
        pt_exp = din("pt_exp", [128, 16], I32)
        scst = din("scst", [128, 8 + 128 + 8 + 128 + 128 + 128])
        lseq_d = din("lseq", [16, 17, 128])
        lblk_d = din("lblk", [9, 2, 128])
        with ExitStack() as sz:
            sbz = mk_sb(sz)
            psz = mk_ps(sz)
            wa_z = sbz("wa_z", [128, 4, D], BF16); wb_z = sbz("wb_z", [128, 4, D], BF16); wo_z = sbz("wo_z", [128, 8, D], BF16)
            lng_z = sbz("lng_z", [128, D]); lnb_z = sbz("lnb_z", [128, D])
            scs = sbz("scs", [128, 528])
            blockind = scs[:, 0:8]
            bexp_s = scs[:, 8:136].rearrange("p (h r) -> p h r", h=8)
            bexp_new = scs[:, 136:144]
            slq_s = scs[:, 144:152]
            lseq_bf = sbz("lseq_bf", [16, 17, 128], BF16); lblk_bf = sbz("lblk_bf", [9, 2, 128], BF16)
            causal_z = sbz("causal_z", [128, 128], BF16); seqm_bf = sbz("seqm_bf", [16, 128], BF16); ident_z = sbz("ident_z", [128, 128], BF16)
            idx_z = sbz("idx_z", [128, 16], I32)
            Kst = sbz("Kst", [128, 16, 512]); Vst = sbz("Vst", [128, 16, 512])
            kT_z = sbz("kT_z", [128, 4, 2048], BF16); V_z = sbz("V_z", [128, 16, 8, 65], BF16)
            kmT_z = sbz("kmT_z", [128, 4, 8]); kmTz_z = sbz("kmTz_z", [128, 8, 8])
            Mq_z = sbz("Mq_z", [128, 8, 9]); gm_z = sbz("gm_z", [128, 8, 8]); mx_z = sbz("mx_z", [128, 8, 8])
            R_z = sbz("R_z", [9, 8, 128], BF16)
            PT_z = [sbz("PT_z%d" % i, [128, 4, 128], BF16) for i in range(2)]
            yacc = sbz("yacc", [128, 8, 65]); rsum_z = sbz("rsum_z", [128, 8])
            yatt_z = sbz("yatt_z", [128, 8, 64]); yattT_z = sbz("yattT_z", [128, 4, 128], BF16)
            mtmp_z = sbz("mtmp_z", [128, 4, 128]); mergedT_z = sbz("mergedT_z", [128, 8, 128], BF16)
            pre_z = sbz("pre_z", [128, D]); xtm_z = sbz("xtm_z", [128, D])
            stats_z = sbz("stats_z", [128, 2, 6]); mv_z = sbz("mv_z", [128, 4])
            lt32_z = sbz("lt32_z", [16, 17, 128]); lb32_z = sbz("lb32_z", [9, 2, 128])
            pFA_z = psz("pFA_z", [128, 4, 128]); pFB_z = psz("pFB_z", [128, 4, 128])
            pT0_z = psz("pT0_z", [128, 512]); pT1_z = psz("pT1_z", [128, 512])
            pS_z = [psz("pS_z%d" % i, [128, 4, 128]) for i in range(2)]
            pO_z = [psz("pO_z%d" % i, [128, 4, 65]) for i in range(2)]
            kst2 = Kst[:].rearrange("p r c -> p (r c)")
            war_z = w_a.rearrange("(k p) n -> p k n", p=128); wbr_z = w_b.rearrange("(k p) n -> p k n", p=128); wor_z = w_o.rearrange("(k p) n -> p k n", p=128)
            S.dma(kst2[:, 0:4096].rearrange("p (k n) -> p k n", k=4), war_z, writes=["Kst"])
            S.op('pool', lambda e: e.tensor_copy(out=wa_z[:], in_=kst2[:, 0:4096].rearrange("p (k n) -> p k n", k=4)), reads=["Kst"], writes=["wa_z"])
            S.dma(kst2[:, 4096:8192].rearrange("p (k n) -> p k n", k=4), wbr_z, writes=["Kst2"])
            S.op('pool', lambda e: e.tensor_copy(out=wb_z[:], in_=kst2[:, 4096:8192].rearrange("p (k n) -> p k n", k=4)), reads=["Kst2"], writes=["wb_z"])
            vst2 = Vst[:].rearrange("p r c -> p (r c)")
            S.dma(vst2.rearrange("p (k n) -> p k n", k=8), wor_z, writes=["Vst"])
            S.op('pool', lambda e: e.tensor_copy(out=wo_z[:], in_=vst2.rearrange("p (k n) -> p k n", k=8)), reads=["Vst"], writes=["wo_z"])
            S.dma(lng_z[:], ln1g_bc, writes=["lng_z"]); S.dma(lnb_z[:], ln1b_bc, writes=["lnb_z"])
            S.dma(scs[:], scst, writes=["scs"])
            S.dma(lt32_z[:], lseq_d, writes=["lt32_z"]); S.dma(lb32_z[:], lblk_d, writes=["lb32_z"])
            S.dma(idx_z[:], pt_exp, writes=["idx_z"])
            S.dma(xtm_z[:], x_tm[NT - 1], writes=["xtm_z"])
            S.op('dve', lambda e: e.tensor_copy(out=lseq_bf[:], in_=lt32_z[:]), reads=["lt32_z"], writes=["lseq_bf"])
            S.op('dve', lambda e: e.tensor_copy(out=lblk_bf[:], in_=lb32_z[:]), reads=["lb32_z"], writes=["lblk_bf"])
            S.op('dve', lambda e: e.tensor_copy(out=causal_z[:], in_=scs[:, 272:400]), reads=["scs"], writes=["causal_z"])
            S.op('dve', lambda e: e.tensor_copy(out=seqm_bf[:], in_=scs[0:16, 400:528]), reads=["scs"], writes=["seqm_bf"])
            S.op('dve', lambda e: e.tensor_copy(out=ident_z[:], in_=ident), reads=["cst"], writes=["ident_z"])
            idf_z = sbz("idf_z", [128, 16])
            S.op('dve', lambda e: e.tensor_copy(out=idf_z[:], in_=idx_z[:]), reads=["idx_z"], writes=["idf_z"])
            S.op('dve', lambda e: e.tensor_scalar(out=idf_z[:], in0=idf_z[:], scalar1=8.0, scalar2=scs[:, 152:153], op0=ALU.mult, op1=ALU.add),
                 reads=["idf_z", "scs"], writes=["idf_z"])
            S.op('dve', lambda e: e.tensor_copy(out=idx_z[:], in_=idf_z[:]), reads=["idf_z"], writes=["idx_z"])
            S.op('pool', lambda e: e.memset(V_z[:, :, :, 64:65], 1.0), writes=["V_z1"])
            S.op('pool', lambda e: e.memset(kmTz_z[:], 0.0), writes=["kmTz_z"])
            S.op('pool', lambda e: e.memset(Mq_z[:, :, 0:8], 0.0), writes=["Mq_z"])
            S.op('dve', lambda e: e.tensor_copy(out=Mq_z[:, :, 8], in_=slq_s), reads=["scs", "Mq_z"], writes=["Mq_z"])
            S.op('pool', lambda e: e.memset(yacc[:], 0.0), writes=["yacc"])
            zctr = [0]

            def mask_rows():
                for h in range(8):
                    tgt, tn = (pT1_z, "pT1_z") if h < 4 else (pFA_z, "pFA_z")
                    tv = tgt[0:9, :].rearrange("p (h r) -> p h r", h=4) if h < 4 else tgt[0:9, :, :]
                    S.op('pe', lambda e, h=h, tv=tv: e.transpose(tv[:, h % 4, :], Mq_z[:, h, :], ident), reads=["Mq_z", "cst"], writes=[tn])
                S.op('act', lambda e: e.activation(out=R_z[:, 0:4, :], in_=pT1_z[0:9, :].rearrange("p (h r) -> p h r", h=4), func=AF.Copy, scale=BIGM),
                     reads=["pT1_z"], writes=["R_z0"])
                S.op('act', lambda e: e.activation(out=R_z[:, 4:8, :], in_=pFA_z[0:9, :, :], func=AF.Copy, scale=BIGM),
                     reads=["pFA_z"], writes=["R_z1"])

            for sq in range(16):
                S.op('pool', lambda e, sq=sq: e.indirect_dma_start(out=Kst[:].rearrange("p r c -> p (r c)"), out_offset=None, in_=cache_k,
                                                                 in_offset=bass.IndirectOffsetOnAxis(ap=idx_z[:, sq:sq + 1].bitcast(U32), axis=0)),
                     reads=["idx_z", "wa_z", "wb_z"], writes=["Kst"], dma_key="Kst_g")
                S.op('pool', lambda e, sq=sq: e.indirect_dma_start(out=Vst[:].rearrange("p r c -> p (r c)"), out_offset=None, in_=cache_v,
                                                                 in_offset=bass.IndirectOffsetOnAxis(ap=idx_z[:, sq:sq + 1].bitcast(U32), axis=0)),
                     reads=["idx_z", "wo_z"], writes=["Vst"], dma_key="Vst_g")
                for r in range(16):
                    for q in range(4):
                        S.op('pe', lambda e, r=r, q=q: e.transpose(pFB_z[:, q, :], Kst[:, r, q * 128:(q + 1) * 128], ident), reads=["Kst", "cst"], writes=["pFB_z"])
                    S.op('act', lambda e, r=r: e.activation(out=kT_z[:, :, r * 128:(r + 1) * 128], in_=pFB_z[:], func=AF.Copy), reads=["pFB_z"], writes=[("kT_z", r)])
                for q in range(4):
                    for r in range(16):
                        S.op('pe', lambda e, r=r, q=q: e.matmul(pT0_z[:, q * 8:(q + 1) * 8], lhsT=Kst[:, r, q * 128:(q + 1) * 128], rhs=blockind,
                                                               start=(r == 0), stop=(r == 15)), reads=["Kst", "scs"], writes=["pT0_z"])
                S.op('dve', lambda e: e.tensor_scalar(out=kmT_z[:], in0=pT0_z[:, 0:32].rearrange("p (c n) -> p c n", c=4), scalar1=1.0 / 256.0, scalar2=None, op0=ALU.mult),
                     reads=["pT0_z"], writes=["kmT_z"])
                kz_z = kmTz_z[:].rearrange("p (c two) n -> p c two n", two=2)
                S.op('dve', lambda e, kz_z=kz_z: e.tensor_copy(out=kz_z[0:64, :, 0, :], in_=kmT_z[0:64, :, :]), reads=["kmT_z", "kmTz_z"], writes=["kmTz_z"])
                S.op('dve', lambda e, kz_z=kz_z: e.tensor_copy(out=kz_z[64:128, :, 1, :], in_=kmT_z[64:128, :, :]), reads=["kmT_z", "kmTz_z"], writes=["kmTz_z"])
                S.op('pool', lambda e: e.tensor_copy(out=V_z[:, :, :, 0:64], in_=Vst[:].rearrange("p r (h d) -> p r h d", h=8)), reads=["Vst"], writes=["V_z"])
                for h in range(8):
                    S.op('pe', lambda e, h=h: e.matmul(pT0_z[:, 64 + h * 8:64 + (h + 1) * 8], lhsT=qT32s[:, h // 2, :], rhs=kmTz_z[:, h, :],
                                                      start=(h == 0), stop=(h == 7)), reads=["qT32", "kmTz_z"], writes=["pT0_z"])
                S.op('dve', lambda e: e.tensor_copy(out=gm_z[:], in_=pT0_z[:, 64:128].rearrange("p (h n) -> p h n", h=8)), reads=["pT0_z"], writes=["gm_z"])
                for h in range(8):
                    S.op('dve', lambda e, h=h: e.max(out=mx_z[:, h, :], in_=gm_z[:, h, :]), reads=["gm_z"], writes=[("mx_z", h)])
                    S.op('dve', lambda e, h=h: e.tensor_scalar(out=Mq_z[:, h, 0:8], in0=gm_z[:, h, :], scalar1=mx_z[:, h, 2:3], scalar2=-1.0,
                                                               op0=ALU.is_ge, op1=ALU.add), reads=["gm_z", ("mx_z", h), "Mq_z"], writes=["Mq_z"])
                mask_rows()
                for h in range(8):
                    hp, hcx = h % 2, h // 2
                    ob, obn = pO_z[h // 4], "pO_z%d" % (h // 4)
                    for r0 in range(0, 16, 4):
                        bk = zctr[0] % 2
                        zctr[0] += 1
                        for sl_ in range(4):
                            r = r0 + sl_
                            S.op('pe', lambda e, hp=hp, hcx=hcx, r=r, bk=bk, sl_=sl_: e.matmul(
                                pS_z[bk][:, sl_, :], lhsT=kT_z[hp * 64:(hp + 1) * 64, hcx, r * 128:(r + 1) * 128],
                                rhs=qTbs[hp * 64:(hp + 1) * 64, hcx, :], start=True, stop=False), reads=[("kT_z", r), "qTb"], writes=[("pS_z", bk)])
                            S.op('pe', lambda e, h=h, bk=bk, sl_=sl_: e.matmul(pS_z[bk][:, sl_, :], lhsT=lblk_bf[0:9, 0, :], rhs=R_z[0:9, h, :], start=False, stop=False),
                                 reads=["lblk_bf", "R_z%d" % (h // 4)], writes=[("pS_z", bk)])
                            S.op('pe', lambda e, sq=sq, bk=bk, sl_=sl_: e.matmul(pS_z[bk][:, sl_, :], lhsT=lseq_bf[0:16, sq, :], rhs=seqm_bf[0:16, :], start=False, stop=True),
                                 reads=["lseq_bf", "seqm_bf"], writes=[("pS_z", bk)])
                        for sl_ in range(4):
                            r = r0 + sl_
                            S.op('act', lambda e, h=h, bk=bk, sl_=sl_, r=r: e.activation(out=PT_z[bk][:, sl_, :], in_=pS_z[bk][:, sl_, :], func=AF.Exp, scale=0.125,
                                                                                       bias=bexp_s[:, h, r:r + 1]), reads=[("pS_z", bk), "scs"], writes=[("PT_z", bk)])
                        for sl_ in range(4):
                            r = r0 + sl_
                            S.op('pe', lambda e, h=h, bk=bk, sl_=sl_, r=r, ob=ob: e.matmul(ob[:, h % 4, :], lhsT=PT_z[bk][:, sl_, :], rhs=V_z[:, r, h, :],
                                                                                       start=(r == 0), stop=(r == 15)),
                                 reads=[("PT_z", bk), "V_z", "V_z1"], writes=[obn])
                for k in range(2):
                    S.op('dve', lambda e, k=k: e.tensor_tensor(out=yacc[:, 4 * k:4 * k + 4, :], in0=yacc[:, 4 * k:4 * k + 4, :], in1=pO_z[k][:], op=ALU.add),
                         reads=["pO_z%d" % k, "yacc"], writes=["yacc"])
            for h in range(8):
                hp, hcx = h % 2, h // 2
                ob, obn = pO_z[h // 4], "pO_z%d" % (h // 4)
                bk = zctr[0] % 2
                zctr[0] += 1
                S.op('pe', lambda e, hp=hp, hcx=hcx, bk=bk: e.matmul(pS_z[bk][:, 0, :], lhsT=kTs_new[hp * 64:(hp + 1) * 64, hcx, :], rhs=qTbs[hp * 64:(hp + 1) * 64, hcx, :],
                                                                 start=True, stop=False), reads=["kTs_new", "qTb"], writes=[("pS_z", bk)])
                S.op('pe', lambda e, h=h, bk=bk: e.matmul(pS_z[bk][:, 0, :], lhsT=lblk_bf[0:9, 1, :], rhs=R_z[0:9, h, :], start=False, stop=False),
                     reads=["lblk_bf", "R_z%d" % (h // 4)], writes=[("pS_z", bk)])
                S.op('pe', lambda e, bk=bk: e.matmul(pS_z[bk][:, 0, :], lhsT=ident_z[:], rhs=causal_z[:], start=False, stop=True),
                     reads=["ident_z", "causal_z"], writes=[("pS_z", bk)])
                S.op('act', lambda e, h=h, bk=bk: e.activation(out=PT_z[bk][:, 0, :], in_=pS_z[bk][:, 0, :], func=AF.Exp, scale=0.125, bias=bexp_new[:, h:h + 1]),
                     reads=[("pS_z", bk), "scs"], writes=[("PT_z", bk)])
                S.op('pe', lambda e, h=h, bk=bk, ob=ob: e.matmul(ob[:, h % 4, :], lhsT=PT_z[bk][:, 0, :], rhs=Vs_new[:, h, :], start=True, stop=True),
                     reads=[("PT_z", bk), "Vs_new", "Vs_new1"], writes=[obn])
            for k in range(2):
                S.op('dve', lambda e, k=k: e.tensor_tensor(out=yacc[:, 4 * k:4 * k + 4, :], in0=yacc[:, 4 * k:4 * k + 4, :], in1=pO_z[k][:], op=ALU.add),
                     reads=["pO_z%d" % k, "yacc"], writes=["yacc"])
            S.op('dve', lambda e: e.reciprocal(out=rsum_z[:], in_=yacc[:, :, 64]), reads=["yacc"], writes=["rsum_z"])
            S.op('dve', lambda e: e.tensor_tensor(out=yatt_z[:], in0=yacc[:, :, 0:64], in1=rsum_z[:].unsqueeze(2).to_broadcast([128, 8, 64]), op=ALU.mult),
                 reads=["yacc", "rsum_z"], writes=["yatt_z"])
            yv_z = yatt_z[:].rearrange("p h d -> p (h d)")
            for q in range(4):
                S.op('pe', lambda e, q=q: e.transpose(pFA_z[:, q, :], yv_z[:, q * 128:(q + 1) * 128], ident), reads=["yatt_z", "cst"], writes=["pFA_z"])
            S.op('act', lambda e: e.activation(out=yattT_z[:], in_=pFA_z[:], func=AF.Copy), reads=["pFA_z"], writes=["yattT_z"])
            tz = NT - 1
            for half in range(2):
                for q in range(4):
                    fc = 4 * half + q
                    for kc in range(4):
                        S.op('pe', lambda e, q=q, fc=fc, kc=kc: e.matmul(pFA_z[:, q, :], lhsT=wa_z[:, kc, fc * 128:(fc + 1) * 128], rhs=yattT_z[:, kc, :],
                                                                      start=(kc == 0), stop=(kc == 3)), reads=["wa_z", "yattT_z"], writes=["pFA_z"])
                for q in range(4):
                    fc = 4 * half + q
                    for kc in range(4):
                        S.op('pe', lambda e, q=q, fc=fc, kc=kc: e.matmul(pFB_z[:, q, :], lhsT=wb_z[:, kc, fc * 128:(fc + 1) * 128],
                                                                      rhs=yssmT[:, kc, tz * 128:(tz + 1) * 128], start=(kc == 0), stop=(kc == 3)),
                             reads=["wb_z", ("yssmT", tz)], writes=["pFB_z"])
                hs_ = slice(4 * half, 4 * half + 4)
                S.op('dve', lambda e, hs_=hs_: e.tensor_tensor(out=mtmp_z[:], in0=pFA_z[:], in1=sgas[:, hs_, :], op=ALU.mult),
                     reads=["pFA_z"] + [("sga", fc) for fc in range(8)], writes=["mtmp_z"])
                S.op('dve', lambda e, hs_=hs_: e.tensor_tensor(out=mergedT_z[:, hs_, :], in0=pFB_z[:], in1=sgbs[:, hs_, :], op=ALU.mult),
                     reads=["pFB_z"] + [("sgb", fc) for fc in range(8)], writes=[("mergedT_z", half)])
                S.op('pool', lambda e, hs_=hs_: e.tensor_tensor(out=mergedT_z[:, hs_, :], in0=mergedT_z[:, hs_, :], in1=mtmp_z[:], op=ALU.add),
                     reads=["mtmp_z", ("mergedT_z", half)], writes=[("mergedT_z", half)])
            for j, (pt, ptn) in enumerate(((pT0_z, "pT0_z"), (pT1_z, "pT1_z"))):
                for kc in range(8):
                    S.op('pe', lambda e, j=j, kc=kc, pt=pt: e.matmul(pt[:], lhsT=mergedT_z[:, kc, :], rhs=wo_z[:, kc, j * 512:(j + 1) * 512],
                                                                 start=(kc == 0), stop=(kc == 7)),
                         reads=[("mergedT_z", 0), ("mergedT_z", 1), "wo_z"], writes=[ptn])
                S.op('dve', lambda e, j=j, pt=pt: e.scalar_tensor_tensor(out=pre_z[:, j * 512:(j + 1) * 512], in0=xtm_z[:, j * 512:(j + 1) * 512],
                                                                     scalar=DN_ALPHA, in1=pt[:], op0=ALU.mult, op1=ALU.add),
                     reads=[ptn, "xtm_z"], writes=[("pre_z", j)])
            layer_norm(S, pre_z, pre_z, stats_z, mv_z, lng_z, lnb_z, [("pre_z", 0), ("pre_z", 1)], "x1_z", ["lng_z", "lnb_z"])
            S.dma(x1_d[tz], pre_z[:], reads=["x1_z", ("pre_z", 0), ("pre_z", 1)], writes=[("dram_x1", tz)], key="x1dma")
    S.barrier()
    s12.close()
    if stop_after < 3:
        S.finish('sp')
        with nc.Block() as block:
            S.replay(block)
        st.close()
        return nc
    w_pq = din("w_pq", [D, 2048]); skT_d = din("skT", [128, 2, 8, 128])
    puT = din("peer_uT", [128, 128, 8, 128])
    pv_d = din("peer_v", [16384, D])
    ln2g_bc = din("ln2g_bc", [128, D]); ln2b_bc = din("ln2b_bc", [128, D])
    y_out = dout("y_out", [NT, 128, D])
    u_scr = nc.dram_tensor("u_scr", [128, 128, 8, 128], BF16, kind="Internal").ap()
    v_scr = nc.dram_tensor("v_scr", [128, 128, D], BF16, kind="Internal").ap()
    with ExitStack() as s3:
        sb3 = mk_sb(s3)
        ps3 = mk_ps(s3)
        wpq_bf = sb3("wpq_bf", [128, 8, 2048], BF16)
        skT = sb3("skT_bf", [128, 2, 8, 128], BF16)
        lng2 = sb3("lng2", [128, D]); lnb2 = sb3("lnb2", [128, D])
        iota16 = sb3("iota16", [128, 16]); iota128 = sb3("iota128", [128, 128])
        with ExitStack() as sl:
            sbl = mk_sb(sl)
            stg3 = [sbl("stg3_%d" % i, [128, 8, 256]) for i in range(2)]
            wpr = w_pq.rearrange("(k p) n -> p k n", p=128)
            for c in range(8):
                b = c % 2
                S.dma(stg3[b][:], wpr[:, :, c * 256:(c + 1) * 256], writes=[("stg3", b)])
                S.op('pool', lambda e, b=b, c=c: e.tensor_copy(out=wpq_bf[:, :, c * 256:(c + 1) * 256], in_=stg3[b][:]), reads=[("stg3", b)], writes=["wpq"])
            S.dma(stg3[0][:].rearrange("p a b -> p (a b)"), skT_d.rearrange("p s h m -> p (s h m)"), writes=[("stg3", 0)])
            S.op('pool', lambda e: e.tensor_copy(out=skT[:].rearrange("p s h m -> p (s h m)"), in_=stg3[0][:].rearrange("p a b -> p (a b)")),
                 reads=[("stg3", 0)], writes=["skT"])
            S.dma(lng2[:], ln2g_bc, writes=["lng2"]); S.dma(lnb2[:], ln2b_bc, writes=["lnb2"])
            S.op('pool', lambda e: e.iota(iota16[:], pattern=[[1, 16]], base=0, channel_multiplier=0, allow_small_or_imprecise_dtypes=True), writes=["iota16"])
            S.op('pool', lambda e: e.iota(iota128[:], pattern=[[1, 128]], base=0, channel_multiplier=0, allow_small_or_imprecise_dtypes=True), writes=["iota128"])
        S.barrier()

        G = sb3("G", [128, 128, 256], BF16)
        x1g = [sb3("x1g%d" % i, [128, D]) for i in range(2)]
        x1T = sb3("x1T", [128, 8, 256], BF16)
        qTp = sb3("qTp", [128, 16, 128], BF16)
        s_sb = sb3("s_sb", [128, 8, 128])
        tmpm = sb3("tmpm", [128, 256])
        vals = sb3("vals", [128, 2, 8, 16]); idx = sb3("idx", [128, 2, 8, 16], U32); idxf = sb3("idxf", [128, 2, 8, 16])
        cand = sb3("cand", [128, 8, 16, 16]); big2 = sb3("big2", [128, 8, 16, 16])
        scv = sb3("scv", [128, 8, 16]); ci = sb3("ci", [128, 8, 16], U32); abi = sb3("abi", [128, 2, 8, 16], I32); abf = sb3("abf", [128, 2, 8, 16])
        nmax = sb3("nmax", [128, 8]); gsum = sb3("gsum", [128, 8]); egt = sb3("egt", [128, 3, 8, 16])
        egT = [sb3("egT%d" % i, [128, 3, 128]) for i in range(2)]
        NLR = 6
        Lb = [sb3("Lb%d" % i, [128, 128], BF16) for i in range(NLR)]
        Rb = [sb3("Rb%d" % i, [128, 128], BF16) for i in range(NLR)]
        ustg = [sb3("ustg%d" % i, [128, 8, 128]) for i in range(2)]
        vstg = [sb3("vstg%d" % i, [128, D]) for i in range(2)]
        NWB = 3
        ubf = [sb3("ubf%d" % i, [128, 8, 128], BF16) for i in range(NWB)]
        vbf = [sb3("vbf%d" % i, [128, D], BF16) for i in range(NWB)]
        glb = [sb3("glb%d" % i, [128, 256], BF16) for i in range(2)]
        WT = [sb3("WT%d" % i, [128, 256], BF16) for i in range(2)]
        pre2 = sb3("pre2", [128, D])
        stats2 = sb3("stats2", [128, 2, 6]); mv2 = sb3("mv2", [128, 4])
        pAcc = [ps3("pAcc%d" % i, [128, 512]) for i in range(4)]
        pAT = [ps3("pAT%d" % i, [128, 256]) for i in range(2)]
        pGb = [ps3("pGb%d" % i, [128, 4, 128]) for i in range(2)]

        groups = [(2 * i, 2 * i + 1) for i in range(8)] + [(16,)]
        if glimit is not None:
            groups = [groups[i] for i in glimit]
        lr_ctr = [0]; gb_ctr = [0]; wctr = [0]
        for gi, tiles in enumerate(groups):
            ntl = len(tiles)
            ntok = 128 * ntl
            for li, t in enumerate(tiles):
                S.dma(x1g[li][:], x1_d[t], reads=[("dram_x1", t)], writes=[("x1g", li)])
                for half in range(2):
                    pb, pbn = pGb[half], "pGb%d" % half
                    for q in range(4):
                        kk = 4 * half + q
                        S.op('pe', lambda e, li=li, kk=kk, q=q, pb=pb: e.transpose(pb[:, q, :], x1g[li][:, kk * 128:(kk + 1) * 128], ident),
                             reads=[("x1g", li), "cst"], writes=[pbn])
                    S.op('act', lambda e, li=li, half=half, pb=pb: e.activation(out=x1T[:, 4 * half:4 * half + 4, li * 128:(li + 1) * 128], in_=pb[:], func=AF.Copy),
                         reads=[pbn], writes=[("x1T", li)])
                for c4 in range(4):
                    pb, pbn = pGb[c4 % 2], "pGb%d" % (c4 % 2)
                    for q in range(4):
                        c = 4 * c4 + q
                        for kk in range(8):
                            S.op('pe', lambda e, li=li, c=c, q=q, kk=kk, pb=pb: e.matmul(pb[:, q, :], lhsT=wpq_bf[:, kk, c * 128:(c + 1) * 128],
                                                                                    rhs=x1T[:, kk, li * 128:(li + 1) * 128], start=(kk == 0), stop=(kk == 7)),
                                 reads=[("x1T", li), "wpq"], writes=[pbn])
                    S.op('act', lambda e, c4=c4, pb=pb: e.activation(out=qTp[:, 4 * c4:4 * c4 + 4, :], in_=pb[:], func=AF.Copy), reads=[pbn], writes=[("qTp", c4)])
                for side in range(2):
                    for hh in range(2):
                        pb, pbn = pGb[hh], "pGb%d" % hh
                        for q in range(4):
                            h = 4 * hh + q
                            S.op('pe', lambda e, h=h, q=q, side=side, pb=pb: e.matmul(pb[:, q, :], lhsT=qTp[:, 2 * h + side, :], rhs=skT[:, side, h, :],
                                                                                 start=True, stop=True),
                                 reads=[("qTp", (2 * h + side) // 4), "skT"], writes=[pbn])
                        S.op('act', lambda e, hh=hh, pb=pb: e.activation(out=s_sb[:, 4 * hh:4 * hh + 4, :], in_=pb[:], func=AF.Copy), reads=[pbn], writes=[("s_sb", hh)])
                    for h in range(8):
                        rr = [("s_sb", h // 4)]
                        S.op('dve', lambda e, h=h, side=side: e.max(out=vals[:, side, h, 0:8], in_=s_sb[:, h, :]), reads=rr, writes=["vals"])
                        S.op('dve', lambda e, h=h, side=side: e.max_index(out=idx[:, side, h, 0:8], in_max=vals[:, side, h, 0:8], in_values=s_sb[:, h, :]),
                             reads=rr + ["vals"], writes=["idx"])
                        S.op('dve', lambda e, h=h, side=side: e.match_replace(out=tmpm[:, 0:128], in_to_replace=vals[:, side, h, 0:8], in_values=s_sb[:, h, :], imm_value=-1.0e30),
                             reads=rr + ["vals"], writes=["tmpm"])
                        S.op('dve', lambda e, h=h, side=side: e.max(out=vals[:, side, h, 8:16], in_=tmpm[:, 0:128]), reads=["tmpm"], writes=["vals"])
                        S.op('dve', lambda e, h=h, side=side: e.max_index(out=idx[:, side, h, 8:16], in_max=vals[:, side, h, 8:16], in_values=tmpm[:, 0:128]),
                             reads=["tmpm", "vals"], writes=["idx"])
                S.op('dve', lambda e: e.tensor_tensor(out=cand[:], in0=vals[:, 0, :, :].unsqueeze(3).to_broadcast([128, 8, 16, 16]),
                                                      in1=vals[:, 1, :, :].unsqueeze(2).to_broadcast([128, 8, 16, 16]), op=ALU.add),
                     reads=["vals"], writes=["cand"])
                for h in range(8):
                    cf = cand[:, h, :, :].rearrange("p a b -> p (a b)")
                    S.op('dve', lambda e, h=h, cf=cf: e.max(out=scv[:, h, 0:8], in_=cf), reads=["cand"], writes=["scv"])
                    S.op('dve', lambda e, h=h, cf=cf: e.max_index(out=ci[:, h, 0:8], in_max=scv[:, h, 0:8], in_values=cf), reads=["cand", "scv"], writes=["ci"])
                    S.op('dve', lambda e, h=h, cf=cf: e.match_replace(out=tmpm[:], in_to_replace=scv[:, h, 0:8], in_values=cf, imm_value=-1.0e30),
                         reads=["cand", "scv"], writes=["tmpm"])
                    S.op('dve', lambda e, h=h: e.max(out=scv[:, h, 8:16], in_=tmpm[:]), reads=["tmpm"], writes=["scv"])
                    S.op('dve', lambda e, h=h: e.max_index(out=ci[:, h, 8:16], in_max=scv[:, h, 8:16], in_values=tmpm[:]), reads=["tmpm", "scv"], writes=["ci"])
                S.op('dve', lambda e: e.tensor_scalar(out=nmax[:], in0=scv[:, :, 0], scalar1=-1.0, scalar2=None, op0=ALU.mult), reads=["scv"], writes=["nmax"])
                for h in range(8):
                    S.op('act', lambda e, h=h: e.activation(out=egt[:, 2, h, :], in_=scv[:, h, :], func=AF.Exp, bias=nmax[:, h:h + 1], accum_out=gsum[:, h:h + 1]),
                         reads=["scv", "nmax"], writes=["g_un"])
                S.op('dve', lambda e: e.reciprocal(out=gsum[:], in_=gsum[:]), reads=["g_un"], writes=["gsum"])
                S.op('dve', lambda e: e.tensor_tensor(out=egt[:, 2, :, :], in0=egt[:, 2, :, :], in1=gsum[:].unsqueeze(2).to_broadcast([128, 8, 16]), op=ALU.mult),
                     reads=["g_un", "gsum"], writes=["egt_g"])
                cii = ci[:].bitcast(I32)
                S.op('dve', lambda e, cii=cii: e.tensor_single_scalar(out=abi[:, 0, :, :], in_=cii, scalar=4, op=ALU.arith_shift_right), reads=["ci"], writes=["abi"])
                S.op('dve', lambda e, cii=cii: e.tensor_single_scalar(out=abi[:, 1, :, :], in_=cii, scalar=15, op=ALU.bitwise_and), reads=["ci"], writes=["abi"])
                S.op('dve', lambda e: e.tensor_copy(out=abf[:], in_=abi[:]), reads=["abi"], writes=["abf"])
                S.op('dve', lambda e: e.tensor_copy(out=idxf[:], in_=idx[:].bitcast(I32)), reads=["idx"], writes=["idxf"])
                for side in range(2):
                    S.op('dve', lambda e, side=side: e.tensor_tensor(out=big2[:], in0=abf[:, side, :, :].unsqueeze(3).to_broadcast([128, 8, 16, 16]),
                                                                   in1=iota16[:].unsqueeze(1).unsqueeze(1).to_broadcast([128, 8, 16, 16]), op=ALU.is_equal),
                         reads=["abf", "iota16"], writes=["big2"])
                    S.op('dve', lambda e, side=side: e.tensor_tensor(out=big2[:], in0=big2[:], in1=idxf[:, side, :, :].unsqueeze(2).to_broadcast([128, 8, 16, 16]), op=ALU.mult),
                         reads=["big2", "idxf"], writes=["big2"])
                    S.op('dve', lambda e, side=side: e.tensor_reduce(out=egt[:, side, :, :], in_=big2[:], axis=AX.X, op=ALU.add),
                         reads=["big2"], writes=[("egt_e", side)])
                eb = egT[li]
                for j in range(3):
                    S.op('pe', lambda e, j=j: e.transpose(pGb[0][:, j, :], egt[:, j, :, :].rearrange("p h k -> p (h k)"), ident),
                         reads=[("egt_e", 0), ("egt_e", 1), "egt_g", "cst"], writes=["pGb0"])
                S.op('act', lambda e, eb=eb: e.activation(out=eb[:], in_=pGb[0][:, 0:3, :], func=AF.Copy), reads=["pGb0"], writes=[("egT", li)])
                for t4 in range(32):
                    gb = gb_ctr[0] % 2
                    gb_ctr[0] += 1
                    for sl_ in range(4):
                        tk = 4 * t4 + sl_
                        i = lr_ctr[0] % NLR
                        lr_ctr[0] += 1
                        S.op('dve', lambda e, i=i, tk=tk, eb=eb: e.tensor_scalar(out=Lb[i][:], in0=iota128[:], scalar1=eb[:, 0, tk:tk + 1], scalar2=eb[:, 2, tk:tk + 1],
                                                                          op0=ALU.is_equal, op1=ALU.mult),
                             reads=[("egT", li), "iota128"], writes=[("Lb", i)])
                        S.op('pool', lambda e, i=i, tk=tk, eb=eb: e.tensor_scalar(out=Rb[i][:], in0=iota128[:], scalar1=eb[:, 1, tk:tk + 1], scalar2=None, op0=ALU.is_equal),
                             reads=[("egT", li), "iota128"], writes=[("Rb", i)])
                        S.op('pe', lambda e, i=i, gb=gb, sl_=sl_: e.matmul(pGb[gb][:, sl_, :], lhsT=Rb[i][:], rhs=Lb[i][:], start=True, stop=True),
                             reads=[("Lb", i), ("Rb", i)], writes=["pGb%d" % gb])
                    c0 = li * 128 + 4 * t4
                    S.op('act', lambda e, gb=gb, c0=c0: e.activation(out=G[:, :, c0:c0 + 4].rearrange("p m t -> p t m"), in_=pGb[gb][:], func=AF.Copy),
                         reads=["pGb%d" % gb], writes=[("G", li)])
            for m1 in range(128):
                wb = wctr[0] % NWB
                wctr[0] += 1
                if gi == 0:
                    sg_ = m1 % 2
                    S.dma(ustg[sg_][:], puT[m1], writes=[("ustg", sg_)])
                    S.dma(vstg[sg_][:], pv_d[m1 * 128:(m1 + 1) * 128, :], writes=[("vstg", sg_)])
                    S.op('pool', lambda e, sg_=sg_, wb=wb: e.tensor_copy(out=ubf[wb][:], in_=ustg[sg_][:]), reads=[("ustg", sg_)], writes=[("ubf", wb)])
                    S.op('pool', lambda e, sg_=sg_, wb=wb: e.tensor_copy(out=vbf[wb][:], in_=vstg[sg_][:]), reads=[("vstg", sg_)], writes=[("vbf", wb)])
                    S.dma(u_scr[m1], ubf[wb][:], reads=[("ubf", wb)], writes=[("dram_uscr", m1)], key=("ubf", wb))
                    S.dma(v_scr[m1], vbf[wb][:], reads=[("vbf", wb)], writes=[("dram_vscr", m1)], key=("vbf", wb))
                else:
                    S.dma(ubf[wb][:], u_scr[m1], reads=[("dram_uscr", m1)], writes=[("ubf", wb)])
                    S.dma(vbf[wb][:], v_scr[m1], reads=[("dram_vscr", m1)], writes=[("vbf", wb)])
                abk = m1 % 2
                for kk in range(8):
                    S.op('pe', lambda e, wb=wb, kk=kk, abk=abk, ntok=ntok: e.matmul(pAT[abk][:, 0:ntok], lhsT=ubf[wb][:, kk, :], rhs=x1T[:, kk, 0:ntok],
                                                                            start=(kk == 0), stop=(kk == 7)),
                         reads=[("ubf", wb)] + [("x1T", li) for li in range(ntl)], writes=["pAT%d" % abk])
                S.op('act', lambda e, abk=abk, ntok=ntok: e.activation(out=glb[abk][:, 0:ntok], in_=pAT[abk][:, 0:ntok], func=AF.Gelu_apprx_tanh),
                     reads=["pAT%d" % abk], writes=[("glb", abk)])
                S.op('dve', lambda e, abk=abk, m1=m1, ntok=ntok: e.tensor_tensor(out=WT[abk][:, 0:ntok], in0=glb[abk][:, 0:ntok], in1=G[:, m1, 0:ntok], op=ALU.mult),
                     reads=[("glb", abk)] + [("G", li) for li in range(ntl)], writes=[("WT", abk)])
                for li in range(ntl):
                    for half in range(2):
                        S.op('pe', lambda e, abk=abk, li=li, half=half, wb=wb, m1=m1: e.matmul(
                            pAcc[2 * li + half][:], lhsT=WT[abk][:, li * 128:(li + 1) * 128], rhs=vbf[wb][:, half * 512:(half + 1) * 512],
                            start=(m1 == 0), stop=(m1 == 127)), reads=[("WT", abk), ("vbf", wb)], writes=["pAcc%d" % (2 * li + half)])
            for li, t in enumerate(tiles):
                for half in range(2):
                    S.op('dve', lambda e, li=li, half=half: e.scalar_tensor_tensor(
                        out=pre2[:, half * 512:(half + 1) * 512], in0=x1g[li][:, half * 512:(half + 1) * 512], scalar=DN_ALPHA,
                        in1=pAcc[2 * li + half][:], op0=ALU.mult, op1=ALU.add),
                        reads=["pAcc%d" % (2 * li + half), ("x1g", li)], writes=[("pre2", half)])
                layer_norm(S, pre2, pre2, stats2, mv2, lng2, lnb2, [("pre2", 0), ("pre2", 1)], "y_sb", ["lng2", "lnb2"])
                S.dma(y_out[t], pre2[:], reads=["y_sb", ("pre2", 0), ("pre2", 1)], writes=[("dram_y", t)], key="ydma")

    S.finish('sp')
    with nc.Block() as block:
        S.replay(block)
    st.close()
    return nc


def _consts():
    ident = np.eye(128, dtype=np.float32)
    psw = np.zeros((128, 128), np.float32)
    for k in range(128):
        psw[k, (k + 64) % 128] = 1.0
    j = np.arange(128)
    tri_p = (j[:, None] <= j[None, :]).astype(np.float32)
    tri_s = tri_p * ((j[:, None] // 8) == (j[None, :] // 8))
    sg = np.zeros((128, 128), np.float32)
    sg[:64, 0] = -1.0
    sg[64:, 0] = 1.0
    return np.ascontiguousarray(np.stack([ident, psw, tri_p, tri_s, sg], axis=1))


def _shared(inp):
    f = lambda a: np.asarray(a, dtype=np.float32)
    a_re = f(inp['a_re']); a_im = f(inp['a_im']); log_dt = f(inp['log_dt'])
    b_re = f(inp['b_re']); b_im = f(inp['b_im']); c_re = f(inp['c_re']); c_im = f(inp['c_im'])
    bc = lambda a, shape: np.ascontiguousarray(np.broadcast_to(a, shape))
    sh = {}
    sh["w_in"] = np.ascontiguousarray(f(inp['w_in']))
    sh["b_in_bc"] = bc(f(inp['b_in'])[None, :], (128, PROJ))
    sh["b_in_fm"] = np.ascontiguousarray(f(inp['b_in']).reshape(32, 128).T)
    sh["are_tm"] = bc(a_re[None], (128, 32, 64)); sh["aim_tm"] = bc(a_im[None], (128, 32, 64))
    sh["ldt_tm"] = bc(log_dt[None], (128, 32))
    sh["are_fm"] = np.ascontiguousarray(np.concatenate([a_re.T, a_re.T], axis=0))
    sh["aim_fm"] = np.ascontiguousarray(np.concatenate([a_im.T, a_im.T], axis=0))
    g_idx = (np.arange(4)[None, :] * 8 + (np.arange(128) // 16)[:, None])
    ci_idx = np.arange(128) % 16
    sh["are_bd"] = np.ascontiguousarray(a_re[g_idx]); sh["aim_bd"] = np.ascontiguousarray(a_im[g_idx])
    sh["ldt_bd"] = np.ascontiguousarray(log_dt[g_idx])
    sh["bre_bd"] = np.ascontiguousarray(b_re[g_idx, :, ci_idx[:, None]])
    sh["bim_bd"] = np.ascontiguousarray(b_im[g_idx, :, ci_idx[:, None]])
    sh["maskbd"] = np.ascontiguousarray((((np.arange(128) // 16) % 4)[:, None] == np.arange(4)[None, :]).astype(np.float32))
    cf = np.concatenate([c_re.transpose(2, 0, 1), c_im.transpose(2, 0, 1)], axis=0)
    sh["c_fm"] = np.ascontiguousarray(cf)
    sh["d_fm"] = np.ascontiguousarray(f(inp['d_skip']).reshape(4, 128).T)
    sh["bglu_fm"] = np.ascontiguousarray(f(inp['b_glu']).reshape(4, 128).T)
    sh["w_glu"] = np.ascontiguousarray(f(inp['w_glu']))
    sh["cst"] = _consts()
    sh["w_a"] = np.ascontiguousarray(f(inp['w_a'])); sh["w_b"] = np.ascontiguousarray(f(inp['w_b'])); sh["w_o"] = np.ascontiguousarray(f(inp['w_o']))
    sh["ln1g_bc"] = bc(f(inp['ln1_g'])[None, :], (128, D)); sh["ln1b_bc"] = bc(f(inp['ln1_b'])[None, :], (128, D))
    slopes = (2.0 ** (-8.0 * (np.arange(8) + 1.0) / 8.0)).astype(np.float64)
    jj = np.arange(128, dtype=np.float64)
    slq = (-8.0 * slopes[None, :] * jj[:, None]) / 30000.0
    bexp = slopes[None, :, None] * (jj[:, None, None] - 128.0 * np.arange(16)[None, None, :])
    causal = np.where(jj[:, None] > jj[None, :], -30000.0, 0.0)
    sh["acst"] = np.ascontiguousarray(np.concatenate([slq, bexp.reshape(128, 128), causal], axis=1).astype(np.float32))
    lt = np.zeros((9, 9, 128), np.float32)
    for v in range(9):
        lt[8, v, :] = 1.0
        if v < 8:
            lt[v, v, :] = 1.0
    sh["ltab"] = lt
    pp = np.arange(128)
    blockind = ((pp[:, None] // 16) == np.arange(8)[None, :]).astype(np.float64)
    posk = 128.0 * (pp // 8) + 16.0 * (pp % 8)
    bexp_s = slopes[None, :, None] * (posk[:, None, None] + np.arange(16)[None, None, :] - 2048.0)
    bexp_new = slopes[None, :] * (pp % 8)[:, None]
    slq_s = np.zeros((128, 128)); slq_s[:, 0:8] = (-8.0 * slopes[None, :] * (pp % 8)[:, None]) / 30000.0
    slq_s[:, 8] = pp % 8
    causal_s = np.where(((pp[:, None] // 8) == (pp[None, :] // 8)) & ((pp[:, None] % 8) <= (pp[None, :] % 8)), 0.0, -30000.0)
    seqm = np.zeros((128, 128)); seqm[0:16, :] = np.where((pp[None, :] // 8) == np.arange(16)[:, None], 0.0, -30000.0)
    sh["scst"] = np.ascontiguousarray(np.concatenate([blockind, bexp_s.reshape(128, 128), bexp_new, slq_s, causal_s, seqm], axis=1).astype(np.float32))
    lseq = np.zeros((16, 17, 128), np.float32)
    for v in range(16):
        lseq[v, v, :] = 1.0
    sh["lseq"] = lseq
    lblk = np.zeros((9, 2, 128), np.float32)
    for n in range(8):
        lblk[n, 0, :] = (pp // 16 == n)
    lblk[8, :, :] = 1.0
    sh["lblk"] = lblk
    sh["cache_k"] = np.asarray(inp['cache_k']).reshape(2560 * 8, 16 * 512)
    sh["cache_v"] = np.asarray(inp['cache_v']).reshape(2560 * 8, 16 * 512)
    sh["w_pq"] = np.ascontiguousarray(f(inp['w_pq']))
    sk = np.stack([f(inp['sub_k1']), f(inp['sub_k2'])], axis=0)
    sh["skT"] = np.ascontiguousarray(sk.transpose(3, 0, 1, 2))
    pu = f(inp['peer_u']).reshape(128, 128, 8, 128)
    sh["peer_uT"] = np.ascontiguousarray(pu.transpose(0, 3, 2, 1))
    sh["peer_v"] = np.ascontiguousarray(f(inp['peer_v']))
    sh["ln2g_bc"] = bc(f(inp['ln2_g'])[None, :], (128, D)); sh["ln2b_bc"] = bc(f(inp['ln2_b'])[None, :], (128, D))
    return sh


def _prep_core(c, inp, sh):
    xp = np.asarray(inp['x_prompt'][c], dtype=np.float32)
    xs = np.asarray(inp['x_sample'][16 * c:16 * c + 16], dtype=np.float32).reshape(128, D)
    x = np.concatenate([xp, xs], axis=0).reshape(NT, 128, D)
    m = dict(sh)
    m["x_tm"] = np.ascontiguousarray(x)
    m["x_fm"] = np.ascontiguousarray(x.reshape(NT, 128, 8, 128).transpose(0, 3, 2, 1))
    hre = np.asarray(inp['state_ssm_re'][16 * c:16 * c + 16], np.float32).transpose(2, 1, 0)
    him = np.asarray(inp['state_ssm_im'][16 * c:16 * c + 16], np.float32).transpose(2, 1, 0)
    m["h0_fm"] = np.ascontiguousarray(np.concatenate([hre, him], axis=0))
    m["h0sw_fm"] = np.ascontiguousarray(np.concatenate([him, hre], axis=0))
    pt = np.asarray(inp['page_table'][16 * c:16 * c + 16], dtype=np.int32)
    m["pt_exp"] = np.ascontiguousarray(pt[:, np.arange(128) // 8].T)
    return m


def kernel(_debug=False, _glimit=None, **inp):
    nc = bass.Bass("TRN2", target_bir_lowering=False)
    build(nc, debug=_debug, glimit=_glimit)
    sh = _shared(inp)
    in_maps = [_prep_core(c, inp, sh) for c in range(NCORES)]
    res = run_bass_kernel_spmd(nc, in_maps, core_ids=list(range(NCORES)))
    R = res.results
    sp = np.stack([R[c]["ssm_p"] for c in range(NCORES)]).reshape(8, 32, 2, 64)
    ss = np.concatenate([R[c]["ssm_s"] for c in range(NCORES)], axis=0).reshape(128, 32, 2, 64)
    y_all = np.stack([R[c]["y_out"] for c in range(NCORES)])
    k_all = np.stack([R[c]["k_out"] for c in range(NCORES)])
    v_all = np.stack([R[c]["v_out"] for c in range(NCORES)])
    outs = (np.ascontiguousarray(y_all[:, :16].reshape(8, SEQ, D)),
            np.ascontiguousarray(y_all[:, 16].reshape(128, 8, D)),
            np.ascontiguousarray(k_all[:, :16].reshape(8, SEQ, 8, 64)),
            np.ascontiguousarray(v_all[:, :16].reshape(8, SEQ, 8, 64)),
            np.ascontiguousarray(k_all[:, 16].reshape(128, 8, 8, 64)),
            np.ascontiguousarray(v_all[:, 16].reshape(128, 8, 8, 64)),
            np.ascontiguousarray(sp[:, :, 0]), np.ascontiguousarray(sp[:, :, 1]),
            np.ascontiguousarray(ss[:, :, 0]), np.ascontiguousarray(ss[:, :, 1]))
    if _debug:
        return outs, R
    return outs
```

```python
from contextlib import ExitStack
import numpy as np
import concourse.bass as bass
import concourse.mybir as mybir
from concourse.bass_utils import run_bass_kernel_spmd

F32 = mybir.dt.float32
BF16 = mybir.dt.bfloat16
I32 = mybir.dt.int32
U32 = mybir.dt.uint32
AF = mybir.ActivationFunctionType
ALU = mybir.AluOpType
AX = mybir.AxisListType

NCORES = 8
D = 1024
SEQ = 2048
NT_P = 16
NT = 17
PROJ = 4096
SAME_ENGINE_SYNC = True


class Sched:
    EPOCH = 12000
    DEPOCH = 700

    def __init__(self, nc, stack):
        self.nc = nc
        self.stack = stack
        self.names = ['sp', 'act', 'dve', 'pool', 'pe']
        self.prog = {e: [] for e in self.names}
        self.cnt = {e: 0 for e in self.names}
        self.esems = {e: [] for e in self.names}
        self.dsems = {}
        self.dcnt = {}
        self.seen = {e: {} for e in self.names}
        self.last_w = {}
        self.readers = {}
        self.nsem = 0

    def _newsem(self, name):
        self.nsem += 1
        return self.stack.enter_context(self.nc.semaphore(name))

    def _esem(self, eng, n):
        ep = (n - 1) // self.EPOCH
        while len(self.esems[eng]) <= ep:
            self.esems[eng].append(self._newsem("e_%s_%d" % (eng, len(self.esems[eng]))))
        return self.esems[eng][ep], (n - 1) % self.EPOCH + 1

    def _dsem(self, key, n):
        ep = (n - 1) // self.DEPOCH
        lst = self.dsems.setdefault(key, [])
        while len(lst) <= ep:
            lst.append(self._newsem("d%d" % self.nsem))
        return lst[ep], ((n - 1) % self.DEPOCH + 1) * 16

    def _wait_for(self, consumer, tok, waits):
        kind, key, n = tok
        if kind == 'e':
            if key == consumer and not SAME_ENGINE_SYNC:
                return
            if key == consumer and consumer in ('pe', 'sp'):
                return
            if self.seen[consumer].get(('e', key), 0) >= n:
                return
            self.seen[consumer][('e', key)] = n
            waits.append(self._esem(key, n))
        else:
            n = self.dcnt[key]
            if self.seen[consumer].get(('d', key), 0) >= n:
                return
            self.seen[consumer][('d', key)] = n
            waits.append(self._dsem(key, n))

    def _deps(self, eng, reads, writes):
        waits = []
        for r in reads:
            t = self.last_w.get(r)
            if t is not None:
                self._wait_for(eng, t, waits)
        for w in writes:
            t = self.last_w.get(w)
            if t is not None:
                self._wait_for(eng, t, waits)
            for t in self.readers.get(w, ()):
                self._wait_for(eng, t, waits)
        return waits

    def _commit(self, tok, reads, writes):
        for r in reads:
            self.readers.setdefault(r, []).append(tok)
        for w in writes:
            self.last_w[w] = tok
            self.readers[w] = []

    def op(self, eng, fn, reads=(), writes=(), dma_key=None):
        waits = self._deps(eng, reads, writes)
        if dma_key is not None:
            self.dcnt[dma_key] = self.dcnt.get(dma_key, 0) + 1
            n = self.dcnt[dma_key]
            sem, _ = self._dsem(dma_key, n)
            self.prog[eng].append((waits, fn, (sem, None), 16))
            self._commit(('d', dma_key, n), reads, writes)
            return
        self.cnt[eng] += 1
        n = self.cnt[eng]
        tok = ('e', eng, n)
        self.prog[eng].append((waits, fn, self._esem(eng, n), 1))
        self._commit(tok, reads, writes)

    def dma(self, out, in_, reads=(), writes=(), key=None, eng='sp', **kw):
        if key is None:
            key = writes[0] if (writes and not str(writes[0]).startswith('dram')) else reads[0]
        waits = self._deps(eng, reads, writes)
        self.dcnt[key] = self.dcnt.get(key, 0) + 1
        n = self.dcnt[key]
        tok = ('d', key, n)
        sem, _ = self._dsem(key, n)
        self.prog[eng].append((waits, (lambda e, o=out, i=in_, k=kw: e.dma_start(out=o, in_=i, **k)), (sem, None), 16))
        self._commit(tok, reads, writes)

    def finish(self, eng='sp'):
        waits = []
        for key in list(self.dcnt):
            self._wait_for(eng, ('d', key, self.dcnt[key]), waits)
        self.prog[eng].append((waits, None, None, 0))

    def barrier(self):
        for e in self.names:
            waits = []
            for o in self.names:
                if o != e and self.cnt[o] > 0:
                    self._wait_for(e, ('e', o, self.cnt[o]), waits)
            for key in list(self.dcnt):
                self._wait_for(e, ('d', key, self.dcnt[key]), waits)
            self.prog[e].append((waits, None, None, 0))

    def replay(self, block):
        def mk(name):
            def run(e):
                for waits, fn, semv, inc in self.prog[name]:
                    for s, v in waits:
                        e.wait_ge(s, v)
                    if fn is None:
                        continue
                    ins = fn(e)
                    ins.then_inc(semv[0], inc)
            return run
        block.sync(mk('sp'))
        block.scalar(mk('act'))
        block.vector(mk('dve'))
        block.gpsimd(mk('pool'))
        block.tensor(mk('pe'))


import math
TWO_PI = 2.0 * math.pi


DN_ALPHA = 2.0 ** 0.25
LN_EPS = 1e-5


def layer_norm(S, src, dst, stats, mv, g_bc, b_bc, src_res, dst_res, gb_res):
    for c in range(2):
        S.op('dve', lambda e, c=c: e.bn_stats(out=stats[:, c, :], in_=src[:, c * 512:(c + 1) * 512]), reads=src_res, writes=[("lnstats", c)])
    S.op('dve', lambda e: e.bn_aggr(out=mv[:, 0:2], in_=stats[:]), reads=[("lnstats", 0), ("lnstats", 1)], writes=["lnmv"])
    S.op('dve', lambda e: e.tensor_scalar(out=mv[:, 2:3], in0=mv[:, 1:2], scalar1=LN_EPS, scalar2=None, op0=ALU.add), reads=["lnmv"], writes=["lnmv2"])
    S.op('act', lambda e: e.activation(out=mv[:, 2:3], in_=mv[:, 2:3], func=AF.Sqrt), reads=["lnmv2"], writes=["lnmv2"])
    S.op('dve', lambda e: e.reciprocal(out=mv[:, 3:4], in_=mv[:, 2:3]), reads=["lnmv2"], writes=["lnmv3"])
    S.op('dve', lambda e: e.tensor_scalar(out=dst[:], in0=src[:], scalar1=mv[:, 0:1], scalar2=mv[:, 3:4], op0=ALU.subtract, op1=ALU.mult),
         reads=src_res + ["lnmv", "lnmv3"], writes=[dst_res])
    S.op('dve', lambda e: e.tensor_tensor(out=dst[:], in0=dst[:], in1=g_bc[:], op=ALU.mult), reads=[dst_res] + gb_res, writes=[dst_res])
    S.op('dve', lambda e: e.tensor_tensor(out=dst[:], in0=dst[:], in1=b_bc[:], op=ALU.add), reads=[dst_res] + gb_res, writes=[dst_res])


def build(nc, debug=False, glimit=None, stop_after=3, p2tiles=NT):
    st = ExitStack()
    S = Sched(nc, st)

    def din(name, shape, dt=F32):
        return nc.dram_tensor(name, list(shape), dt, kind="ExternalInput").ap()

    def dout(name, shape, dt=F32):
        return nc.dram_tensor(name, list(shape), dt, kind="ExternalOutput").ap()

    def mk_sb(stack):
        return lambda name, shape, dt=F32: stack.enter_context(nc.sbuf_tensor(name, list(shape), dt))

    def mk_ps(stack):
        return lambda name, shape, dt=F32: stack.enter_context(nc.psum_tensor(name, list(shape), dt))

    sb = mk_sb(st)

    x_tm = din("x_tm", [NT, 128, D])
    x_fm = din("x_fm", [NT, 128, 8, 128])
    w_in = din("w_in", [D, PROJ])
    b_in_bc = din("b_in_bc", [128, PROJ])
    b_in_fm = din("b_in_fm", [128, 32])
    are_tm = din("are_tm", [128, 32, 64]); aim_tm = din("aim_tm", [128, 32, 64]); ldt_tm = din("ldt_tm", [128, 32])
    are_fm = din("are_fm", [128, 32]); aim_fm = din("aim_fm", [128, 32])
    are_bd = din("are_bd", [128, 4, 64]); aim_bd = din("aim_bd", [128, 4, 64]); ldt_bd = din("ldt_bd", [128, 4])
    bre_bd = din("bre_bd", [128, 4, 64]); bim_bd = din("bim_bd", [128, 4, 64]); maskbd = din("maskbd", [128, 4])
    c_fm = din("c_fm", [128, 32, 16]); d_fm = din("d_fm", [128, 4]); bglu_fm = din("bglu_fm", [128, 4])
    w_glu = din("w_glu", [512, 512])
    h0_fm = din("h0_fm", [128, 32, 16]); h0sw_fm = din("h0sw_fm", [128, 32, 16])
    cst = din("cst", [128, 5, 128])
    k_out = dout("k_out", [NT, 128, 512])
    v_out = dout("v_out", [NT, 128, 512])
    ssm_p = dout("ssm_p", [32, 128])
    ssm_s = dout("ssm_s", [16, 32, 128])
    if debug:
        dbg_y = dout("dbg_y", [128, 4, NT * 128], BF16)

    bfm = sb("bfm", [128, 32])
    cst_sb = sb("cst_sb", [128, 5, 128])
    ident = cst_sb[:, 0, :]
    psw = cst_sb[:, 1, :]
    sgn = cst_sb[:, 4, 0:1]
    s12 = ExitStack()
    yssmT = s12.enter_context(nc.sbuf_tensor("yssmT", [128, 4, NT * 128], BF16))
    qT32s = s12.enter_context(nc.sbuf_tensor("qT32s", [128, 4, 128], F32))
    qTbs = s12.enter_context(nc.sbuf_tensor("qTbs", [128, 4, 128], BF16))
    kTs_new = s12.enter_context(nc.sbuf_tensor("kTs_new", [128, 4, 128], BF16))
    Vs_new = s12.enter_context(nc.sbuf_tensor("Vs_new", [128, 8, 65], BF16))
    sgas = s12.enter_context(nc.sbuf_tensor("sgas", [128, 8, 128], BF16))
    sgbs = s12.enter_context(nc.sbuf_tensor("sgbs", [128, 8, 128], BF16))

    S.dma(bfm[:], b_in_fm, writes=["bfm"])
    S.dma(cst_sb[:], cst, writes=["cst"])

    w_in_r = w_in.rearrange("(k p) n -> p k n", p=128)

    with ExitStack() as s1:
        sb1 = mk_sb(s1)
        ps1 = mk_ps(s1)
        EmR = sb1("EmR", [128, 32, 64]); EmI = sb1("EmI", [128, 32, 64])
        EpR = sb1("EpR", [128, 32, 128]); EpI = sb1("EpI", [128, 32, 128])
        BD = sb1("BD", [128, 4, 4, 128], BF16)
        Cmat = sb1("Cmat", [128, 32, 16], BF16)
        Dd = sb1("Dd", [128, 4, 128], BF16)
        wglu_bf = sb1("wglu_bf", [128, 4, 512], BF16)
        bglu = sb1("bglu", [128, 4])
        tri_bf = sb1("tri_bf", [128, 2, 128], BF16)
        h0f = sb1("h0f", [128, 32, 16]); h0sw = sb1("h0sw", [128, 32, 16])
        S.dma(h0f[:], h0_fm, writes=["h0f"]); S.dma(h0sw[:], h0sw_fm, writes=["h0sw"])
        w_u = sb1("w_u", [128, 8, 512], BF16)
        S.dma(bglu[:], bglu_fm, writes=["bglu"])
        S.op('dve', lambda e: e.tensor_copy(out=tri_bf[:], in_=cst_sb[:, 2:4, :]), reads=["cst"], writes=["tri_bf"])

        with ExitStack() as sp:
            sbp = mk_sb(sp)
            T = [sbp("ptmp%d" % i, [128, 1024]) for i in range(9)]
            Ti = sbp("ptmpi", [128, 1024], I32)
            small = sbp("psmall", [128, 8, 32])
            jp1 = sbp("jp1", [128, 2])
            ip1 = sbp("ip1", [128, 128])

            def rs(name):
                return ("prep", name)
            PR = [rs("x")]

            def P(eng, fn):
                S.op(eng, fn, reads=PR + ["cst"], writes=PR)

            def PD(out, in_):
                S.dma(out, in_, reads=PR, writes=PR, key="prep_dma")

            def sincos(theta, n, out_sin, out_cos):
                for (off, dst) in ((math.pi + TWO_PI, out_sin), (1.5 * math.pi + TWO_PI, out_cos)):
                    a = T[7][:, 0:n]; kf = T[8][:, 0:n]; ki = Ti[:, 0:n]
                    P('dve', lambda e, a=a, off=off: e.tensor_scalar(out=a, in0=theta, scalar1=off, scalar2=None, op0=ALU.add))
                    P('dve', lambda e, a=a, ki=ki: e.tensor_scalar(out=ki, in0=a, scalar1=1.0 / TWO_PI, scalar2=None, op0=ALU.mult))
                    P('dve', lambda e, kf=kf, ki=ki: e.tensor_copy(out=kf, in_=ki))
                    P('dve', lambda e, a=a, kf=kf: e.scalar_tensor_tensor(out=a, in0=kf, scalar=-TWO_PI, in1=a, op0=ALU.mult, op1=ALU.add))
                    P('dve', lambda e, a=a, kf=kf: e.tensor_scalar(out=kf, in0=a, scalar1=TWO_PI, scalar2=-TWO_PI, op0=ALU.is_ge, op1=ALU.mult))
                    P('dve', lambda e, a=a, kf=kf: e.tensor_tensor(out=a, in0=a, in1=kf, op=ALU.add))
                    P('dve', lambda e, a=a, kf=kf: e.tensor_scalar(out=kf, in0=a, scalar1=0.0, scalar2=TWO_PI, op0=ALU.is_lt, op1=ALU.mult))
                    P('dve', lambda e, a=a, kf=kf: e.tensor_tensor(out=a, in0=a, in1=kf, op=ALU.add))
                    P('dve', lambda e, a=a: e.tensor_scalar(out=a, in0=a, scalar1=-math.pi, scalar2=None, op0=ALU.add))
                    P('act', lambda e, a=a, dst=dst: e.activation(out=dst, in_=a, func=AF.Sin))

            S.op('pool', lambda e: e.iota(jp1[:, 0:1], pattern=[[0, 1]], base=1, channel_multiplier=1, allow_small_or_imprecise_dtypes=True), writes=PR)
            S.op('pool', lambda e: e.iota(ip1[:], pattern=[[1, 128]], base=1, channel_multiplier=0, allow_small_or_imprecise_dtypes=True), reads=PR, writes=PR)
            P('dve', lambda e: e.tensor_scalar(out=jp1[:, 1:2], in0=jp1[:, 0:1], scalar1=-1.0, scalar2=None, op0=ALU.mult))
            ldt = small[:, 0, :]; dtt = small[:, 1, :]; arf = small[:, 2, :]; aif = small[:, 3, :]
            PD(ldt, ldt_tm)
            PD(arf, are_fm)
            PD(aif, aim_fm)
            tq = small[:, 4, :]
            P('dve', lambda e: e.tensor_scalar(out=tq, in0=ldt, scalar1=0.125, scalar2=None, op0=ALU.mult))
            P('dve', lambda e: e.tensor_scalar(out=dtt, in0=tq, scalar1=1.0 / 12.0, scalar2=1.0, op0=ALU.mult, op1=ALU.add))
            for n in range(11, 0, -1):
                P('dve', lambda e: e.tensor_tensor(out=dtt, in0=dtt, in1=tq, op=ALU.mult))
                P('dve', lambda e, n=n: e.tensor_scalar(out=dtt, in0=dtt, scalar1=1.0 / n, scalar2=1.0, op0=ALU.mult, op1=ALU.add))
            for _ in range(3):
                P('dve', lambda e: e.tensor_tensor(out=dtt, in0=dtt, in1=dtt, op=ALU.mult))
            P('dve', lambda e: e.tensor_tensor(out=arf, in0=arf, in1=dtt, op=ALU.mult))
            P('dve', lambda e: e.tensor_tensor(out=aif, in0=aif, in1=dtt, op=ALU.mult))

            for gc in range(4):
                gs = slice(8 * gc, 8 * gc + 8)
                v3 = lambda ap: ap.rearrange("p (g q) -> p g q", g=8)
                ar = T[0][:, 0:512]; ai = T[1][:, 0:512]; mg = T[2][:, 0:512]; sn = T[3][:, 0:512]; cs = T[4][:, 0:512]
                t4 = T[5][:, 0:512]; th = T[5][:, 512:1024]
                PD(v3(ar), are_tm[:, gs, :])
                PD(v3(ai), aim_tm[:, gs, :])
                dtb = dtt[:, gs].unsqueeze(2).to_broadcast([128, 8, 64])
                P('dve', lambda e, ar=ar, dtb=dtb: e.tensor_tensor(out=v3(ar), in0=v3(ar), in1=dtb, op=ALU.mult))
                P('dve', lambda e, ai=ai, dtb=dtb: e.tensor_tensor(out=v3(ai), in0=v3(ai), in1=dtb, op=ALU.mult))
                P('act', lambda e, mg=mg, ar=ar: e.activation(out=mg, in_=ar, func=AF.Exp, scale=jp1[:, 1:2]))
                P('dve', lambda e, th=th, ai=ai: e.tensor_scalar(out=th, in0=ai, scalar1=jp1[:, 0:1], scalar2=None, op0=ALU.mult))
                sincos(th, 512, sn, cs)
                P('dve', lambda e, gs=gs, cs=cs, mg=mg: e.tensor_tensor(out=EmR[:, gs, :], in0=v3(cs), in1=v3(mg), op=ALU.mult))
                P('dve', lambda e, gs=gs, sn=sn, mg=mg: e.scalar_tensor_tensor(out=EmI[:, gs, :], in0=v3(sn), scalar=-1.0, in1=v3(mg), op0=ALU.mult, op1=ALU.mult))
                v3f = lambda ap: ap.rearrange("p (g i) -> p g i", g=8)
                ipb = ip1[:].unsqueeze(1).to_broadcast([128, 8, 128])
                P('dve', lambda e, gs=gs, ipb=ipb: e.tensor_tensor(out=v3f(T[0][:]), in0=arf[:, gs].unsqueeze(2).to_broadcast([128, 8, 128]), in1=ipb, op=ALU.mult))
                P('dve', lambda e, gs=gs, ipb=ipb: e.tensor_tensor(out=v3f(T[1][:]), in0=aif[:, gs].unsqueeze(2).to_broadcast([128, 8, 128]), in1=ipb, op=ALU.mult))
                P('act', lambda e: e.activation(out=T[0][:], in_=T[0][:], func=AF.Exp))
                sincos(T[1][:], 1024, T[2][:], T[3][:])
                P('dve', lambda e, gs=gs: e.tensor_tensor(out=EpR[:, gs, :], in0=v3f(T[3][:]), in1=v3f(T[0][:]), op=ALU.mult))
                P('dve', lambda e, gs=gs: e.scalar_tensor_tensor(out=EpI[:, gs, :], in0=v3f(T[2][:]), scalar=sgn, in1=v3f(T[0][:]), op0=ALU.mult, op1=ALU.mult))

            q3 = lambda t, o=0: t[:, o:o + 256].rearrange("p (q r) -> p q r", q=4)
            ab = q3(T[0]); ai_ = q3(T[0], 256); br = q3(T[0], 512); bi = q3(T[0], 768)
            ldb = small[:, 4, 0:4]; dtbd = small[:, 5, 0:4]; mk = small[:, 6, 0:4]; dsk = small[:, 7, 0:4]
            PD(ab, are_bd)
            PD(ai_, aim_bd)
            PD(ldb, ldt_bd)
            PD(br, bre_bd)
            PD(bi, bim_bd)
            PD(mk, maskbd)
            PD(dsk, d_fm)
            P('act', lambda e: e.activation(out=dtbd, in_=ldb, func=AF.Exp))
            dtq = dtbd.unsqueeze(2).to_broadcast([128, 4, 64])
            dar = q3(T[1]); dai = q3(T[1], 256); mg = q3(T[1], 512)
            P('dve', lambda e: e.tensor_tensor(out=dar, in0=ab, in1=dtq, op=ALU.mult))
            P('dve', lambda e: e.tensor_tensor(out=dai, in0=ai_, in1=dtq, op=ALU.mult))
            P('act', lambda e: e.activation(out=mg, in_=dar, func=AF.Exp))
            sincos(T[1][:, 256:512], 256, T[2][:, 0:256], T[2][:, 256:512])
            ni = q3(T[2], 0); nr = q3(T[2], 256)
            P('dve', lambda e: e.tensor_tensor(out=nr, in0=nr, in1=mg, op=ALU.mult))
            P('dve', lambda e: e.tensor_scalar(out=nr, in0=nr, scalar1=-1.0, scalar2=None, op0=ALU.add))
            P('dve', lambda e: e.tensor_tensor(out=ni, in0=ni, in1=mg, op=ALU.mult))
            den = q3(T[2], 512); t1 = q3(T[2], 768); t2 = q3(T[3], 0); fr = q3(T[3], 256); fi = q3(T[3], 512)
            P('dve', lambda e: e.tensor_tensor(out=den, in0=ab, in1=ab, op=ALU.mult))
            P('dve', lambda e: e.tensor_tensor(out=t1, in0=ai_, in1=ai_, op=ALU.mult))
            P('dve', lambda e: e.tensor_tensor(out=den, in0=den, in1=t1, op=ALU.add))
            P('dve', lambda e: e.reciprocal(out=den, in_=den))
            P('dve', lambda e: e.tensor_tensor(out=t1, in0=nr, in1=ab, op=ALU.mult))
            P('dve', lambda e: e.tensor_tensor(out=t2, in0=ni, in1=ai_, op=ALU.mult))
            P('dve', lambda e: e.tensor_tensor(out=t1, in0=t1, in1=t2, op=ALU.add))
            P('dve', lambda e: e.tensor_tensor(out=fr, in0=t1, in1=den, op=ALU.mult))
            P('dve', lambda e: e.tensor_tensor(out=t1, in0=ni, in1=ab, op=ALU.mult))
            P('dve', lambda e: e.tensor_tensor(out=t2, in0=nr, in1=ai_, op=ALU.mult))
            P('dve', lambda e: e.tensor_tensor(out=t1, in0=t1, in1=t2, op=ALU.subtract))
            P('dve', lambda e: e.tensor_tensor(out=fi, in0=t1, in1=den, op=ALU.mult))
            bfull = T[4][:, 0:512].rearrange("p (q h r) -> p q h r", q=4, h=2)
            P('dve', lambda e: e.tensor_tensor(out=t1, in0=fr, in1=br, op=ALU.mult))
            P('dve', lambda e: e.tensor_tensor(out=t2, in0=fi, in1=bi, op=ALU.mult))
            P('dve', lambda e: e.tensor_tensor(out=bfull[:, :, 0, :], in0=t1, in1=t2, op=ALU.subtract))
            P('dve', lambda e: e.tensor_tensor(out=t1, in0=fr, in1=bi, op=ALU.mult))
            P('dve', lambda e: e.tensor_tensor(out=t2, in0=fi, in1=br, op=ALU.mult))
            P('dve', lambda e: e.tensor_tensor(out=bfull[:, :, 1, :], in0=t1, in1=t2, op=ALU.add))
            bfl = T[4][:, 0:512].rearrange("p (q r) -> p q r", q=4)
            for gl4 in range(4):
                P('dve', lambda e, gl4=gl4: e.tensor_scalar(out=BD[:, :, gl4, :], in0=bfl, scalar1=mk[:, gl4:gl4 + 1], scalar2=None, op0=ALU.mult))
            cst32 = T[5][:, 0:512].rearrange("p (g c) -> p g c", g=32)
            PD(cst32, c_fm)
            P('dve', lambda e: e.tensor_copy(out=Cmat[0:64], in_=cst32[0:64]))
            P('dve', lambda e: e.tensor_scalar(out=Cmat[64:128], in0=cst32[64:128], scalar1=-1.0, scalar2=None, op0=ALU.mult))
            for q in range(4):
                P('dve', lambda e, q=q: e.tensor_scalar(out=Dd[:, q, :], in0=ident, scalar1=dsk[:, q:q + 1], scalar2=None, op0=ALU.mult))
            w_glu_r = w_glu.rearrange("(k p) n -> p k n", p=128)
            for half in range(2):
                wg32 = T[6][:, 0:1024].rearrange("p (k n) -> p k n", k=2)
                PD(wg32, w_glu_r[:, 2 * half:2 * half + 2, :])
                P('dve', lambda e, half=half, wg32=wg32: e.tensor_copy(out=wglu_bf[:, 2 * half:2 * half + 2, :], in_=wg32))
            for kh in range(4):
                wu32 = T[kh % 2][:, 0:1024].rearrange("p (k n) -> p k n", k=2)
                PD(wu32, w_in_r[:, 2 * kh:2 * kh + 2, 1536:2048])
                P('pool', lambda e, kh=kh, wu32=wu32: e.tensor_copy(out=w_u[:, 2 * kh:2 * kh + 2, :], in_=wu32))
            S.op('dve', lambda e: e.engine_nop(), reads=PR, writes=["s5tab"])
        S.barrier()
        if debug:
            dbg_em = dout("dbg_em", [2, 128, 32, 64]); dbg_ep = dout("dbg_ep", [2, 128, 32, 128])
            S.dma(dbg_em[0], EmR[:], reads=["s5tab"], writes=["dram_dbg1"], key="dbgk")
            S.dma(dbg_em[1], EmI[:], reads=["s5tab"], writes=["dram_dbg2"], key="dbgk")
            S.dma(dbg_ep[0], EpR[:], reads=["s5tab"], writes=["dram_dbg3"], key="dbgk")
            S.dma(dbg_ep[1], EpI[:], reads=["s5tab"], writes=["dram_dbg4"], key="dbgk")

        xT32 = [sb1("xT32_%d" % i, [128, 8, 128], F32) for i in range(2)]
        xT = [sb1("xT_%d" % i, [128, 8, 128], BF16) for i in range(2)]
        uT = [sb1("uT_%d" % i, [128, 4, 128], BF16) for i in range(2)]
        Xp = sb1("Xp", [128, 32, 192], BF16)
        zh = sb1("zh", [128, 4, 16, 8]); zh2 = sb1("zh2", [128, 4, 16, 8]); tmpz = sb1("tmpz", [128, 4, 128])
        tA = [sb1("tA%d" % i, [128, 2, 2, 64]) for i in range(2)]
        tB = [sb1("tB%d" % i, [128, 2, 2, 64]) for i in range(2)]
        T1 = [sb1("T1_%d" % i, [128, 4, 128]) for i in range(2)]
        T2 = [sb1("T2_%d" % i, [128, 4, 128]) for i in range(2)]
        ZT = sb1("ZT", [128, 32, 128], BF16)
        hc1 = [sb1("hc1_%d" % i, [128, 32]) for i in range(2)]
        hc2 = [sb1("hc2_%d" % i, [128, 32]) for i in range(2)]
        hs = sb1("hs", [128, 32, 16])
        hsT = sb1("hsT", [16, 2, 4, 128])
        hpT = sb1("hpT", [32, 128])
        z_sb = sb1("z_sb", [128, 512])
        zT_bf = sb1("zT_bf", [128, 4, 128], BF16)
        sg = sb1("sg", [128, 4, 128])
        pA = ps1("pA", [128, 4, 128])
        pXl = [ps1("pX%d" % i, [128, 256]) for i in range(2)]
        pW1 = ps1("pW1", [128, 4, 128]); pW2 = ps1("pW2", [128, 4, 128])
        py = ps1("py", [128, 512])
        pzT = ps1("pzT", [128, 4, 128])
        phc = ps1("phc", [128, 512])

        S.op('pool', lambda e: e.memset(hc1[0][:], 0.0), writes=[("hc1", 0)])
        S.op('pool', lambda e: e.memset(hc2[0][:], 0.0), writes=[("hc2", 0)])

        for t in range(NT):
            b = t % 2
            sample = (t == NT - 1)
            hb = 0 if (sample or t == 0) else (t % 2)
            if sample:
                S.op('pool', lambda e: e.memset(hc1[0][:], 0.0), writes=[("hc1", 0)])
                S.op('pool', lambda e: e.memset(hc2[0][:], 0.0), writes=[("hc2", 0)])
            hn = 1 - hb
            S.dma(xT32[b][:], x_fm[t], writes=[("xT32", b)])
            S.op('pool', lambda e, b=b: e.tensor_copy(out=xT[b][:], in_=xT32[b][:]),
                 reads=[("xT32", b)], writes=[("xT", b)])
            for q in range(4):
                for kk in range(8):
                    S.op('pe', lambda e, b=b, q=q, kk=kk: e.matmul(
                        pA[:, q, :], lhsT=w_u[:, kk, 128 * q:128 * (q + 1)], rhs=xT[b][:, kk, :],
                        start=(kk == 0), stop=(kk == 7)),
                        reads=[("xT", b), "s5tab"], writes=["pA"])
                S.op('act', lambda e, b=b, q=q: e.activation(out=uT[b][:, q, :], in_=pA[:, q, :], func=AF.Identity,
                                                             bias=bfm[:, 12 + q:13 + q]),
                     reads=["pA", "bfm"], writes=[("uT", b, q)])
            for hcx in range(16):
                g0 = 2 * hcx
                q = g0 // 8
                base = 64 * ((g0 % 8) // 4)
                gl4 = g0 % 4
                xb = hcx % 2
                S.op('pe', lambda e, b=b, q=q, base=base, gl4=gl4, xb=xb: e.matmul(
                    pXl[xb][:], lhsT=uT[b][base:base + 64, q, :],
                    rhs=BD[base:base + 64, q, gl4:gl4 + 2, :].rearrange("p a r -> p (a r)"), start=True, stop=True),
                    reads=[("uT", b, q), "s5tab"], writes=[("pX", xb)])
                pxv = pXl[xb][:].rearrange("p (g h r) -> p g h r", g=2, h=2)
                src = pxv
                srcres = ("pX", xb)
                emr = EmR[:, g0:g0 + 2, :].unsqueeze(2).to_broadcast([128, 2, 2, 64])
                emi = EmI[:, g0:g0 + 2, :].unsqueeze(2).to_broadcast([128, 2, 2, 64])
                S.op('dve', lambda e, xb=xb, src=src, emr=emr: e.tensor_tensor(out=tA[xb][:], in0=src, in1=emr, op=ALU.mult),
                     reads=[srcres, "s5tab"], writes=[("tA", xb)])
                S.op('dve', lambda e, xb=xb, src=src, emi=emi: e.tensor_tensor(out=tB[xb][:], in0=src, in1=emi, op=ALU.mult),
                     reads=[srcres, "s5tab"], writes=[("tB", xb)])
                S.op('dve', lambda e, xb=xb, g0=g0: e.tensor_tensor(out=Xp[:, g0:g0 + 2, 0:64], in0=tA[xb][:, :, 0, :], in1=tB[xb][:, :, 1, :], op=ALU.subtract),
                     reads=[("tA", xb), ("tB", xb)], writes=[("Xp", g0, 0)])
                S.op('dve', lambda e, xb=xb, g0=g0: e.tensor_tensor(out=Xp[:, g0:g0 + 2, 64:128], in0=tA[xb][:, :, 1, :], in1=tB[xb][:, :, 0, :], op=ALU.add),
                     reads=[("tA", xb), ("tB", xb)], writes=[("Xp", g0, 1)])
                S.op('pool', lambda e, g0=g0: e.tensor_copy(out=Xp[:, g0:g0 + 2, 128:192], in_=Xp[:, g0:g0 + 2, 0:64]),
                     reads=[("Xp", g0, 0)], writes=[("Xp", g0, 2)])
            tri = tri_bf[:, 1 if sample else 0, :]
            for gq in range(8):
                tb = gq % 2
                for gl in range(4):
                    g = 4 * gq + gl
                    g0 = (g // 2) * 2
                    S.op('pe', lambda e, g=g, gl=gl, tri=tri: e.matmul(pW1[:, gl, :], lhsT=Xp[:, g, 0:128], rhs=tri, start=True, stop=True),
                         reads=[("Xp", g0, 0), ("Xp", g0, 1), "tri_bf"], writes=["pW1"])
                    S.op('pe', lambda e, g=g, gl=gl, tri=tri: e.matmul(pW2[:, gl, :], lhsT=Xp[:, g, 64:192], rhs=tri, start=True, stop=True),
                         reads=[("Xp", g0, 1), ("Xp", g0, 2), "tri_bf"], writes=["pW2"])
                for gl in range(4):
                    g = 4 * gq + gl
                    S.op('dve', lambda e, g=g, gl=gl, tb=tb, hb=hb: e.scalar_tensor_tensor(
                        out=T1[tb][:, gl, :], in0=pW1[:, gl, :], scalar=hc1[hb][:, g:g + 1], in1=EpR[:, g, :], op0=ALU.add, op1=ALU.mult),
                        reads=["pW1", ("hc1", hb), "s5tab"], writes=[("T1", tb, gl)])
                    S.op('dve', lambda e, g=g, gl=gl, tb=tb, hb=hb: e.scalar_tensor_tensor(
                        out=T2[tb][:, gl, :], in0=pW2[:, gl, :], scalar=hc2[hb][:, g:g + 1], in1=EpI[:, g, :], op0=ALU.add, op1=ALU.mult),
                        reads=["pW2", ("hc2", hb), "s5tab"], writes=[("T2", tb, gl)])
                rT = [("T1", tb, gl) for gl in range(4)] + [("T2", tb, gl) for gl in range(4)]
                if sample:
                    gsl = slice(4 * gq, 4 * gq + 4)
                    S.op('dve', lambda e, gsl=gsl: e.tensor_tensor(out=zh[:], in0=EpR[:, gsl, 0:8].unsqueeze(2).to_broadcast([128, 4, 16, 8]),
                                                                   in1=h0f[:, gsl, :].unsqueeze(3).to_broadcast([128, 4, 16, 8]), op=ALU.mult),
                         reads=["s5tab", "h0f"], writes=["zh"])
                    S.op('dve', lambda e, gsl=gsl: e.tensor_tensor(out=zh2[:], in0=EpI[:, gsl, 0:8].unsqueeze(2).to_broadcast([128, 4, 16, 8]),
                                                                   in1=h0sw[:, gsl, :].unsqueeze(3).to_broadcast([128, 4, 16, 8]), op=ALU.mult),
                         reads=["s5tab", "h0sw"], writes=["zh2"])
                    S.op('pool', lambda e, tb=tb: e.tensor_tensor(out=tmpz[:], in0=T1[tb][:], in1=T2[tb][:], op=ALU.add), reads=rT, writes=["tmpz"])
                    S.op('pool', lambda e: e.tensor_tensor(out=tmpz[:], in0=tmpz[:], in1=zh[:].rearrange("p g a b -> p g (a b)"), op=ALU.add), reads=["tmpz", "zh"], writes=["tmpz"])
                    S.op('pool', lambda e: e.tensor_tensor(out=tmpz[:], in0=tmpz[:], in1=zh2[:].rearrange("p g a b -> p g (a b)"), op=ALU.add), reads=["tmpz", "zh2"], writes=["tmpz"])
                    S.op('pool', lambda e, gq=gq: e.tensor_copy(out=ZT[:, 4 * gq:4 * gq + 4, :], in_=tmpz[:]), reads=["tmpz"], writes=[("ZT", gq)])
                    S.op('pool', lambda e, gq=gq: e.tensor_copy(out=hs[:, 4 * gq:4 * gq + 4, :], in_=tmpz[:, :, 7::8]), reads=["tmpz"], writes=[("hs", gq)])
                else:
                    S.op('pool', lambda e, gq=gq, tb=tb: e.tensor_tensor(out=ZT[:, 4 * gq:4 * gq + 4, :], in0=T1[tb][:], in1=T2[tb][:], op=ALU.add),
                         reads=rT, writes=[("ZT", gq)])
                    S.op('pool', lambda e, gq=gq, tb=tb, hn=hn: e.tensor_tensor(out=hc1[hn][:, 4 * gq:4 * gq + 4], in0=T1[tb][:, :, 127], in1=T2[tb][:, :, 127], op=ALU.add),
                         reads=rT, writes=[("hc1", hn, gq)])
            if not sample:
                S.op('pe', lambda e, hn=hn: e.matmul(phc[:, 0:32], lhsT=psw, rhs=hc1[hn][:], start=True, stop=True),
                     reads=[("hc1", hn, gq) for gq in range(8)] + ["cst"], writes=["phc"])
                S.op('dve', lambda e, hn=hn: e.tensor_copy(out=hc2[hn][:], in_=phc[:, 0:32]), reads=["phc"], writes=[("hc2", hn)])
                S.op('dve', lambda e, hn=hn: e.engine_nop(), reads=[("hc1", hn, gq) for gq in range(8)], writes=[("hc1", hn)])
            if debug and t == 0:
                dbg_xp = dout("dbg_xp", [128, 32, 192], BF16); dbg_zt = dout("dbg_zt", [128, 32, 128], BF16)
                S.dma(dbg_xp, Xp[:], reads=[("Xp", g0, k) for g0 in range(0, 32, 2) for k in range(3)], writes=["dram_dbg5"], key="dbgk")
                S.dma(dbg_zt, ZT[:], reads=[("ZT", gq) for gq in range(8)], writes=["dram_dbg6"], key="dbgk")
            for q in range(4):
                S.op('pe', lambda e, b=b, q=q: e.matmul(py[:, q * 128:(q + 1) * 128], lhsT=uT[b][:, q, :], rhs=Dd[:, q, :], start=(q == 0), stop=False),
                     reads=[("uT", b, q), "s5tab"], writes=["py"])
            for g in range(32):
                S.op('pe', lambda e, g=g: e.matmul(py[:, g * 16:(g + 1) * 16], lhsT=ZT[:, g, :], rhs=Cmat[:, g, :], start=False, stop=(g == 31)),
                     reads=[("ZT", g // 4), "s5tab"], writes=["py"])
            S.op('act', lambda e: e.activation(out=z_sb[:], in_=py[:], func=AF.Gelu_apprx_tanh), reads=["py"], writes=["z_sb"])
            for q in range(4):
                S.op('pe', lambda e, q=q: e.transpose(pzT[:, q, :], z_sb[:, q * 128:(q + 1) * 128], ident), reads=["z_sb", "cst"], writes=["pzT"])
            S.op('act', lambda e: e.activation(out=zT_bf[:], in_=pzT[:], func=AF.Copy), reads=["pzT"], writes=["zT_bf"])
            for fc in range(4):
                for kc in range(4):
                    S.op('pe', lambda e, fc=fc, kc=kc: e.matmul(pA[:, fc, :], lhsT=wglu_bf[:, kc, fc * 128:(fc + 1) * 128], rhs=zT_bf[:, kc, :],
                                                               start=(kc == 0), stop=(kc == 3)),
                         reads=["zT_bf", "s5tab"], writes=["pA"])
                S.op('act', lambda e, fc=fc: e.activation(out=sg[:, fc, :], in_=pA[:, fc, :], func=AF.Sigmoid, bias=bglu[:, fc:fc + 1]),
                     reads=["pA", "bglu"], writes=[("sg", fc)])
            S.op('dve', lambda e, t=t: e.tensor_tensor(out=yssmT[:, :, t * 128:(t + 1) * 128], in0=pzT[:], in1=sg[:], op=ALU.mult),
                 reads=["pzT"] + [("sg", fc) for fc in range(4)], writes=[("yssmT", t)])
            if t == NT_P - 1:
                S.op('pe', lambda e, hn=hn: e.transpose(phc[0:32, 128:256], hc1[hn][:], ident), reads=[("hc1", hn), "cst"], writes=["phc"])
                S.op('dve', lambda e: e.tensor_copy(out=hpT[:], in_=phc[0:32, 128:256]), reads=["phc"], writes=["hpT"])
                S.dma(ssm_p, hpT[:], reads=["hpT"], writes=["dram_ssm_p"])
            if sample:
                for gq in range(8):
                    for gl in range(4):
                        g = 4 * gq + gl
                        S.op('pe', lambda e, g=g, gl=gl: e.transpose(phc[0:16, gl * 128:(gl + 1) * 128], hs[:, g, :], ident),
                             reads=[("hs", gq), "cst"], writes=["phc"])
                    S.op('dve', lambda e, gq=gq: e.tensor_copy(out=hsT[:, gq % 2, :, :], in_=phc[0:16, :].rearrange("p (g r) -> p g r", g=4)),
                         reads=["phc"], writes=[("hsT", gq % 2)])
                    S.dma(ssm_s[:, 4 * gq:4 * gq + 4, :], hsT[:, gq % 2, :, :], reads=[("hsT", gq % 2)], writes=[("dram_ssm_s", gq)])
        if debug:
            S.dma(dbg_y, yssmT[:], reads=[("yssmT", t) for t in range(NT)], writes=["dram_dbg_y"])


    S.barrier()
    if stop_after < 2:
        S.finish('sp')
        with nc.Block() as block:
            S.replay(block)
        s12.close()
        st.close()
        return nc
    x1_d = nc.dram_tensor("x1_scratch", [NT, 128, D], F32, kind="Internal").ap() if not debug else dout("x1_scratch", [NT, 128, D])
    w_a = din("w_a", [512, D]); w_b = din("w_b", [512, D]); w_o = din("w_o", [D, D])
    ln1g_bc = din("ln1g_bc", [128, D]); ln1b_bc = din("ln1b_bc", [128, D])
    acst = din("acst", [128, 8 + 128 + 128])
    ltab = din("ltab", [9, 9, 128])
    BIGM = 30000.0
    with ExitStack() as s2:
        sb2 = mk_sb(s2)
        ps2 = mk_ps(s2)
        wq = sb2("wq", [128, 8, 3584], BF16)
        kT_all = sb2("kT_all", [128, 4, SEQ], BF16)
        V_all = sb2("V_all", [128, NT_P, 8, 65], BF16)
        wa_bf = sb2("wa_bf", [128, 4, D], BF16); wb_bf = sb2("wb_bf", [128, 4, D], BF16); wo_bf = sb2("wo_bf", [128, 8, D], BF16)
        lng = sb2("lng", [128, D]); lnb = sb2("lnb", [128, D])
        bkv = sb2("bkv", [128, 1024])
        acs = sb2("acs", [128, 264])
        slq = acs[:, 0:8]; bexp = acs[:, 8:136].rearrange("p (h d) -> p h d", h=8)
        ltab_bf = sb2("ltab_bf", [9, 9, 128], BF16)
        causal_bf = sb2("causal_bf", [128, 128], BF16)
        ident_bf = sb2("ident_bf", [128, 128], BF16)
        ksum = sb2("ksum", [128, NT_P, 4])
        kmT = sb2("kmT", [128, 4, 8])
        kmTz = sb2("kmTz", [128, 8, 8])
        Mq = sb2("Mq", [128, 8, 9])
        with ExitStack() as sl:
            sbl = mk_sb(sl)
            stg = [sbl("stg%d" % i, [128, 8, 256]) for i in range(2)]
            lt32 = sbl("lt32", [9, 9, 128])
            ns = [0]

            def load_cast(dst_ap_fn, src_ap, nk, ncol, res):
                b = ns[0] % 2
                ns[0] += 1
                S.dma(stg[b][:, 0:nk, 0:ncol], src_ap, writes=[("stg", b)])
                S.op('pool', lambda e, b=b: e.tensor_copy(out=dst_ap_fn(), in_=stg[b][:, 0:nk, 0:ncol]), reads=[("stg", b)], writes=[res])
            for c in range(14):
                src_c = c * 256 if c < 6 else 2048 + (c - 6) * 256
                load_cast(lambda c=c: wq[:, :, c * 256:(c + 1) * 256], w_in_r[:, :, src_c:src_c + 256], 8, 256, "wq")
            war = w_a.rearrange("(k p) n -> p k n", p=128); wbr = w_b.rearrange("(k p) n -> p k n", p=128)
            wor = w_o.rearrange("(k p) n -> p k n", p=128)
            for c in range(4):
                load_cast(lambda c=c: wa_bf[:, :, c * 256:(c + 1) * 256], war[:, :, c * 256:(c + 1) * 256], 4, 256, "wa")
                load_cast(lambda c=c: wb_bf[:, :, c * 256:(c + 1) * 256], wbr[:, :, c * 256:(c + 1) * 256], 4, 256, "wb")
                load_cast(lambda c=c: wo_bf[:, :, c * 256:(c + 1) * 256], wor[:, :, c * 256:(c + 1) * 256], 8, 256, "wo")
            S.dma(lng[:], ln1g_bc, writes=["lng"]); S.dma(lnb[:], ln1b_bc, writes=["lnb"])
            S.dma(bkv[:], b_in_bc[:, 512:1536], writes=["bkv"])
            S.dma(acs[:], acst, writes=["acs"])
            S.dma(lt32[:], ltab, writes=["lt32"])
            S.op('dve', lambda e: e.tensor_copy(out=ltab_bf[:], in_=lt32[:]), reads=["lt32"], writes=["ltab_bf"])
            S.op('dve', lambda e: e.tensor_copy(out=causal_bf[:], in_=acs[:, 136:264]), reads=["acs"], writes=["causal_bf"])
            S.op('dve', lambda e: e.tensor_copy(out=ident_bf[:], in_=ident), reads=["cst"], writes=["ident_bf"])
            S.op('pool', lambda e: e.memset(V_all[:, :, :, 64:65], 1.0), writes=["V_ones"])
            S.op('pool', lambda e: e.memset(kmT[:], 0.0), writes=["kmT"])
            S.op('pool', lambda e: e.memset(kmTz[:], 0.0), writes=["kmTz"])
            S.op('pool', lambda e: e.memset(Mq[:, :, 0:8], 0.0), writes=["Mq"])
            S.op('dve', lambda e: e.tensor_copy(out=Mq[:, :, 8], in_=slq), reads=["acs", "Mq"], writes=["Mq"])
        S.barrier()

        xU32 = [sb2("p2xT32_0", [128, 8, 128], F32)] * 2
        xU = [sb2("p2xT_0", [128, 8, 128], BF16)] * 2
        xtm = [sb2("xtm0", [128, D])] * 2
        qT32 = sb2("qT32", [128, 4, 128]); qTb = sb2("qTb", [128, 4, 128], BF16)
        kv_sb = sb2("kv_sb", [128, 1024])
        sga = sb2("sga", [128, 8, 128], BF16); sgb = sb2("sgb", [128, 8, 128], BF16)
        gm = sb2("gm", [128, 8, 8]); mx = sb2("mx", [128, 8, 8])
        R_bf = sb2("R_bf", [9, 8, 128], BF16)
        PT = [sb2("PT%d" % i, [128, 4, 128], BF16) for i in range(2)]
        rsum = sb2("rsum", [128, 8])
        yatt = sb2("yatt", [128, 8, 64])
        yattT = sb2("yattT", [128, 4, 128], BF16)
        mtmp = sb2("mtmp", [128, 4, 128])
        mergedT = sb2("mergedT", [128, 8, 128], BF16)
        pre = sb2("pre", [128, D]); x1 = pre
        stats = sb2("stats", [128, 2, 6]); mv = sb2("mv", [128, 4])
        pFA = ps2("pFA", [128, 4, 128]); pFB = ps2("pFB", [128, 4, 128])
        pT0 = ps2("pT0", [128, 512]); pT1 = ps2("pT1", [128, 512])
        pS = [ps2("pS%d" % i, [128, 4, 128]) for i in range(2)]
        pO = [ps2("pO%d" % i, [128, 4, 65]) for i in range(2)]
        sctr = [0]

        for t in range(p2tiles):
            b = t % 2
            sample = (t == NT - 1)
            cur = t // 2
            S.dma(xU32[b][:], x_fm[t], writes=[("p2xT32", 0)])
            S.dma(xtm[b][:], x_tm[t], writes=[("xtm", 0)])
            S.op('pool', lambda e, b=b: e.tensor_copy(out=xU[b][:], in_=xU32[b][:]), reads=[("p2xT32", 0)], writes=[("p2xT", 0)])
            for (pb, pbn, col0, which) in ((pFA, "pFA", 0, 'q'), (pFB, "pFB", 512, 'k')):
                for q in range(4):
                    for kk in range(8):
                        S.op('pe', lambda e, b=b, q=q, kk=kk, pb=pb, col0=col0: e.matmul(
                            pb[:, q, :], lhsT=wq[:, kk, col0 + 128 * q:col0 + 128 * (q + 1)], rhs=xU[b][:, kk, :],
                            start=(kk == 0), stop=(kk == 7)), reads=[("p2xT", 0), "wq"], writes=[pbn])
                for q in range(4):
                    if which == 'q':
                        qdst = qT32s if sample else qT32
                        S.op('act', lambda e, q=q, qdst=qdst: e.activation(out=qdst[:, q, :], in_=pFA[:, q, :], func=AF.Identity, bias=bfm[:, q:q + 1]),
                             reads=["pFA", "bfm"], writes=["qT32"])
                    elif sample:
                        S.op('act', lambda e, q=q: e.activation(out=kTs_new[:, q, :], in_=pFB[:, q, :], func=AF.Identity, bias=bfm[:, 4 + q:5 + q]),
                             reads=["pFB", "bfm"], writes=["kTs_new"])
                    else:
                        S.op('act', lambda e, q=q, t=t: e.activation(out=kT_all[:, q, t * 128:(t + 1) * 128], in_=pFB[:, q, :], func=AF.Identity,
                                                                   bias=bfm[:, 4 + q:5 + q], accum_out=ksum[:, t, q:q + 1]),
                             reads=["pFB", "bfm"], writes=[("kT", t), ("ksum", t)])
                if which == 'q':
                    if sample:
                        S.op('pool', lambda e: e.tensor_copy(out=qTbs[:], in_=qT32s[:]), reads=["qT32"], writes=["qTb"])
                    else:
                        S.op('pool', lambda e: e.tensor_copy(out=qTb[:], in_=qT32[:]), reads=["qT32"], writes=["qTb"])
            if (not sample) and t % 2 == 1:
                n = t // 2
                S.op('dve', lambda e, t=t, n=n: e.tensor_tensor(out=kmT[:, :, n], in0=ksum[:, t - 1, :], in1=ksum[:, t, :], op=ALU.add),
                     reads=[("ksum", t - 1), ("ksum", t), "kmT"], writes=["kmT"])
                S.op('dve', lambda e, n=n: e.tensor_scalar(out=kmT[:, :, n], in0=kmT[:, :, n], scalar1=1.0 / 256.0, scalar2=None, op0=ALU.mult),
                     reads=["kmT"], writes=["kmT"])
                kz = kmTz[:].rearrange("p (c two) n -> p c two n", two=2)
                S.op('dve', lambda e, n=n, kz=kz: e.tensor_copy(out=kz[0:64, :, 0, n], in_=kmT[0:64, :, n]), reads=["kmT", "kmTz"], writes=["kmTz"])
                S.op('dve', lambda e, n=n, kz=kz: e.tensor_copy(out=kz[64:128, :, 1, n], in_=kmT[64:128, :, n]), reads=["kmT", "kmTz"], writes=["kmTz"])
            for j, (pt, ptn) in enumerate(((pT0, "pT0"), (pT1, "pT1"))):
                c0 = 512 + 512 * j
                for kk in range(8):
                    S.op('pe', lambda e, b=b, kk=kk, pt=pt, c0=c0: e.matmul(pt[:], lhsT=xU[b][:, kk, :], rhs=wq[:, kk, c0:c0 + 512],
                                                                       start=(kk == 0), stop=(kk == 7)),
                         reads=[("p2xT", 0), "wq"], writes=[ptn])
                S.op('dve', lambda e, j=j, pt=pt: e.tensor_tensor(out=kv_sb[:, j * 512:(j + 1) * 512], in0=pt[:], in1=bkv[:, j * 512:(j + 1) * 512], op=ALU.add),
                     reads=[ptn, "bkv"], writes=[("kv_sb", j)])
            S.dma(k_out[t], kv_sb[:, 0:512], reads=[("kv_sb", 0)], writes=[("dram_k", t)], key="kv_sb")
            S.dma(v_out[t], kv_sb[:, 512:1024], reads=[("kv_sb", 1)], writes=[("dram_v", t)], key="kv_sb")
            if not sample:
                S.op('pool', lambda e, t=t: e.tensor_copy(out=V_all[:, t, :, 0:64], in_=kv_sb[:, 512:1024].rearrange("p (h d) -> p h d", h=8)),
                     reads=[("kv_sb", 1)], writes=[("V", t)])
            else:
                S.op('pool', lambda e: e.memset(Vs_new[:, :, 64:65], 1.0), writes=["Vs_new1"])
                S.op('pool', lambda e: e.tensor_copy(out=Vs_new[:, :, 0:64], in_=kv_sb[:, 512:1024].rearrange("p (h d) -> p h d", h=8)),
                     reads=[("kv_sb", 1)], writes=["Vs_new"])
            for gi, (dst, dn, col0, bcol) in enumerate((((sgas if sample else sga), "sga", 1536, 16), ((sgbs if sample else sgb), "sgb", 2560, 24))):
                for half in range(2):
                    pb, pbn = ((pFA, "pFA"), (pFB, "pFB"))[half]
                    for q in range(4):
                        fc = 4 * half + q
                        for kk in range(8):
                            S.op('pe', lambda e, b=b, q=q, kk=kk, pb=pb, col0=col0, fc=fc: e.matmul(
                                pb[:, q, :], lhsT=wq[:, kk, col0 + 128 * fc:col0 + 128 * (fc + 1)], rhs=xU[b][:, kk, :],
                                start=(kk == 0), stop=(kk == 7)), reads=[("p2xT", 0), "wq"], writes=[pbn])
                    for q in range(4):
                        fc = 4 * half + q
                        S.op('act', lambda e, q=q, fc=fc, pb=pb, dst=dst, bcol=bcol: e.activation(
                            out=dst[:, fc, :], in_=pb[:, q, :], func=AF.Sigmoid, bias=bfm[:, bcol + fc:bcol + fc + 1]),
                            reads=[pbn, "bfm"], writes=[(dn, fc)])
            if sample:
                continue
            if not sample:
                if cur >= 1:
                    for h in range(8):
                        hp, hcx = h % 2, h // 2
                        S.op('pe', lambda e, h=h, hp=hp, hcx=hcx: e.matmul(
                            pT0[:, h * 8:(h + 1) * 8], lhsT=qT32[:, hcx, :], rhs=kmTz[:, h, :],
                            start=(h == 0), stop=(h == 7)), reads=["qT32", "kmTz"], writes=["pT0"])
                    S.op('pool', lambda e: e.memset(gm[:], -1.0e30), writes=["gm"])
                    S.op('dve', lambda e, cur=cur: e.tensor_copy(out=gm[:, :, 0:cur], in_=pT0[:, 0:64].rearrange("p (h n) -> p h n", h=8)[:, :, 0:cur]),
                         reads=["pT0", "gm"], writes=["gm"])
                    for h in range(8):
                        S.op('dve', lambda e, h=h: e.max(out=mx[:, h, :], in_=gm[:, h, :]), reads=["gm"], writes=[("mx", h)])
                        S.op('dve', lambda e, h=h: e.tensor_scalar(out=Mq[:, h, 0:8], in0=gm[:, h, :], scalar1=mx[:, h, 2:3], scalar2=-1.0,
                                                                   op0=ALU.is_ge, op1=ALU.add),
                             reads=["gm", ("mx", h), "Mq"], writes=["Mq"])
                for h in range(8):
                    tgt, tn = (pT1, "pT1") if h < 4 else (pFA, "pFA")
                    tv = tgt[0:9, :].rearrange("p (h r) -> p h r", h=4) if h < 4 else tgt[0:9, :, :]
                    S.op('pe', lambda e, h=h, tv=tv: e.transpose(tv[:, h % 4, :], Mq[:, h, :], ident), reads=["Mq", "cst"], writes=[tn])
                S.op('act', lambda e: e.activation(out=R_bf[:, 0:4, :], in_=pT1[0:9, :].rearrange("p (h r) -> p h r", h=4), func=AF.Copy, scale=BIGM),
                     reads=["pT1"], writes=["R_bf0"])
                S.op('act', lambda e: e.activation(out=R_bf[:, 4:8, :], in_=pFA[0:9, :, :], func=AF.Copy, scale=BIGM),
                     reads=["pFA"], writes=["R_bf1"])
                for h in range(8):
                    hp, hcx = h % 2, h // 2
                    ob, obn = pO[h // 4], "pO%d" % (h // 4)
                    kt = 0
                    while kt <= t:
                        nsl = min(4, t + 1 - kt)
                        bk = sctr[0] % 2
                        sctr[0] += 1
                        for sl_ in range(nsl):
                            k2 = kt + sl_
                            n = k2 // 2
                            var = n if n < cur else 8
                            S.op('pe', lambda e, hp=hp, hcx=hcx, k2=k2, bk=bk, sl_=sl_: e.matmul(
                                pS[bk][:, sl_, :], lhsT=kT_all[hp * 64:(hp + 1) * 64, hcx, k2 * 128:(k2 + 1) * 128],
                                rhs=qTb[hp * 64:(hp + 1) * 64, hcx, :], start=True, stop=False),
                                reads=[("kT", k2), "qTb"], writes=[("pS", bk)])
                            S.op('pe', lambda e, h=h, var=var, bk=bk, sl_=sl_, last=(k2 != t): e.matmul(
                                pS[bk][:, sl_, :], lhsT=ltab_bf[0:9, var, :], rhs=R_bf[0:9, h, :], start=False, stop=last),
                                reads=["ltab_bf", "R_bf%d" % (h // 4)], writes=[("pS", bk)])
                            if k2 == t:
                                S.op('pe', lambda e, bk=bk, sl_=sl_: e.matmul(pS[bk][:, sl_, :], lhsT=ident_bf[:], rhs=causal_bf[:], start=False, stop=True),
                                     reads=["ident_bf", "causal_bf"], writes=[("pS", bk)])
                        for sl_ in range(nsl):
                            k2 = kt + sl_
                            S.op('act', lambda e, h=h, bk=bk, sl_=sl_, dl=t - k2: e.activation(
                                out=PT[bk][:, sl_, :], in_=pS[bk][:, sl_, :], func=AF.Exp, scale=0.125, bias=bexp[:, h, dl:dl + 1]),
                                reads=[("pS", bk), "acs"], writes=[("PT", bk)])
                        for sl_ in range(nsl):
                            k2 = kt + sl_
                            S.op('pe', lambda e, h=h, bk=bk, sl_=sl_, k2=k2, ob=ob, t=t: e.matmul(
                                ob[:, h % 4, :], lhsT=PT[bk][:, sl_, :], rhs=V_all[:, k2, h, :], start=(k2 == 0), stop=(k2 == t)),
                                reads=[("PT", bk), ("V", k2), "V_ones"], writes=[obn])
                        kt += nsl
                for k in range(2):
                    S.op('dve', lambda e, k=k: e.reciprocal(out=rsum[:, 4 * k:4 * k + 4], in_=pO[k][:, :, 64]), reads=["pO%d" % k], writes=[("rsum", k)])
                    S.op('dve', lambda e, k=k: e.tensor_tensor(out=yatt[:, 4 * k:4 * k + 4, :], in0=pO[k][:, :, 0:64],
                                                               in1=rsum[:, 4 * k:4 * k + 4].unsqueeze(2).to_broadcast([128, 4, 64]), op=ALU.mult),
                         reads=["pO%d" % k, ("rsum", k)], writes=[("yatt", k)])
            else:
                S.op('pool', lambda e: e.memset(yatt[:], 0.0), writes=[("yatt", 0), ("yatt", 1)])
            yv = yatt[:].rearrange("p h d -> p (h d)")
            for q in range(4):
                S.op('pe', lambda e, q=q, yv=yv: e.transpose(pFA[:, q, :], yv[:, q * 128:(q + 1) * 128], ident),
                     reads=[("yatt", 0), ("yatt", 1), "cst"], writes=["pFA"])
            S.op('act', lambda e: e.activation(out=yattT[:], in_=pFA[:], func=AF.Copy), reads=["pFA"], writes=["yattT"])
            for half in range(2):
                for q in range(4):
                    fc = 4 * half + q
                    for kc in range(4):
                        S.op('pe', lambda e, q=q, fc=fc, kc=kc: e.matmul(pFA[:, q, :], lhsT=wa_bf[:, kc, fc * 128:(fc + 1) * 128], rhs=yattT[:, kc, :],
                                                                      start=(kc == 0), stop=(kc == 3)), reads=["wa", "yattT"], writes=["pFA"])
                for q in range(4):
                    fc = 4 * half + q
                    for kc in range(4):
                        S.op('pe', lambda e, q=q, fc=fc, kc=kc, t=t: e.matmul(pFB[:, q, :], lhsT=wb_bf[:, kc, fc * 128:(fc + 1) * 128],
                                                                           rhs=yssmT[:, kc, t * 128:(t + 1) * 128],
                                                                           start=(kc == 0), stop=(kc == 3)), reads=["wb", ("yssmT", t)], writes=["pFB"])
                hs_ = slice(4 * half, 4 * half + 4)
                S.op('dve', lambda e, hs_=hs_: e.tensor_tensor(out=mtmp[:], in0=pFA[:], in1=sga[:, hs_, :], op=ALU.mult),
                     reads=["pFA"] + [("sga", fc) for fc in range(8)], writes=["mtmp"])
                S.op('dve', lambda e, hs_=hs_: e.tensor_tensor(out=mergedT[:, hs_, :], in0=pFB[:], in1=sgb[:, hs_, :], op=ALU.mult),
                     reads=["pFB"] + [("sgb", fc) for fc in range(8)], writes=[("mergedT", half)])
                S.op('pool', lambda e, hs_=hs_: e.tensor_tensor(out=mergedT[:, hs_, :], in0=mergedT[:, hs_, :], in1=mtmp[:], op=ALU.add),
                     reads=["mtmp", ("mergedT", half)], writes=[("mergedT", half)])
            for j, (pt, ptn) in enumerate(((pT0, "pT0"), (pT1, "pT1"))):
                for kc in range(8):
                    S.op('pe', lambda e, j=j, kc=kc, pt=pt: e.matmul(pt[:], lhsT=mergedT[:, kc, :], rhs=wo_bf[:, kc, j * 512:(j + 1) * 512],
                                                                 start=(kc == 0), stop=(kc == 7)),
                         reads=[("mergedT", 0), ("mergedT", 1), "wo"], writes=[ptn])
                S.op('dve', lambda e, j=j, pt=pt, b=b: e.scalar_tensor_tensor(out=pre[:, j * 512:(j + 1) * 512], in0=xtm[b][:, j * 512:(j + 1) * 512],
                                                                          scalar=DN_ALPHA, in1=pt[:], op0=ALU.mult, op1=ALU.add),
                     reads=[ptn, ("xtm", 0)], writes=[("pre", j)])
            layer_norm(S, pre, x1, stats, mv, lng, lnb, [("pre", 0), ("pre", 1)], "x1", ["lng", "lnb"])
            S.dma(x1_d[t], x1[:], reads=["x1", ("pre", 0), ("pre", 1)], writes=[("dram_x1", t)], key="x1dma")


    S.barrier()
    if p2tiles == NT:
        cache_k = din("cache_k", [2560 * 8, 16 * 512]); cache_v = din("cache_v", [2560 * 8, 16 * 512])
        pt_exp = din("pt_exp", [128, 16], I32)
        scst = din("scst", [128, 8 + 128 + 8 + 128 + 128 + 128])
        lseq_d = din("lseq", [16, 17, 128])
        lblk_d = din("lblk", [9, 2, 128])
        with ExitStack() as sz:
            sbz = mk_sb(sz)
            psz = mk_ps(sz)
            wa_z = sbz("wa_z", [128, 4, D], BF16); wb_z = sbz("wb_z", [128, 4, D], BF16); wo_z = sbz("wo_z", [128, 8, D], BF16)
            lng_z = sbz("lng_z", [128, D]); lnb_z = sbz("lnb_z", [128, D])
            scs = sbz("scs", [128, 528])
            blockind = scs[:, 0:8]
            bexp_s = scs[:, 8:136].rearrange("p (h r) -> p h r", h=8)
            bexp_new = scs[:, 136:144]
            slq_s = scs[:, 144:152]
            lseq_bf = sbz("lseq_bf", [16, 17, 128], BF16); lblk_bf = sbz("lblk_bf", [9, 2, 128], BF16)
            causal_z = sbz("causal_z", [128, 128], BF16); seqm_bf = sbz("seqm_bf", [16, 128], BF16); ident_z = sbz("ident_z", [128, 128], BF16)
            idx_z = sbz("idx_z", [128, 16], I32)
            Kst = sbz("Kst", [128, 16, 512]); Vst = sbz("Vst", [128, 16, 512])
            kT_z = sbz("kT_z", [128, 4, 2048], BF16); V_z = sbz("V_z", [128, 16, 8, 65], BF16)
            kmT_z = sbz("kmT_z", [128, 4, 8]); kmTz_z = sbz("kmTz_z", [128, 8, 8])
            Mq_z = sbz("Mq_z", [128, 8, 9]); gm_z = sbz("gm_z", [128, 8, 8]); mx_z = sbz("mx_z", [128, 8, 8])
            R_z = sbz("R_z", [9, 8, 128], BF16)
            PT_z = [sbz("PT_z%d" % i, [128, 4, 128], BF16) for i in range(2)]
            yacc = sbz("yacc", [128, 8, 65]); rsum_z = sbz("rsum_z", [128, 8])
            yatt_z = sbz("yatt_z", [128, 8, 64]); yattT_z = sbz("yattT_z", [128, 4, 128], BF16)
            mtmp_z = sbz("mtmp_z", [128, 4, 128]); mergedT_z = sbz("mergedT_z", [128, 8, 128], BF16)
            pre_z = sbz("pre_z", [128, D]); xtm_z = sbz("xtm_z", [128, D])
            stats_z = sbz("stats_z", [128, 2, 6]); mv_z = sbz("mv_z", [128, 4])
            lt32_z = sbz("lt32_z", [16, 17, 128]); lb32_z = sbz("lb32_z", [9, 2, 128])
            pFA_z = psz("pFA_z", [128, 4, 128]); pFB_z = psz("pFB_z", [128, 4, 128])
            pT0_z = psz("pT0_z", [128, 512]); pT1_z = psz("pT1_z", [128, 512])
            pS_z = [psz("pS_z%d" % i, [128, 4, 128]) for i in range(2)]
            pO_z = [psz("pO_z%d" % i, [128, 4, 65]) for i in range(2)]
            kst2 = Kst[:].rearrange("p r c -> p (r c)")
            war_z = w_a.rearrange("(k p) n -> p k n", p=128); wbr_z = w_b.rearrange("(k p) n -> p k n", p=128); wor_z = w_o.rearrange("(k p) n -> p k n", p=128)
            S.dma(kst2[:, 0:4096].rearrange("p (k n) -> p k n", k=4), war_z, writes=["Kst"])
            S.op('pool', lambda e: e.tensor_copy(out=wa_z[:], in_=kst2[:, 0:4096].rearrange("p (k n) -> p k n", k=4)), reads=["Kst"], writes=["wa_z"])
            S.dma(kst2[:, 4096:8192].rearrange("p (k n) -> p k n", k=4), wbr_z, writes=["Kst2"])
            S.op('pool', lambda e: e.tensor_copy(out=wb_z[:], in_=kst2[:, 4096:8192].rearrange("p (k n) -> p k n", k=4)), reads=["Kst2"], writes=["wb_z"])
            vst2 = Vst[:].rearrange("p r c -> p (r c)")
            S.dma(vst2.rearrange("p (k n) -> p k n", k=8), wor_z, writes=["Vst"])
            S.op('pool', lambda e: e.tensor_copy(out=wo_z[:], in_=vst2.rearrange("p (k n) -> p k n", k=8)), reads=["Vst"], writes=["wo_z"])
            S.dma(lng_z[:], ln1g_bc, writes=["lng_z"]); S.dma(lnb_z[:], ln1b_bc, writes=["lnb_z"])
            S.dma(scs[:], scst, writes=["scs"])
            S.dma(lt32_z[:], lseq_d, writes=["lt32_z"]); S.dma(lb32_z[:], lblk_d, writes=["lb32_z"])
            S.dma(idx_z[:], pt_exp, writes=["idx_z"])
            S.dma(xtm_z[:], x_tm[NT - 1], writes=["xtm_z"])
            S.op('dve', lambda e: e.tensor_copy(out=lseq_bf[:], in_=lt32_z[:]), reads=["lt32_z"], writes=["lseq_bf"])
            S.op('dve', lambda e: e.tensor_copy(out=lblk_bf[:], in_=lb32_z[:]), reads=["lb32_z"], writes=["lblk_bf"])
            S.op('dve', lambda e: e.tensor_copy(out=causal_z[:], in_=scs[:, 272:400]), reads=["scs"], writes=["causal_z"])
            S.op('dve', lambda e: e.tensor_copy(out=seqm_bf[:], in_=scs[0:16, 400:528]), reads=["scs"], writes=["seqm_bf"])
            S.op('dve', lambda e: e.tensor_copy(out=ident_z[:], in_=ident), reads=["cst"], writes=["ident_z"])
            idf_z = sbz("idf_z", [128, 16])
            S.op('dve', lambda e: e.tensor_copy(out=idf_z[:], in_=idx_z[:]), reads=["idx_z"], writes=["idf_z"])
            S.op('dve', lambda e: e.tensor_scalar(out=idf_z[:], in0=idf_z[:], scalar1=8.0, scalar2=scs[:, 152:153], op0=ALU.mult, op1=ALU.add),
                 reads=["idf_z", "scs"], writes=["idf_z"])
            S.op('dve', lambda e: e.tensor_copy(out=idx_z[:], in_=idf_z[:]), reads=["idf_z"], writes=["idx_z"])
            S.op('pool', lambda e: e.memset(V_z[:, :, :, 64:65], 1.0), writes=["V_z1"])
            S.op('pool', lambda e: e.memset(kmTz_z[:], 0.0), writes=["kmTz_z"])
            S.op('pool', lambda e: e.memset(Mq_z[:, :, 0:8], 0.0), writes=["Mq_z"])
            S.op('dve', lambda e: e.tensor_copy(out=Mq_z[:, :, 8], in_=slq_s), reads=["scs", "Mq_z"], writes=["Mq_z"])
            S.op('pool', lambda e: e.memset(yacc[:], 0.0), writes=["yacc"])
            zctr = [0]

            def mask_rows():
                for h in range(8):
                    tgt, tn = (pT1_z, "pT1_z") if h < 4 else (pFA_z, "pFA_z")
                    tv = tgt[0:9, :].rearrange("p (h r) -> p h r", h=4) if h < 4 else tgt[0:9, :, :]
                    S.op('pe', lambda e, h=h, tv=tv: e.transpose(tv[:, h % 4, :], Mq_z[:, h, :], ident), reads=["Mq_z", "cst"], writes=[tn])
                S.op('act', lambda e: e.activation(out=R_z[:, 0:4, :], in_=pT1_z[0:9, :].rearrange("p (h r) -> p h r", h=4), func=AF.Copy, scale=BIGM),
                     reads=["pT1_z"], writes=["R_z0"])
                S.op('act', lambda e: e.activation(out=R_z[:, 4:8, :], in_=pFA_z[0:9, :, :], func=AF.Copy, scale=BIGM),
                     reads=["pFA_z"], writes=["R_z1"])

            for sq in range(16):
                S.op('pool', lambda e, sq=sq: e.indirect_dma_start(out=Kst[:].rearrange("p r c -> p (r c)"), out_offset=None, in_=cache_k,
                                                                 in_offset=bass.IndirectOffsetOnAxis(ap=idx_z[:, sq:sq + 1].bitcast(U32), axis=0)),
                     reads=["idx_z", "wa_z", "wb_z"], writes=["Kst"], dma_key="Kst_g")
                S.op('pool', lambda e, sq=sq: e.indirect_dma_start(out=Vst[:].rearrange("p r c -> p (r c)"), out_offset=None, in_=cache_v,
                                                                 in_offset=bass.IndirectOffsetOnAxis(ap=idx_z[:, sq:sq + 1].bitcast(U32), axis=0)),
                     reads=["idx_z", "wo_z"], writes=["Vst"], dma_key="Vst_g")
                for r in range(16):
                    for q in range(4):
                        S.op('pe', lambda e, r=r, q=q: e.transpose(pFB_z[:, q, :], Kst[:, r, q * 128:(q + 1) * 128], ident), reads=["Kst", "cst"], writes=["pFB_z"])
                    S.op('act', lambda e, r=r: e.activation(out=kT_z[:, :, r * 128:(r + 1) * 128], in_=pFB_z[:], func=AF.Copy), reads=["pFB_z"], writes=[("kT_z", r)])
                for q in range(4):
                    for r in range(16):
                        S.op('pe', lambda e, r=r, q=q: e.matmul(pT0_z[:, q * 8:(q + 1) * 8], lhsT=Kst[:, r, q * 128:(q + 1) * 128], rhs=blockind,
                                                               start=(r == 0), stop=(r == 15)), reads=["Kst", "scs"], writes=["pT0_z"])
                S.op('dve', lambda e: e.tensor_scalar(out=kmT_z[:], in0=pT0_z[:, 0:32].rearrange("p (c n) -> p c n", c=4), scalar1=1.0 / 256.0, scalar2=None, op0=ALU.mult),
                     reads=["pT0_z"], writes=["kmT_z"])
                kz_z = kmTz_z[:].rearrange("p (c two) n -> p c two n", two=2)
                S.op('dve', lambda e, kz_z=kz_z: e.tensor_copy(out=kz_z[0:64, :, 0, :], in_=kmT_z[0:64, :, :]), reads=["kmT_z", "kmTz_z"], writes=["kmTz_z"])
                S.op('dve', lambda e, kz_z=kz_z: e.tensor_copy(out=kz_z[64:128, :, 1, :], in_=kmT_z[64:128, :, :]), reads=["kmT_z", "kmTz_z"], writes=["kmTz_z"])
                S.op('pool', lambda e: e.tensor_copy(out=V_z[:, :, :, 0:64], in_=Vst[:].rearrange("p r (h d) -> p r h d", h=8)), reads=["Vst"], writes=["V_z"])
                for h in range(8):
                    S.op('pe', lambda e, h=h: e.matmul(pT0_z[:, 64 + h * 8:64 + (h + 1) * 8], lhsT=qT32s[:, h // 2, :], rhs=kmTz_z[:, h, :],
                                                      start=(h == 0), stop=(h == 7)), reads=["qT32", "kmTz_z"], writes=["pT0_z"])
                S.op('dve', lambda e: e.tensor_copy(out=gm_z[:], in_=pT0_z[:, 64:128].rearrange("p (h n) -> p h n", h=8)), reads=["pT0_z"], writes=["gm_z"])
                for h in range(8):
                    S.op('dve', lambda e, h=h: e.max(out=mx_z[:, h, :], in_=gm_z[:, h, :]), reads=["gm_z"], writes=[("mx_z", h)])
                    S.op('dve', lambda e, h=h: e.tensor_scalar(out=Mq_z[:, h, 0:8], in0=gm_z[:, h, :], scalar1=mx_z[:, h, 2:3], scalar2=-1.0,
                                                               op0=ALU.is_ge, op1=ALU.add), reads=["gm_z", ("mx_z", h), "Mq_z"], writes=["Mq_z"])
                mask_rows()
                for h in range(8):
                    hp, hcx = h % 2, h // 2
                    ob, obn = pO_z[h // 4], "pO_z%d" % (h // 4)
                    for r0 in range(0, 16, 4):
                        bk = zctr[0] % 2
                        zctr[0] += 1
                        for sl_ in range(4):
                            r = r0 + sl_
                            S.op('pe', lambda e, hp=hp, hcx=hcx, r=r, bk=bk, sl_=sl_: e.matmul(
                                pS_z[bk][:, sl_, :], lhsT=kT_z[hp * 64:(hp + 1) * 64, hcx, r * 128:(r + 1) * 128],
                                rhs=qTbs[hp * 64:(hp + 1) * 64, hcx, :], start=True, stop=False), reads=[("kT_z", r), "qTb"], writes=[("pS_z", bk)])
                            S.op('pe', lambda e, h=h, bk=bk, sl_=sl_: e.matmul(pS_z[bk][:, sl_, :], lhsT=lblk_bf[0:9, 0, :], rhs=R_z[0:9, h, :], start=False, stop=False),
                                 reads=["lblk_bf", "R_z%d" % (h // 4)], writes=[("pS_z", bk)])
                            S.op('pe', lambda e, sq=sq, bk=bk, sl_=sl_: e.matmul(pS_z[bk][:, sl_, :], lhsT=lseq_bf[0:16, sq, :], rhs=seqm_bf[0:16, :], start=False, stop=True),
                                 reads=["lseq_bf", "seqm_bf"], writes=[("pS_z", bk)])
                        for sl_ in range(4):
                            r = r0 + sl_
                            S.op('act', lambda e, h=h, bk=bk, sl_=sl_, r=r: e.activation(out=PT_z[bk][:, sl_, :], in_=pS_z[bk][:, sl_, :], func=AF.Exp, scale=0.125,
                                                                                       bias=bexp_s[:, h, r:r + 1]), reads=[("pS_z", bk), "scs"], writes=[("PT_z", bk)])
                        for sl_ in range(4):
                            r = r0 + sl_
                            S.op('pe', lambda e, h=h, bk=bk, sl_=sl_, r=r, ob=ob: e.matmul(ob[:, h % 4, :], lhsT=PT_z[bk][:, sl_, :], rhs=V_z[:, r, h, :],
                                                                                       start=(r == 0), stop=(r == 15)),
                                 reads=[("PT_z", bk), "V_z", "V_z1"], writes=[obn])
                for k in range(2):
                    S.op('dve', lambda e, k=k: e.tensor_tensor(out=yacc[:, 4 * k:4 * k + 4, :], in0=yacc[:, 4 * k:4 * k + 4, :], in1=pO_z[k][:], op=ALU.add),
                         reads=["pO_z%d" % k, "yacc"], writes=["yacc"])
            for h in range(8):
                hp, hcx = h % 2, h // 2
                ob, obn = pO_z[h // 4], "pO_z%d" % (h // 4)
                bk = zctr[0] % 2
                zctr[0] += 1
                S.op('pe', lambda e, hp=hp, hcx=hcx, bk=bk: e.matmul(pS_z[bk][:, 0, :], lhsT=kTs_new[hp * 64:(hp + 1) * 64, hcx, :], rhs=qTbs[hp * 64:(hp + 1) * 64, hcx, :],
                                                                 start=True, stop=False), reads=["kTs_new", "qTb"], writes=[("pS_z", bk)])
                S.op('pe', lambda e, h=h, bk=bk: e.matmul(pS_z[bk][:, 0, :], lhsT=lblk_bf[0:9, 1, :], rhs=R_z[0:9, h, :], start=False, stop=False),
                     reads=["lblk_bf", "R_z%d" % (h // 4)], writes=[("pS_z", bk)])
                S.op('pe', lambda e, bk=bk: e.matmul(pS_z[bk][:, 0, :], lhsT=ident_z[:], rhs=causal_z[:], start=False, stop=True),
                     reads=["ident_z", "causal_z"], writes=[("pS_z", bk)])
                S.op('act', lambda e, h=h, bk=bk: e.activation(out=PT_z[bk][:, 0, :], in_=pS_z[bk][:, 0, :], func=AF.Exp, scale=0.125, bias=bexp_new[:, h:h + 1]),
                     reads=[("pS_z", bk), "scs"], writes=[("PT_z", bk)])
                S.op('pe', lambda e, h=h, bk=bk, ob=ob: e.matmul(ob[:, h % 4, :], lhsT=PT_z[bk][:, 0, :], rhs=Vs_new[:, h, :], start=True, stop=True),
                     reads=[("PT_z", bk), "Vs_new", "Vs_new1"], writes=[obn])
            for k in range(2):
                S.op('dve', lambda e, k=k: e.tensor_tensor(out=yacc[:, 4 * k:4 * k + 4, :], in0=yacc[:, 4 * k:4 * k + 4, :], in1=pO_z[k][:], op=ALU.add),
                     reads=["pO_z%d" % k, "yacc"], writes=["yacc"])
            S.op('dve', lambda e: e.reciprocal(out=rsum_z[:], in_=yacc[:, :, 64]), reads=["yacc"], writes=["rsum_z"])
            S.op('dve', lambda e: e.tensor_tensor(out=yatt_z[:], in0=yacc[:, :, 0:64], in1=rsum_z[:].unsqueeze(2).to_broadcast([128, 8, 64]), op=ALU.mult),
                 reads=["yacc", "rsum_z"], writes=["yatt_z"])
            yv_z = yatt_z[:].rearrange("p h d -> p (h d)")
            for q in range(4):
                S.op('pe', lambda e, q=q: e.transpose(pFA_z[:, q, :], yv_z[:, q * 128:(q + 1) * 128], ident), reads=["yatt_z", "cst"], writes=["pFA_z"])
            S.op('act', lambda e: e.activation(out=yattT_z[:], in_=pFA_z[:], func=AF.Copy), reads=["pFA_z"], writes=["yattT_z"])
            tz = NT - 1
            for half in range(2):
                for q in range(4):
                    fc = 4 * half + q
                    for kc in range(4):
                        S.op('pe', lambda e, q=q, fc=fc, kc=kc: e.matmul(pFA_z[:, q, :], lhsT=wa_z[:, kc, fc * 128:(fc + 1) * 128], rhs=yattT_z[:, kc, :],
                                                                      start=(kc == 0), stop=(kc == 3)), reads=["wa_z", "yattT_z"], writes=["pFA_z"])
                for q in range(4):
                    fc = 4 * half + q
                    for kc in range(4):
                        S.op('pe', lambda e, q=q, fc=fc, kc=kc: e.matmul(pFB_z[:, q, :], lhsT=wb_z[:, kc, fc * 128:(fc + 1) * 128],
                                                                      rhs=yssmT[:, kc, tz * 128:(tz + 1) * 128], start=(kc == 0), stop=(kc == 3)),
                             reads=["wb_z", ("yssmT", tz)], writes=["pFB_z"])
                hs_ = slice(4 * half, 4 * half + 4)
                S.op('dve', lambda e, hs_=hs_: e.tensor_tensor(out=mtmp_z[:], in0=pFA_z[:], in1=sgas[:, hs_, :], op=ALU.mult),
                     reads=["pFA_z"] + [("sga", fc) for fc in range(8)], writes=["mtmp_z"])
                S.op('dve', lambda e, hs_=hs_: e.tensor_tensor(out=mergedT_z[:, hs_, :], in0=pFB_z[:], in1=sgbs[:, hs_, :], op=ALU.mult),
                     reads=["pFB_z"] + [("sgb", fc) for fc in range(8)], writes=[("mergedT_z", half)])
                S.op('pool', lambda e, hs_=hs_: e.tensor_tensor(out=mergedT_z[:, hs_, :], in0=mergedT_z[:, hs_, :], in1=mtmp_z[:], op=ALU.add),
                     reads=["mtmp_z", ("mergedT_z", half)], writes=[("mergedT_z", half)])
            for j, (pt, ptn) in enumerate(((pT0_z, "pT0_z"), (pT1_z, "pT1_z"))):
                for kc in range(8):
                    S.op('pe', lambda e, j=j, kc=kc, pt=pt: e.matmul(pt[:], lhsT=mergedT_z[:, kc, :], rhs=wo_z[:, kc, j * 512:(j + 1) * 512],
                                                                 start=(kc == 0), stop=(kc == 7)),
                         reads=[("mergedT_z", 0), ("mergedT_z", 1), "wo_z"], writes=[ptn])
                S.op('dve', lambda e, j=j, pt=pt: e.scalar_tensor_tensor(out=pre_z[:, j * 512:(j + 1) * 512], in0=xtm_z[:, j * 512:(j + 1) * 512],
                                                                     scalar=DN_ALPHA, in1=pt[:], op0=ALU.mult, op1=ALU.add),
                     reads=[ptn, "xtm_z"], writes=[("pre_z", j)])
            layer_norm(S, pre_z, pre_z, stats_z, mv_z, lng_z, lnb_z, [("pre_z", 0), ("pre_z", 1)], "x1_z", ["lng_z", "lnb_z"])
            S.dma(x1_d[tz], pre_z[:], reads=["x1_z", ("pre_z", 0), ("pre_z", 1)], writes=[("dram_x1", tz)], key="x1dma")
    S.barrier()
    s12.close()
    if stop_after < 3:
        S.finish('sp')
        with nc.Block() as block:
            S.replay(block)
        st.close()
        return nc
    w_pq = din("w_pq", [D, 2048]); skT_d = din("skT", [128, 2, 8, 128])
    puT = din("peer_uT", [128, 128, 8, 128])
    pv_d = din("peer_v", [16384, D])
    ln2g_bc = din("ln2g_bc", [128, D]); ln2b_bc = din("ln2b_bc", [128, D])
    y_out = dout("y_out", [NT, 128, D])
    u_scr = nc.dram_tensor("u_scr", [128, 128, 8, 128], BF16, kind="Internal").ap()
    v_scr = nc.dram_tensor("v_scr", [128, 128, D], BF16, kind="Internal").ap()
    with ExitStack() as s3:
        sb3 = mk_sb(s3)
        ps3 = mk_ps(s3)
        wpq_bf = sb3("wpq_bf", [128, 8, 2048], BF16)
        skT = sb3("skT_bf", [128, 2, 8, 128], BF16)
        lng2 = sb3("lng2", [128, D]); lnb2 = sb3("lnb2", [128, D])
        iota16 = sb3("iota16", [128, 16]); iota128 = sb3("iota128", [128, 128])
        with ExitStack() as sl:
            sbl = mk_sb(sl)
            stg3 = [sbl("stg3_%d" % i, [128, 8, 256]) for i in range(2)]
            wpr = w_pq.rearrange("(k p) n -> p k n", p=128)
            for c in range(8):
                b = c % 2
                S.dma(stg3[b][:], wpr[:, :, c * 256:(c + 1) * 256], writes=[("stg3", b)])
                S.op('pool', lambda e, b=b, c=c: e.tensor_copy(out=wpq_bf[:, :, c * 256:(c + 1) * 256], in_=stg3[b][:]), reads=[("stg3", b)], writes=["wpq"])
            S.dma(stg3[0][:].rearrange("p a b -> p (a b)"), skT_d.rearrange("p s h m -> p (s h m)"), writes=[("stg3", 0)])
            S.op('pool', lambda e: e.tensor_copy(out=skT[:].rearrange("p s h m -> p (s h m)"), in_=stg3[0][:].rearrange("p a b -> p (a b)")),
                 reads=[("stg3", 0)], writes=["skT"])
            S.dma(lng2[:], ln2g_bc, writes=["lng2"]); S.dma(lnb2[:], ln2b_bc, writes=["lnb2"])
            S.op('pool', lambda e: e.iota(iota16[:], pattern=[[1, 16]], base=0, channel_multiplier=0, allow_small_or_imprecise_dtypes=True), writes=["iota16"])
            S.op('pool', lambda e: e.iota(iota128[:], pattern=[[1, 128]], base=0, channel_multiplier=0, allow_small_or_imprecise_dtypes=True), writes=["iota128"])
        S.barrier()

        G = sb3("G", [128, 128, 256], BF16)
        x1g = [sb3("x1g%d" % i, [128, D]) for i in range(2)]
        x1T = sb3("x1T", [128, 8, 256], BF16)
        qTp = sb3("qTp", [128, 16, 128], BF16)
        s_sb = sb3("s_sb", [128, 8, 128])
        tmpm = sb3("tmpm", [128, 256])
        vals = sb3("vals", [128, 2, 8, 16]); idx = sb3("idx", [128, 2, 8, 16], U32); idxf = sb3("idxf", [128, 2, 8, 16])
        cand = sb3("cand", [128, 8, 16, 16]); big2 = sb3("big2", [128, 8, 16, 16])
        scv = sb3("scv", [128, 8, 16]); ci = sb3("ci", [128, 8, 16], U32); abi = sb3("abi", [128, 2, 8, 16], I32); abf = sb3("abf", [128, 2, 8, 16])
        nmax = sb3("nmax", [128, 8]); gsum = sb3("gsum", [128, 8]); egt = sb3("egt", [128, 3, 8, 16])
        egT = [sb3("egT%d" % i, [128, 3, 128]) for i in range(2)]
        NLR = 6
        Lb = [sb3("Lb%d" % i, [128, 128], BF16) for i in range(NLR)]
        Rb = [sb3("Rb%d" % i, [128, 128], BF16) for i in range(NLR)]
        ustg = [sb3("ustg%d" % i, [128, 8, 128]) for i in range(2)]
        vstg = [sb3("vstg%d" % i, [128, D]) for i in range(2)]
        NWB = 3
        ubf = [sb3("ubf%d" % i, [128, 8, 128], BF16) for i in range(NWB)]
        vbf = [sb3("vbf%d" % i, [128, D], BF16) for i in range(NWB)]
        glb = [sb3("glb%d" % i, [128, 256], BF16) for i in range(2)]
        WT = [sb3("WT%d" % i, [128, 256], BF16) for i in range(2)]
        pre2 = sb3("pre2", [128, D])
        stats2 = sb3("stats2", [128, 2, 6]); mv2 = sb3("mv2", [128, 4])
        pAcc = [ps3("pAcc%d" % i, [128, 512]) for i in range(4)]
        pAT = [ps3("pAT%d" % i, [128, 256]) for i in range(2)]
        pGb = [ps3("pGb%d" % i, [128, 4, 128]) for i in range(2)]

        groups = [(2 * i, 2 * i + 1) for i in range(8)] + [(16,)]
        if glimit is not None:
            groups = [groups[i] for i in glimit]
        lr_ctr = [0]; gb_ctr = [0]; wctr = [0]
        for gi, tiles in enumerate(groups):
            ntl = len(tiles)
            ntok = 128 * ntl
            for li, t in enumerate(tiles):
                S.dma(x1g[li][:], x1_d[t], reads=[("dram_x1", t)], writes=[("x1g", li)])
                for half in range(2):
                    pb, pbn = pGb[half], "pGb%d" % half
                    for q in range(4):
                        kk = 4 * half + q
                        S.op('pe', lambda e, li=li, kk=kk, q=q, pb=pb: e.transpose(pb[:, q, :], x1g[li][:, kk * 128:(kk + 1) * 128], ident),
                             reads=[("x1g", li), "cst"], writes=[pbn])
                    S.op('act', lambda e, li=li, half=half, pb=pb: e.activation(out=x1T[:, 4 * half:4 * half + 4, li * 128:(li + 1) * 128], in_=pb[:], func=AF.Copy),
                         reads=[pbn], writes=[("x1T", li)])
                for c4 in range(4):
                    pb, pbn = pGb[c4 % 2], "pGb%d" % (c4 % 2)
                    for q in range(4):
                        c = 4 * c4 + q
                        for kk in range(8):
                            S.op('pe', lambda e, li=li, c=c, q=q, kk=kk, pb=pb: e.matmul(pb[:, q, :], lhsT=wpq_bf[:, kk, c * 128:(c + 1) * 128],
                                                                                    rhs=x1T[:, kk, li * 128:(li + 1) * 128], start=(kk == 0), stop=(kk == 7)),
                                 reads=[("x1T", li), "wpq"], writes=[pbn])
                    S.op('act', lambda e, c4=c4, pb=pb: e.activation(out=qTp[:, 4 * c4:4 * c4 + 4, :], in_=pb[:], func=AF.Copy), reads=[pbn], writes=[("qTp", c4)])
                for side in range(2):
                    for hh in range(2):
                        pb, pbn = pGb[hh], "pGb%d" % hh
                        for q in range(4):
                            h = 4 * hh + q
                            S.op('pe', lambda e, h=h, q=q, side=side, pb=pb: e.matmul(pb[:, q, :], lhsT=qTp[:, 2 * h + side, :], rhs=skT[:, side, h, :],
                                                                                 start=True, stop=True),
                                 reads=[("qTp", (2 * h + side) // 4), "skT"], writes=[pbn])
                        S.op('act', lambda e, hh=hh, pb=pb: e.activation(out=s_sb[:, 4 * hh:4 * hh + 4, :], in_=pb[:], func=AF.Copy), reads=[pbn], writes=[("s_sb", hh)])
                    for h in range(8):
                        rr = [("s_sb", h // 4)]
                        S.op('dve', lambda e, h=h, side=side: e.max(out=vals[:, side, h, 0:8], in_=s_sb[:, h, :]), reads=rr, writes=["vals"])
                        S.op('dve', lambda e, h=h, side=side: e.max_index(out=idx[:, side, h, 0:8], in_max=vals[:, side, h, 0:8], in_values=s_sb[:, h, :]),
                             reads=rr + ["vals"], writes=["idx"])
                        S.op('dve', lambda e, h=h, side=side: e.match_replace(out=tmpm[:, 0:128], in_to_replace=vals[:, side, h, 0:8], in_values=s_sb[:, h, :], imm_value=-1.0e30),
                             reads=rr + ["vals"], writes=["tmpm"])
                        S.op('dve', lambda e, h=h, side=side: e.max(out=vals[:, side, h, 8:16], in_=tmpm[:, 0:128]), reads=["tmpm"], writes=["vals"])
                        S.op('dve', lambda e, h=h, side=side: e.max_index(out=idx[:, side, h, 8:16], in_max=vals[:, side, h, 8:16], in_values=tmpm[:, 0:128]),
                             reads=["tmpm", "vals"], writes=["idx"])
                S.op('dve', lambda e: e.tensor_tensor(out=cand[:], in0=vals[:, 0, :, :].unsqueeze(3).to_broadcast([128, 8, 16, 16]),
                                                      in1=vals[:, 1, :, :].unsqueeze(2).to_broadcast([128, 8, 16, 16]), op=ALU.add),
                     reads=["vals"], writes=["cand"])
                for h in range(8):
                    cf = cand[:, h, :, :].rearrange("p a b -> p (a b)")
                    S.op('dve', lambda e, h=h, cf=cf: e.max(out=scv[:, h, 0:8], in_=cf), reads=["cand"], writes=["scv"])
                    S.op('dve', lambda e, h=h, cf=cf: e.max_index(out=ci[:, h, 0:8], in_max=scv[:, h, 0:8], in_values=cf), reads=["cand", "scv"], writes=["ci"])
                    S.op('dve', lambda e, h=h, cf=cf: e.match_replace(out=tmpm[:], in_to_replace=scv[:, h, 0:8], in_values=cf, imm_value=-1.0e30),
                         reads=["cand", "scv"], writes=["tmpm"])
                    S.op('dve', lambda e, h=h: e.max(out=scv[:, h, 8:16], in_=tmpm[:]), reads=["tmpm"], writes=["scv"])
                    S.op('dve', lambda e, h=h: e.max_index(out=ci[:, h, 8:16], in_max=scv[:, h, 8:16], in_values=tmpm[:]), reads=["tmpm", "scv"], writes=["ci"])
                S.op('dve', lambda e: e.tensor_scalar(out=nmax[:], in0=scv[:, :, 0], scalar1=-1.0, scalar2=None, op0=ALU.mult), reads=["scv"], writes=["nmax"])
                for h in range(8):
                    S.op('act', lambda e, h=h: e.activation(out=egt[:, 2, h, :], in_=scv[:, h, :], func=AF.Exp, bias=nmax[:, h:h + 1], accum_out=gsum[:, h:h + 1]),
                         reads=["scv", "nmax"], writes=["g_un"])
                S.op('dve', lambda e: e.reciprocal(out=gsum[:], in_=gsum[:]), reads=["g_un"], writes=["gsum"])
                S.op('dve', lambda e: e.tensor_tensor(out=egt[:, 2, :, :], in0=egt[:, 2, :, :], in1=gsum[:].unsqueeze(2).to_broadcast([128, 8, 16]), op=ALU.mult),
                     reads=["g_un", "gsum"], writes=["egt_g"])
                cii = ci[:].bitcast(I32)
                S.op('dve', lambda e, cii=cii: e.tensor_single_scalar(out=abi[:, 0, :, :], in_=cii, scalar=4, op=ALU.arith_shift_right), reads=["ci"], writes=["abi"])
                S.op('dve', lambda e, cii=cii: e.tensor_single_scalar(out=abi[:, 1, :, :], in_=cii, scalar=15, op=ALU.bitwise_and), reads=["ci"], writes=["abi"])
                S.op('dve', lambda e: e.tensor_copy(out=abf[:], in_=abi[:]), reads=["abi"], writes=["abf"])
                S.op('dve', lambda e: e.tensor_copy(out=idxf[:], in_=idx[:].bitcast(I32)), reads=["idx"], writes=["idxf"])
                for side in range(2):
                    S.op('dve', lambda e, side=side: e.tensor_tensor(out=big2[:], in0=abf[:, side, :, :].unsqueeze(3).to_broadcast([128, 8, 16, 16]),
                                                                   in1=iota16[:].unsqueeze(1).unsqueeze(1).to_broadcast([128, 8, 16, 16]), op=ALU.is_equal),
                         reads=["abf", "iota16"], writes=["big2"])
                    S.op('dve', lambda e, side=side: e.tensor_tensor(out=big2[:], in0=big2[:], in1=idxf[:, side, :, :].unsqueeze(2).to_broadcast([128, 8, 16, 16]), op=ALU.mult),
                         reads=["big2", "idxf"], writes=["big2"])
                    S.op('dve', lambda e, side=side: e.tensor_reduce(out=egt[:, side, :, :], in_=big2[:], axis=AX.X, op=ALU.add),
                         reads=["big2"], writes=[("egt_e", side)])
                eb = egT[li]
                for j in range(3):
                    S.op('pe', lambda e, j=j: e.transpose(pGb[0][:, j, :], egt[:, j, :, :].rearrange("p h k -> p (h k)"), ident),
                         reads=[("egt_e", 0), ("egt_e", 1), "egt_g", "cst"], writes=["pGb0"])
                S.op('act', lambda e, eb=eb: e.activation(out=eb[:], in_=pGb[0][:, 0:3, :], func=AF.Copy), reads=["pGb0"], writes=[("egT", li)])
                for t4 in range(32):
                    gb = gb_ctr[0] % 2
                    gb_ctr[0] += 1
                    for sl_ in range(4):
                        tk = 4 * t4 + sl_
                        i = lr_ctr[0] % NLR
                        lr_ctr[0] += 1
                        S.op('dve', lambda e, i=i, tk=tk, eb=eb: e.tensor_scalar(out=Lb[i][:], in0=iota128[:], scalar1=eb[:, 0, tk:tk + 1], scalar2=eb[:, 2, tk:tk + 1],
                                                                          op0=ALU.is_equal, op1=ALU.mult),
                             reads=[("egT", li), "iota128"], writes=[("Lb", i)])
                        S.op('dve', lambda e, i=i, tk=tk, eb=eb: e.tensor_scalar(out=Rb[i][:], in0=iota128[:], scalar1=eb[:, 1, tk:tk + 1], scalar2=None, op0=ALU.is_equal),
                             reads=[("egT", li), "iota128"], writes=[("Rb", i)])
                        S.op('pe', lambda e, i=i, gb=gb, sl_=sl_: e.matmul(pGb[gb][:, sl_, :], lhsT=Rb[i][:], rhs=Lb[i][:], start=True, stop=True),
                             reads=[("Lb", i), ("Rb", i)], writes=["pGb%d" % gb])
                    c0 = li * 128 + 4 * t4
                    S.op('act', lambda e, gb=gb, c0=c0: e.activation(out=G[:, :, c0:c0 + 4].rearrange("p m t -> p t m"), in_=pGb[gb][:], func=AF.Copy),
                         reads=["pGb%d" % gb], writes=[("G", li)])
            for m1 in range(128):
                wb = wctr[0] % NWB
                wctr[0] += 1
                if gi == 0:
                    sg_ = m1 % 2
                    S.dma(ustg[sg_][:], puT[m1], writes=[("ustg", sg_)])
                    S.dma(vstg[sg_][:], pv_d[m1 * 128:(m1 + 1) * 128, :], writes=[("vstg", sg_)])
                    S.op('pool', lambda e, sg_=sg_, wb=wb: e.tensor_copy(out=ubf[wb][:], in_=ustg[sg_][:]), reads=[("ustg", sg_)], writes=[("ubf", wb)])
                    S.op('act', lambda e, sg_=sg_, wb=wb: e.activation(out=vbf[wb][:], in_=vstg[sg_][:], func=AF.Copy), reads=[("vstg", sg_)], writes=[("vbf", wb)])
                    S.dma(u_scr[m1], ubf[wb][:], reads=[("ubf", wb)], writes=[("dram_uscr", m1)], key=("ubf", wb))
                    S.dma(v_scr[m1], vbf[wb][:], reads=[("vbf", wb)], writes=[("dram_vscr", m1)], key=("vbf", wb))
                else:
                    S.dma(ubf[wb][:], u_scr[m1], reads=[("dram_uscr", m1)], writes=[("ubf", wb)])
                    S.dma(vbf[wb][:], v_scr[m1], reads=[("dram_vscr", m1)], writes=[("vbf", wb)])
                abk = m1 % 2
                for kk in range(8):
                    S.op('pe', lambda e, wb=wb, kk=kk, abk=abk, ntok=ntok: e.matmul(pAT[abk][:, 0:ntok], lhsT=ubf[wb][:, kk, :], rhs=x1T[:, kk, 0:ntok],
                                                                            start=(kk == 0), stop=(kk == 7)),
                         reads=[("ubf", wb)] + [("x1T", li) for li in range(ntl)], writes=["pAT%d" % abk])
                S.op('act', lambda e, abk=abk, ntok=ntok: e.activation(out=glb[abk][:, 0:ntok], in_=pAT[abk][:, 0:ntok], func=AF.Gelu_apprx_tanh),
                     reads=["pAT%d" % abk], writes=[("glb", abk)])
                S.op('dve', lambda e, abk=abk, m1=m1, ntok=ntok: e.tensor_tensor(out=WT[abk][:, 0:ntok], in0=glb[abk][:, 0:ntok], in1=G[:, m1, 0:ntok], op=ALU.mult),
                     reads=[("glb", abk)] + [("G", li) for li in range(ntl)], writes=[("WT", abk)])
                for li in range(ntl):
                    for half in range(2):
                        S.op('pe', lambda e, abk=abk, li=li, half=half, wb=wb, m1=m1: e.matmul(
                            pAcc[2 * li + half][:], lhsT=WT[abk][:, li * 128:(li + 1) * 128], rhs=vbf[wb][:, half * 512:(half + 1) * 512],
                            start=(m1 == 0), stop=(m1 == 127)), reads=[("WT", abk), ("vbf", wb)], writes=["pAcc%d" % (2 * li + half)])
            for li, t in enumerate(tiles):
                for half in range(2):
                    S.op('dve', lambda e, li=li, half=half: e.scalar_tensor_tensor(
                        out=pre2[:, half * 512:(half + 1) * 512], in0=x1g[li][:, half * 512:(half + 1) * 512], scalar=DN_ALPHA,
                        in1=pAcc[2 * li + half][:], op0=ALU.mult, op1=ALU.add),
                        reads=["pAcc%d" % (2 * li + half), ("x1g", li)], writes=[("pre2", half)])
                layer_norm(S, pre2, pre2, stats2, mv2, lng2, lnb2, [("pre2", 0), ("pre2", 1)], "y_sb", ["lng2", "lnb2"])
                S.dma(y_out[t], pre2[:], reads=["y_sb", ("pre2", 0), ("pre2", 1)], writes=[("dram_y", t)], key="ydma")

    S.finish('sp')
    with nc.Block() as block:
        S.replay(block)
    st.close()
    return nc


def _consts():
    ident = np.eye(128, dtype=np.float32)
    psw = np.zeros((128, 128), np.float32)
    for k in range(128):
        psw[k, (k + 64) % 128] = 1.0
    j = np.arange(128)
    tri_p = (j[:, None] <= j[None, :]).astype(np.float32)
    tri_s = tri_p * ((j[:, None] // 8) == (j[None, :] // 8))
    sg = np.zeros((128, 128), np.float32)
    sg[:64, 0] = -1.0
    sg[64:, 0] = 1.0
    return np.ascontiguousarray(np.stack([ident, psw, tri_p, tri_s, sg], axis=1))


def _shared(inp):
    f = lambda a: np.asarray(a, dtype=np.float32)
    a_re = f(inp['a_re']); a_im = f(inp['a_im']); log_dt = f(inp['log_dt'])
    b_re = f(inp['b_re']); b_im = f(inp['b_im']); c_re = f(inp['c_re']); c_im = f(inp['c_im'])
    bc = lambda a, shape: np.ascontiguousarray(np.broadcast_to(a, shape))
    sh = {}
    sh["w_in"] = np.ascontiguousarray(f(inp['w_in']))
    sh["b_in_bc"] = bc(f(inp['b_in'])[None, :], (128, PROJ))
    sh["b_in_fm"] = np.ascontiguousarray(f(inp['b_in']).reshape(32, 128).T)
    sh["are_tm"] = bc(a_re[None], (128, 32, 64)); sh["aim_tm"] = bc(a_im[None], (128, 32, 64))
    sh["ldt_tm"] = bc(log_dt[None], (128, 32))
    sh["are_fm"] = np.ascontiguousarray(np.concatenate([a_re.T, a_re.T], axis=0))
    sh["aim_fm"] = np.ascontiguousarray(np.concatenate([a_im.T, a_im.T], axis=0))
    g_idx = (np.arange(4)[None, :] * 8 + (np.arange(128) // 16)[:, None])
    ci_idx = np.arange(128) % 16
    sh["are_bd"] = np.ascontiguousarray(a_re[g_idx]); sh["aim_bd"] = np.ascontiguousarray(a_im[g_idx])
    sh["ldt_bd"] = np.ascontiguousarray(log_dt[g_idx])
    sh["bre_bd"] = np.ascontiguousarray(b_re[g_idx, :, ci_idx[:, None]])
    sh["bim_bd"] = np.ascontiguousarray(b_im[g_idx, :, ci_idx[:, None]])
    sh["maskbd"] = np.ascontiguousarray((((np.arange(128) // 16) % 4)[:, None] == np.arange(4)[None, :]).astype(np.float32))
    cf = np.concatenate([c_re.transpose(2, 0, 1), c_im.transpose(2, 0, 1)], axis=0)
    sh["c_fm"] = np.ascontiguousarray(cf)
    sh["d_fm"] = np.ascontiguousarray(f(inp['d_skip']).reshape(4, 128).T)
    sh["bglu_fm"] = np.ascontiguousarray(f(inp['b_glu']).reshape(4, 128).T)
    sh["w_glu"] = np.ascontiguousarray(f(inp['w_glu']))
    sh["cst"] = _consts()
    sh["w_a"] = np.ascontiguousarray(f(inp['w_a'])); sh["w_b"] = np.ascontiguousarray(f(inp['w_b'])); sh["w_o"] = np.ascontiguousarray(f(inp['w_o']))
    sh["ln1g_bc"] = bc(f(inp['ln1_g'])[None, :], (128, D)); sh["ln1b_bc"] = bc(f(inp['ln1_b'])[None, :], (128, D))
    slopes = (2.0 ** (-8.0 * (np.arange(8) + 1.0) / 8.0)).astype(np.float64)
    jj = np.arange(128, dtype=np.float64)
    slq = (-8.0 * slopes[None, :] * jj[:, None]) / 30000.0
    bexp = slopes[None, :, None] * (jj[:, None, None] - 128.0 * np.arange(16)[None, None, :])
    causal = np.where(jj[:, None] > jj[None, :], -30000.0, 0.0)
    sh["acst"] = np.ascontiguousarray(np.concatenate([slq, bexp.reshape(128, 128), causal], axis=1).astype(np.float32))
    lt = np.zeros((9, 9, 128), np.float32)
    for v in range(9):
        lt[8, v, :] = 1.0
        if v < 8:
            lt[v, v, :] = 1.0
    sh["ltab"] = lt
    pp = np.arange(128)
    blockind = ((pp[:, None] // 16) == np.arange(8)[None, :]).astype(np.float64)
    posk = 128.0 * (pp // 8) + 16.0 * (pp % 8)
    bexp_s = slopes[None, :, None] * (posk[:, None, None] + np.arange(16)[None, None, :] - 2048.0)
    bexp_new = slopes[None, :] * (pp % 8)[:, None]
    slq_s = np.zeros((128, 128)); slq_s[:, 0:8] = (-8.0 * slopes[None, :] * (pp % 8)[:, None]) / 30000.0
    slq_s[:, 8] = pp % 8
    causal_s = np.where(((pp[:, None] // 8) == (pp[None, :] // 8)) & ((pp[:, None] % 8) <= (pp[None, :] % 8)), 0.0, -30000.0)
    seqm = np.zeros((128, 128)); seqm[0:16, :] = np.where((pp[None, :] // 8) == np.arange(16)[:, None], 0.0, -30000.0)
    sh["scst"] = np.ascontiguousarray(np.concatenate([blockind, bexp_s.reshape(128, 128), bexp_new, slq_s, causal_s, seqm], axis=1).astype(np.float32))
    lseq = np.zeros((16, 17, 128), np.float32)
    for v in range(16):
        lseq[v, v, :] = 1.0
    sh["lseq"] = lseq
    lblk = np.zeros((9, 2, 128), np.float32)
    for n in range(8):
        lblk[n, 0, :] = (pp // 16 == n)
    lblk[8, :, :] = 1.0
    sh["lblk"] = lblk
    sh["cache_k"] = np.asarray(inp['cache_k']).reshape(2560 * 8, 16 * 512)
    sh["cache_v"] = np.asarray(inp['cache_v']).reshape(2560 * 8, 16 * 512)
    sh["w_pq"] = np.ascontiguousarray(f(inp['w_pq']))
    sk = np.stack([f(inp['sub_k1']), f(inp['sub_k2'])], axis=0)
    sh["skT"] = np.ascontiguousarray(sk.transpose(3, 0, 1, 2))
    pu = f(inp['peer_u']).reshape(128, 128, 8, 128)
    sh["peer_uT"] = np.ascontiguousarray(pu.transpose(0, 3, 2, 1))
    sh["peer_v"] = np.ascontiguousarray(f(inp['peer_v']))
    sh["ln2g_bc"] = bc(f(inp['ln2_g'])[None, :], (128, D)); sh["ln2b_bc"] = bc(f(inp['ln2_b'])[None, :], (128, D))
    return sh


def _prep_core(c, inp, sh):
    xp = np.asarray(inp['x_prompt'][c], dtype=np.float32)
    xs = np.asarray(inp['x_sample'][16 * c:16 * c + 16], dtype=np.float32).reshape(128, D)
    x = np.concatenate([xp, xs], axis=0).reshape(NT, 128, D)
    m = dict(sh)
    m["x_tm"] = np.ascontiguousarray(x)
    m["x_fm"] = np.ascontiguousarray(x.reshape(NT, 128, 8, 128).transpose(0, 3, 2, 1))
    hre = np.asarray(inp['state_ssm_re'][16 * c:16 * c + 16], np.float32).transpose(2, 1, 0)
    him = np.asarray(inp['state_ssm_im'][16 * c:16 * c + 16], np.float32).transpose(2, 1, 0)
    m["h0_fm"] = np.ascontiguousarray(np.concatenate([hre, him], axis=0))
    m["h0sw_fm"] = np.ascontiguousarray(np.concatenate([him, hre], axis=0))
    pt = np.asarray(inp['page_table'][16 * c:16 * c + 16], dtype=np.int32)
    m["pt_exp"] = np.ascontiguousarray(pt[:, np.arange(128) // 8].T)
    return m


def kernel(_debug=False, _glimit=None, **inp):
    nc = bass.Bass("TRN2", target_bir_lowering=False)
    build(nc, debug=_debug, glimit=_glimit)
    sh = _shared(inp)
    in_maps = [_prep_core(c, inp, sh) for c in range(NCORES)]
    res = run_bass_kernel_spmd(nc, in_maps, core_ids=list(range(NCORES)))
    R = res.results
    sp = np.stack([R[c]["ssm_p"] for c in range(NCORES)]).reshape(8, 32, 2, 64)
    ss = np.concatenate([R[c]["ssm_s"] for c in range(NCORES)], axis=0).reshape(128, 32, 2, 64)
    y_all = np.stack([R[c]["y_out"] for c in range(NCORES)])
    k_all = np.stack([R[c]["k_out"] for c in range(NCORES)])
    v_all = np.stack([R[c]["v_out"] for c in range(NCORES)])
    outs = (np.ascontiguousarray(y_all[:, :16].reshape(8, SEQ, D)),
            np.ascontiguousarray(y_all[:, 16].reshape(128, 8, D)),
            np.ascontiguousarray(k_all[:, :16].reshape(8, SEQ, 8, 64)),
            np.ascontiguousarray(v_all[:, :16].reshape(8, SEQ, 8, 64)),
            np.ascontiguousarray(k_all[:, 16].reshape(128, 8, 8, 64)),
            np.ascontiguousarray(v_all[:, 16].reshape(128, 8, 8, 64)),
            np.ascontiguousarray(sp[:, :, 0]), np.ascontiguousarray(sp[:, :, 1]),
            np.ascontiguousarray(ss[:, :, 0]), np.ascontiguousarray(ss[:, :, 1]))
    if _debug:
        return outs, R
    return outs
```
